# Optimizing a Trainium2 kernel written in Bass

```python
import jax
import jax.numpy as jnp
from jax import lax
import numpy as np

D_MODEL = 1024
BATCH = 8
SEQ = 2048
DEPTH = 2

CHUNK = 64

RET_HEADS = D_MODEL // 256
RET_DK = 64
RET_DV = 64
RET_W = RET_HEADS * RET_DV

SSD_HEADS = D_MODEL // 128
SSD_HEAD_DIM = 64
SSD_STATE = 64
SSD_GROUPS = 2
SSD_CONV = 4
SSD_W = SSD_HEADS * SSD_HEAD_DIM
SSD_XBC = SSD_W + 2 * SSD_GROUPS * SSD_STATE

GLA_HEADS = D_MODEL // 256
GLA_DK = 32
GLA_DV = 64
GLA_GATE_RANK = 16
GLA_GATE_TEMP = 16.0
GLA_W = GLA_HEADS * GLA_DV

D_MIX = RET_W + SSD_W + GLA_W

IN_SPLITS = (RET_HEADS * RET_DK, RET_HEADS * RET_DK, RET_W, RET_W,
             SSD_W, SSD_XBC, SSD_HEADS,
             GLA_HEADS * GLA_DK, GLA_HEADS * GLA_DK, GLA_W, GLA_GATE_RANK, GLA_W)
D_IN = sum(IN_SPLITS)

N_EXPERTS = 32
TOP_K = 4
D_FF = D_MODEL
SWIGLU_LIMIT = 7.0
SWIGLU_ALPHA = 1.702
MOE_BLOCK = 128

ROPE_BASE = 10000.0
LN_EPS = 1e-5
NORM_EPS = 1e-6
DEEPNORM_ALPHA = (2.0 * DEPTH) ** 0.25
DEEPNORM_BETA = (8.0 * DEPTH) ** -0.25

kernel_name = 'hybrid_ret_ssd_gla_moe_deepnorm'


def _chunks(t):
    return t.reshape(t.shape[0], t.shape[1] // CHUNK, CHUNK, *t.shape[2:])


def _unchunk(t):
    return t.reshape(t.shape[0], t.shape[1] * t.shape[2], *t.shape[3:])


def _layer_norm(x, g, b):
    xf = x.astype(jnp.float32)
    mu = jnp.mean(xf, -1, keepdims=True)
    var = jnp.mean(jnp.square(xf - mu), -1, keepdims=True)
    return ((xf - mu) * lax.rsqrt(var + LN_EPS) * g + b).astype(x.dtype)


def _rms(t):
    return t * lax.rsqrt(jnp.mean(jnp.square(t), -1, keepdims=True) + NORM_EPS)


def _scan_chunk_states(decay, contrib):
    def step(state, inp):
        a, u = inp
        return a * state + u, state
    init = jnp.zeros_like(contrib[:, 0])
    _, prev = lax.scan(step, init, (jnp.moveaxis(decay, 1, 0), jnp.moveaxis(contrib, 1, 0)))
    return jnp.moveaxis(prev, 0, 1)


def _rotary(t, positions):
    half = t.shape[-1] // 2
    inv_freq = ROPE_BASE ** (-jnp.arange(half, dtype=jnp.float32) / half)
    ang = positions.astype(jnp.float32)[:, :, None] * inv_freq
    cos = jnp.cos(ang)[:, :, None, :]
    sin = jnp.sin(ang)[:, :, None, :]
    t1, t2 = t[..., :half], t[..., half:]
    return jnp.concatenate([t1 * cos - t2 * sin, t1 * sin + t2 * cos], -1)


def _retention(q, k, v, g, positions, norm_w):
    bsz, seq, _ = q.shape
    f32 = jnp.float32
    q = _rotary(q.reshape(bsz, seq, RET_HEADS, RET_DK).astype(f32), positions)
    k = _rotary(k.reshape(bsz, seq, RET_HEADS, RET_DK).astype(f32), positions) * RET_DK ** -0.5
    v = v.reshape(bsz, seq, RET_HEADS, RET_DV).astype(f32)
    log_gamma = jnp.log(1.0 - 2.0 ** (-5.0 - jnp.arange(RET_HEADS, dtype=f32)))
    pos = jnp.arange(CHUNK, dtype=f32)
    dist = pos[:, None] - pos[None, :]
    intra = jnp.where(dist >= 0, jnp.exp(log_gamma[:, None, None] * jnp.maximum(dist, 0.0)), 0.0)
    qc, kc, vc = _chunks(q), _chunks(k), _chunks(v)
    scores = jnp.einsum('bnchd,bnshd->bnhcs', qc, kc) * intra
    o = jnp.einsum('bnhcs,bnshe->bnche', scores, vc)
    k_decay = jnp.exp(log_gamma[None, :] * (CHUNK - 1.0 - pos)[:, None])
    contrib = jnp.einsum('bnshd,bnshe->bnhde', kc * k_decay[:, :, None], vc)
    chunk_decay = jnp.broadcast_to(jnp.exp(log_gamma * CHUNK)[:, None, None],
                                   (bsz, seq // CHUNK, RET_HEADS, 1, 1))
    s_prev = _scan_chunk_states(chunk_decay, contrib)
    q_decay = jnp.exp(log_gamma[None, :] * (pos + 1.0)[:, None])
    o = o + jnp.einsum('bnchd,bnhde->bnche', qc * q_decay[:, :, None], s_prev)
    o = _unchunk(o)
    mu = jnp.mean(o, -1, keepdims=True)
    o = (o - mu) * lax.rsqrt(jnp.mean(jnp.square(o - mu), -1, keepdims=True) + LN_EPS)
    o = o.reshape(bsz, seq, RET_W) * norm_w
    return jax.nn.silu(g.astype(f32)) * o


def _ssd(z, xbc, dt_raw, conv_w, conv_b, dt_bias, a_log, d_skip, norm_w):
    bsz, seq, _ = xbc.shape
    f32 = jnp.float32
    hpg = SSD_HEADS // SSD_GROUPS
    xbc = lax.conv_general_dilated(xbc.astype(f32), conv_w.astype(f32)[:, None, :],
                                   window_strides=(1,), padding=[(SSD_CONV - 1, 0)],
                                   dimension_numbers=('NWC', 'WIO', 'NWC'),
                                   feature_group_count=SSD_XBC)
    xbc = jax.nn.silu(xbc + conv_b)
    xs, bm, cm = jnp.split(xbc, [SSD_W, SSD_W + SSD_GROUPS * SSD_STATE], axis=-1)
    xc = _chunks(xs.reshape(bsz, seq, SSD_GROUPS, hpg, SSD_HEAD_DIM))
    bc = _chunks(bm.reshape(bsz, seq, SSD_GROUPS, SSD_STATE))
    cc = _chunks(cm.reshape(bsz, seq, SSD_GROUPS, SSD_STATE))
    dt = jax.nn.softplus(dt_raw.astype(f32) + dt_bias)
    dtc = _chunks(dt.reshape(bsz, seq, SSD_GROUPS, hpg))
    a = -jnp.exp(a_log.astype(f32)).reshape(SSD_GROUPS, hpg)
    a_cum = jnp.cumsum(dtc * a, axis=2)
    causal = jnp.tril(jnp.ones((CHUNK, CHUNK), dtype=bool))[:, :, None, None]
    seg = a_cum[:, :, :, None] - a_cum[:, :, None, :]
    decay_mat = jnp.exp(jnp.where(causal, seg, -jnp.inf))
    cb = jnp.einsum('bncgk,bnsgk->bncsg', cc, bc)
    xdt = xc * dtc[..., None]
    y = jnp.einsum('bncsgj,bnsgjp->bncgjp', cb[..., None] * decay_mat, xdt)
    state_decay = jnp.exp(a_cum[:, :, -1:] - a_cum)
    contrib = jnp.einsum('bnsgk,bnsgjp->bngjpk', bc, xdt * state_decay[..., None])
    s_prev = _scan_chunk_states(jnp.exp(a_cum[:, :, -1])[..., None, None], contrib)
    y = y + jnp.einsum('bncgk,bngjpk->bncgjp', cc, s_prev) * jnp.exp(a_cum)[..., None]
    y = y + d_skip.astype(f32).reshape(SSD_GROUPS, hpg)[:, :, None] * xc
    y = _unchunk(y).reshape(bsz, seq, SSD_GROUPS, hpg * SSD_HEAD_DIM)
    zg = jax.nn.silu(z.astype(f32)).reshape(bsz, seq, SSD_GROUPS, hpg * SSD_HEAD_DIM)
    return _rms(y * zg).reshape(bsz, seq, SSD_W) * norm_w


def _gla(q, k, v, gk_low, g, w_gk2, b_gk2, norm_w):
    bsz, seq, _ = q.shape
    f32 = jnp.float32
    q = q.reshape(bsz, seq, GLA_HEADS, GLA_DK).astype(f32) * GLA_DK ** -0.5
    k = k.reshape(bsz, seq, GLA_HEADS, GLA_DK).astype(f32)
    v = v.reshape(bsz, seq, GLA_HEADS, GLA_DV).astype(f32)
    gk = jnp.einsum('bsr,rk->bsk', gk_low.astype(f32), w_gk2.astype(f32)) + b_gk2
    log_a = (jax.nn.log_sigmoid(gk) / GLA_GATE_TEMP).reshape(bsz, seq, GLA_HEADS, GLA_DK)
    qc, kc, vc = _chunks(q), _chunks(k), _chunks(v)
    b = jnp.cumsum(_chunks(log_a), axis=2)
    q_t = qc * jnp.exp(b)
    att = jnp.einsum('bnchd,bnshd->bnhcs', q_t, kc * jnp.exp(-b))
    causal = jnp.tril(jnp.ones((CHUNK, CHUNK), dtype=bool))
    o = jnp.einsum('bnhcs,bnshe->bnche', jnp.where(causal, att, 0.0), vc)
    b_last = b[:, :, -1:]
    contrib = jnp.einsum('bnshd,bnshe->bnhde', kc * jnp.exp(b_last - b), vc)
    s_prev = _scan_chunk_states(jnp.exp(b_last[:, :, 0])[..., None], contrib)
    o = o + jnp.einsum('bnchd,bnhde->bnche', q_t, s_prev)
    o = _rms(_unchunk(o)).reshape(bsz, seq, GLA_W) * norm_w
    return jax.nn.silu(g.astype(f32)) * o


def _moe(x, w_router, b_router, w_gate, b_gate, w_up, b_up, w_down, b_down):
    bsz, seq, d = x.shape
    n_tok = bsz * seq
    xf = x.reshape(n_tok, d)
    logits = (xf @ w_router + b_router).astype(jnp.float32)
    top_logit, top_idx = lax.top_k(logits, TOP_K)
    gates = jax.nn.softmax(top_logit, axis=-1)
    n_assign = n_tok * TOP_K
    expert_of = top_idx.reshape(-1)
    order = jnp.argsort(expert_of)
    sorted_e = expert_of[order]
    sorted_tok = order // TOP_K
    counts = jnp.zeros((N_EXPERTS,), jnp.int32).at[expert_of].add(1)
    padded = (counts + MOE_BLOCK - 1) // MOE_BLOCK * MOE_BLOCK
    start_sorted = jnp.cumsum(counts) - counts
    end_padded = jnp.cumsum(padded)
    start_padded = end_padded - padded
    dest = start_padded[sorted_e] + jnp.arange(n_assign, dtype=jnp.int32) - start_sorted[sorted_e]
    n_blocks = -(-n_assign // MOE_BLOCK) + N_EXPERTS
    cap = n_blocks * MOE_BLOCK
    slot_tok = jnp.zeros((cap,), jnp.int32).at[dest].set(sorted_tok)
    block_start = jnp.arange(n_blocks, dtype=jnp.int32) * MOE_BLOCK
    block_expert = jnp.minimum(jnp.searchsorted(end_padded, block_start, side='right'), N_EXPERTS - 1)
    xin = xf[slot_tok].reshape(n_blocks, MOE_BLOCK, d)

    def expert_block(args):
        xb, e = args
        h_g = jnp.minimum(xb @ w_gate[e] + b_gate[e], SWIGLU_LIMIT)
        h_u = jnp.clip(xb @ w_up[e] + b_up[e], -SWIGLU_LIMIT, SWIGLU_LIMIT)
        h = (h_u + 1.0) * h_g * jax.nn.sigmoid(SWIGLU_ALPHA * h_g)
        return h @ w_down[e] + b_down[e]

    yb = lax.map(expert_block, (xin, block_expert)).reshape(cap, d)
    y_sorted = yb[dest] * gates.reshape(-1)[order][:, None]
    y = jax.ops.segment_sum(y_sorted, sorted_tok, num_segments=n_tok)
    return y.reshape(bsz, seq, d).astype(x.dtype)


def setup_inputs(seed: int = 0) -> dict:
    key = jax.random.key(seed)
    ks = jax.random.split(key, 32)
    f32 = jnp.float32
    L = DEPTH

    def nrm(k, shape, s):
        return jax.random.normal(k, shape, f32) * s

    col_scale = np.ones((D_IN,), np.float32)
    off = np.cumsum((0,) + IN_SPLITS)
    for seg, width in ((2, RET_W), (5, SSD_W), (9, GLA_W)):
        col_scale[off[seg]:off[seg] + width] = DEEPNORM_BETA
    x = nrm(ks[0], (BATCH, SEQ, D_MODEL), 1.0)
    positions = (jax.random.randint(ks[1], (BATCH, 1), 0, 16, dtype=jnp.int32) * CHUNK
                 + jnp.arange(SEQ, dtype=jnp.int32)[None, :])
    w_in = nrm(ks[2], (L, D_MODEL, D_IN), D_MODEL ** -0.5) * jnp.asarray(col_scale)
    w_out = nrm(ks[3], (L, D_MIX, D_MODEL), D_MIX ** -0.5 * DEEPNORM_BETA)
    ret_norm_w = 1.0 + nrm(ks[4], (L, RET_W), 0.02)
    ssd_conv_w = nrm(ks[5], (L, SSD_CONV, SSD_XBC), SSD_CONV ** -0.5)
    ssd_conv_b = nrm(ks[6], (L, SSD_XBC), 0.02)
    dt0 = jnp.exp(jax.random.uniform(ks[7], (L, SSD_HEADS), f32, np.log(1e-3), np.log(1e-1)))
    ssd_dt_bias = dt0 + jnp.log(-jnp.expm1(-dt0))
    ssd_a_log = jnp.log(jax.random.uniform(ks[8], (L, SSD_HEADS), f32, 1.0, 16.0))
    ssd_d = 1.0 + nrm(ks[9], (L, SSD_HEADS), 0.02)
    ssd_norm_w = 1.0 + nrm(ks[10], (L, SSD_W), 0.02)
    gla_w_gk2 = nrm(ks[11], (L, GLA_GATE_RANK, GLA_HEADS * GLA_DK), GLA_GATE_RANK ** -0.5)
    gla_b_gk2 = nrm(ks[12], (L, GLA_HEADS * GLA_DK), 0.02)
    gla_norm_w = 1.0 + nrm(ks[13], (L, GLA_W), 0.02)
    ln1_g = 1.0 + nrm(ks[14], (L, D_MODEL), 0.02)
    ln1_b = nrm(ks[15], (L, D_MODEL), 0.02)
    w_router = nrm(ks[16], (L, D_MODEL, N_EXPERTS), D_MODEL ** -0.5)
    b_router = nrm(ks[17], (L, N_EXPERTS), 0.01)
    w_gate = nrm(ks[18], (L, N_EXPERTS, D_MODEL, D_FF), D_MODEL ** -0.5 * DEEPNORM_BETA)
    b_gate = nrm(ks[19], (L, N_EXPERTS, D_FF), 0.02)
    w_up = nrm(ks[20], (L, N_EXPERTS, D_MODEL, D_FF), D_MODEL ** -0.5 * DEEPNORM_BETA)
    b_up = nrm(ks[21], (L, N_EXPERTS, D_FF), 0.02)
    w_down = nrm(ks[22], (L, N_EXPERTS, D_FF, D_MODEL), D_FF ** -0.5 * DEEPNORM_BETA)
    b_down = nrm(ks[23], (L, N_EXPERTS, D_MODEL), 0.02)
    ln2_g = 1.0 + nrm(ks[24], (L, D_MODEL), 0.02)
    ln2_b = nrm(ks[25], (L, D_MODEL), 0.02)
    return {'x': x, 'positions': positions, 'w_in': w_in, 'w_out': w_out,
            'ret_norm_w': ret_norm_w, 'ssd_conv_w': ssd_conv_w, 'ssd_conv_b': ssd_conv_b,
            'ssd_dt_bias': ssd_dt_bias, 'ssd_a_log': ssd_a_log, 'ssd_d': ssd_d,
            'ssd_norm_w': ssd_norm_w, 'gla_w_gk2': gla_w_gk2, 'gla_b_gk2': gla_b_gk2,
            'gla_norm_w': gla_norm_w, 'ln1_g': ln1_g, 'ln1_b': ln1_b,
            'w_router': w_router, 'b_router': b_router, 'w_gate': w_gate, 'b_gate': b_gate,
            'w_up': w_up, 'b_up': b_up, 'w_down': w_down, 'b_down': b_down,
            'ln2_g': ln2_g, 'ln2_b': ln2_b}


def reference(x, positions, w_in, w_out, ret_norm_w, ssd_conv_w, ssd_conv_b, ssd_dt_bias,
              ssd_a_log, ssd_d, ssd_norm_w, gla_w_gk2, gla_b_gk2, gla_norm_w, ln1_g, ln1_b,
              w_router, b_router, w_gate, b_gate, w_up, b_up, w_down, b_down, ln2_g, ln2_b):
    bounds = []
    acc = 0
    for width in IN_SPLITS[:-1]:
        acc += width
        bounds.append(acc)
    for l in range(DEPTH):
        proj = jnp.einsum('bsd,de->bse', x, w_in[l])
        (r_q, r_k, r_v, r_g, s_z, s_xbc, s_dt,
         g_q, g_k, g_v, g_gk, g_g) = jnp.split(proj, bounds, axis=-1)
        h_ret = _retention(r_q, r_k, r_v, r_g, positions, ret_norm_w[l])
        h_ssd = _ssd(s_z, s_xbc, s_dt, ssd_conv_w[l], ssd_conv_b[l], ssd_dt_bias[l],
                     ssd_a_log[l], ssd_d[l], ssd_norm_w[l])
        h_gla = _gla(g_q, g_k, g_v, g_gk, g_g, gla_w_gk2[l], gla_b_gk2[l], gla_norm_w[l])
        h = jnp.concatenate([h_ret, h_ssd, h_gla], axis=-1).astype(x.dtype)
        mix = jnp.einsum('bse,ed->bsd', h, w_out[l])
        x = _layer_norm(DEEPNORM_ALPHA * x + mix, ln1_g[l], ln1_b[l])
        ffn = _moe(x, w_router[l], b_router[l], w_gate[l], b_gate[l], w_up[l], b_up[l],
                   w_down[l], b_down[l])
        x = _layer_norm(DEEPNORM_ALPHA * x + ffn, ln2_g[l], ln2_b[l])
    return x
```

```python
import contextlib
import numpy as np
import concourse.bass as bass
import concourse.mybir as mybir
from concourse.bass_utils import run_bass_kernel_spmd

F32 = mybir.dt.float32
BF16 = mybir.dt.bfloat16
I32 = mybir.dt.int32
AF = mybir.ActivationFunctionType
ALU = mybir.AluOpType
AX = mybir.AxisListType

D = 1024
T = 2048
DEPTH = 2
NE = 32
ALPHA = (2.0 * DEPTH) ** 0.25
LN_EPS = 1e-5
NORM_EPS = 1e-6
NCH = 8
NTG = 4
TG = 512

ENGS = ("pe", "act", "dve", "pool", "sp")
EPOCH = 8000
NDMASEM = 10


class Op:
    __slots__ = ("eng", "fn", "dma", "deps", "idx", "sig", "dsem", "dval", "nsig", "prewait")

    def __init__(self, eng, fn, dma):
        self.eng = eng
        self.fn = fn
        self.dma = dma
        self.deps = {}
        self.sig = False
        self.nsig = None
        self.dsem = None
        self.dval = None
        self.prewait = None


class Sched:
    def __init__(self, nc):
        self.nc = nc
        self.ops = []
        self.state = {}
        self.dma_count = {e: 0 for e in ENGS}
        self.dma_hist = {e: [] for e in ENGS}
        self.fence_op = None
        self.fenced = set()
        self.last = {e: None for e in ENGS}

    def _dep(self, op, prod, kind):
        if prod is None or prod is op:
            return
        if prod.dma:
            op.deps[("d", id(prod))] = prod
            return
        if prod.eng == op.eng and not op.dma:
            if op.eng == "pe":
                return
        cur = op.deps.get(prod.eng)
        if cur is None or cur.idx < prod.idx:
            op.deps[prod.eng] = prod

    def add(self, eng, fn, reads=(), writes=(), dma=False):
        op = Op(eng, fn, dma)
        op.idx = len(self.ops)
        if self.fence_op is not None and eng not in self.fenced:
            self.fenced.add(eng)
            for f in self.fence_op:
                self._dep(op, f, "raw" if f.eng != eng else "waw")
        for r in reads:
            st = self.state.get(r)
            if st is None:
                st = self.state[r] = [None, []]
            self._dep(op, st[0], "raw")
            if isinstance(r, tuple) and r[0] == "ps":
                for rd in st[1]:
                    if rd.eng != eng:
                        self._dep(op, rd, "war")
        for w in writes:
            st = self.state.get(w)
            if st is None:
                st = self.state[w] = [None, []]
            self._dep(op, st[0], "waw")
            for rd in st[1]:
                self._dep(op, rd, "war")
        for r in reads:
            self.state[r][1].append(op)
        for w in writes:
            st = self.state[w]
            st[0] = op
            st[1] = []
        if dma:
            k = self.dma_count[eng]
            self.dma_count[eng] = k + 1
            op.dsem = k % NDMASEM
            op.dval = 16 * (k // NDMASEM + 1)
            hist = self.dma_hist[eng]
            if k >= NDMASEM:
                op.prewait = hist[k - NDMASEM]
            hist.append(op)
        else:
            self.last[eng] = op
        self.ops.append(op)
        return op

    def fence(self):
        prods = [o for o in self.last.values() if o is not None]
        for e in ENGS:
            prods.extend(self.dma_hist[e][-NDMASEM:])
        self.fence_op = prods
        self.fenced = set()

    def emit(self, final_ops=()):
        nc = self.nc
        for op in self.ops:
            for p in op.deps.values():
                if not p.dma:
                    p.sig = True
        counts = {e: 0 for e in ENGS}
        for op in self.ops:
            if op.sig and not op.dma:
                counts[op.eng] += 1
                op.nsig = counts[op.eng]
        with contextlib.ExitStack() as es:
            csem = {}
            for e in ENGS:
                n_ep = counts[e] // EPOCH + 1
                csem[e] = [es.enter_context(nc.semaphore(f"c_{e}_{i}")) for i in range(n_ep)]
            dsem = {}
            for e in ENGS:
                if self.dma_count[e]:
                    dsem[e] = [es.enter_context(nc.semaphore(f"d_{e}_{i}"))
                               for i in range(min(NDMASEM, self.dma_count[e]))]
            block = es.enter_context(nc.Block())
            per_eng = {e: [o for o in self.ops if o.eng == e] for e in ENGS}

            def wait_for(engine, p, waited):
                if p.dma:
                    key, val = (p.eng, "d", p.dsem), p.dval
                else:
                    ep, v = divmod(p.nsig - 1, EPOCH)
                    if waited.get((p.eng, "ep"), -1) > ep:
                        return
                    key, val = (p.eng, "c", ep), v + 1
                if waited.get(key, 0) >= val:
                    return
                waited[key] = val
                if p.dma:
                    engine.wait_ge(dsem[p.eng][p.dsem], val)
                else:
                    waited[(p.eng, "ep")] = max(waited.get((p.eng, "ep"), -1), ep)
                    engine.wait_ge(csem[p.eng][ep], val)

            def body(ename, tail):
                def f(engine):
                    waited = {}
                    for op in per_eng[ename]:
                        if op.prewait is not None:
                            wait_for(engine, op.prewait, waited)
                        for p in op.deps.values():
                            wait_for(engine, p, waited)
                        ins = op.fn(engine)
                        if op.dma:
                            ins.then_inc(dsem[ename][op.dsem], 16)
                        elif op.sig:
                            ep, v = divmod(op.nsig - 1, EPOCH)
                            ins.then_inc(csem[ename][ep], 1)
                    if tail:
                        for p in final_ops:
                            wait_for(engine, p, waited)
                return f

            block.sync(body("sp", True))
            block.tensor(body("pe", False))
            block.scalar(body("act", False))
            block.vector(body("dve", False))
            block.gpsimd(body("pool", False))


def _fm(v):
    v = np.asarray(v, np.float32)
    return np.ascontiguousarray(v.reshape(-1, 128).T)


class Pack:
    def __init__(self):
        self.cols = {}
        self.n = 0
        self.parts = []

    def put(self, name, arr):
        arr = np.asarray(arr, np.float32)
        if arr.ndim == 1:
            arr = arr[:, None]
        p, w = arr.shape
        full = np.zeros((128, w), np.float32)
        full[:p] = arr
        self.cols[name] = (self.n, w)
        self.n += w
        self.parts.append(full)

    def array(self):
        return np.ascontiguousarray(np.concatenate(self.parts, axis=1))


def moe_pack(l, inp):
    pk = Pack()
    pk.put("ln2_g", _fm(inp["ln2_g"][l]))
    pk.put("ln2_b", _fm(inp["ln2_b"][l]))
    pk.put("b_gate", inp["b_gate"][l].reshape(NE, 8, 128).transpose(2, 0, 1).reshape(128, NE * 8))
    pk.put("b_up", inp["b_up"][l].reshape(NE, 8, 128).transpose(2, 0, 1).reshape(128, NE * 8))
    pk.put("w_router", inp["w_router"][l].reshape(8, 128, NE).transpose(1, 0, 2).reshape(128, 8 * NE))
    pk.put("b_router", np.broadcast_to(inp["b_router"][l][None, :], (128, NE)))
    pk.put("b_down", inp["b_down"][l])
    return pk


NWIN = 3096 + 512
CAP = 512


def w_in_ext(w_in_l):
    w = np.asarray(w_in_l, np.float32)
    idx = []
    for base in (0, 256):
        for h in range(4):
            idx += list(range(base + 64 * h + 32, base + 64 * h + 64)) + list(range(base + 64 * h, base + 64 * h + 32))
    return np.ascontiguousarray(np.concatenate([w, w[:, idx]], axis=1))


def mix_pack(l, inp):
    pk = Pack()
    pk.put("ln1_g", _fm(inp["ln1_g"][l]))
    pk.put("ln1_b", _fm(inp["ln1_b"][l]))
    cw = np.asarray(inp["ssd_conv_w"][l], np.float32)
    pk.put("conv_w", cw.reshape(4, 6, 128).transpose(2, 1, 0).reshape(128, 24))
    pk.put("conv_b", np.asarray(inp["ssd_conv_b"][l], np.float32).reshape(6, 128).T)
    pk.put("dt_bias", np.asarray(inp["ssd_dt_bias"][l], np.float32).reshape(8, 1))
    pk.put("a_log", np.asarray(inp["ssd_a_log"][l], np.float32).reshape(8, 1))
    pk.put("bgk", np.asarray(inp["gla_b_gk2"][l], np.float32).reshape(2, 64).T)
    pk.put("w_gk2", np.asarray(inp["gla_w_gk2"][l], np.float32))
    pk.put("ret_nw", np.broadcast_to(np.asarray(inp["ret_norm_w"][l], np.float32)[None, :], (128, 256)))
    pk.put("ssd_nw", np.broadcast_to(np.asarray(inp["ssd_norm_w"][l], np.float32)[None, :], (128, 512)))
    pk.put("gla_nw", np.broadcast_to(np.asarray(inp["gla_norm_w"][l], np.float32)[None, :], (128, 256)))
    pk.put("ssd_d", np.broadcast_to(np.asarray(inp["ssd_d"][l], np.float32)[None, :], (128, 8)))
    return pk


def const_pack():
    pk = Pack()
    pk.put("ident", np.eye(128, dtype=np.float32))
    pk.put("onesm", np.full((128, 128), 1.0 / D, np.float32))
    pk.put("ones", np.ones((128, 128), np.float32))
    s_ = np.arange(128)[:, None].astype(np.float64)
    c_ = np.arange(128)[None, :].astype(np.float64)
    causal = (c_ >= s_).astype(np.float64)
    pk.put("causal", causal)
    gam = [1.0 - 2.0 ** (-5.0 - h) for h in range(4)]
    pk.put("retmask", np.concatenate([np.where(c_ >= s_, gam[h] ** np.maximum(c_ - s_, 0.0), 0.0) * 0.125 for h in range(4)], axis=1))
    pk.put("gq", np.concatenate([np.broadcast_to(gam[h] ** (c_ + 1.0), (64, 128)) for h in range(4)], axis=1))
    pk.put("gk", np.concatenate([gam[h] ** (127.0 - s_) * 0.125 for h in range(4)], axis=1))
    d_ = np.arange(128) % 64
    invf = (10000.0 ** (-(d_ % 32).astype(np.float64) / 32.0)).astype(np.float32).astype(np.float64) / (2.0 * np.pi)
    pk.put("invf", invf.reshape(128, 1))
    pk.put("sgn", np.where(d_ < 32, -1.0, 1.0).reshape(128, 1))
    pk.put("gq2", np.concatenate([np.concatenate([np.broadcast_to(gam[2 * p2 + j] ** (c_ + 1.0), (64, 128)) for j in range(2)], axis=0) for p2 in range(2)], axis=1))
    pk.put("triu", (s_ < c_).astype(np.float64))
    pk.put("ec1", np.broadcast_to((np.arange(NE) * CAP + 1.0)[None, :], (128, NE)))
    sel = np.zeros((8, 8 * 128))
    for h in range(8):
        sel[h, h * 128:(h + 1) * 128] = 1.0
    pk.put("sel", sel)
    selm = np.zeros((8, 2 * 4 * 128))
    for g_ in range(2):
        for j_ in range(4):
            selm[4 * g_ + j_, g_ * 512 + j_ * 128:g_ * 512 + (j_ + 1) * 128] = 1.0
    pk.put("selm", selm)
    return pk


class Builder:
    def __init__(self, nlayers, moe_cols, const_cols, mode="full", n_experts=NE, e_lo=0, e_hi=NE, init=True, ln=True,
                 mix_cols=None):
        self.L = nlayers
        self.mode = mode
        self.n_experts = n_experts
        self.e_lo, self.e_hi, self.init, self.ln = e_lo, e_hi, init, ln
        self.mix_cols = mix_cols
        self.moe_cols = moe_cols
        self.const_cols = const_cols
        self.nc = bass.Bass("TRN2", target_bir_lowering=False)
        self.S = Sched(self.nc)
        self.sb_off = (self.nc.sbuf_base + 31) // 32 * 32
        self.sb_top = self.nc.sbuf_top
        self.uid = 0
        self.finals = []

    def sb(self, shape, dt, name=None):
        nbytes = int(np.prod(shape[1:])) * (4 if dt in (F32, I32) else 2)
        nbytes = (nbytes + 31) // 32 * 32
        off = self.sb_off
        assert off + nbytes <= self.sb_top, f"SBUF overflow allocating {name} {shape}: {off + nbytes - self.sb_top}"
        self.sb_off += nbytes
        self.uid += 1
        return self.nc.alloc_sbuf_tensor_at(f"{name or 't'}{self.uid}", list(shape), dt, offset=off)

    def sb_cached(self, key, shape, dt):
        if not hasattr(self, "_cached"):
            self._cached = {}
        if key in self._cached:
            t, off, nbytes = self._cached[key]
            assert off == self.sb_off, (key, off, self.sb_off)
            self.sb_off += nbytes
            return t
        off = self.sb_off
        t = self.sb(shape, dt, key)
        self._cached[key] = (t, off, self.sb_off - off)
        return t

    def mark(self):
        return self.sb_off

    def release(self, m):
        self.sb_off = m

    def dram_in(self, name, shape, dt=F32):
        return self.nc.dram_tensor(name, list(shape), dt, kind="ExternalInput").ap()

    def dram_out(self, name, shape, dt=F32):
        return self.nc.dram_tensor(name, list(shape), dt, kind="ExternalOutput").ap()

    def col(self, tile, cols, name, lo=0, n=None):
        c0, w = cols[name]
        if n is None:
            n = w - lo
        return tile[:, c0 + lo:c0 + lo + n]

    def build(self):
        nc, S, L = self.nc, self.S, self.L
        ncc = max(v[0] + v[1] for v in self.const_cols.values())
        nmc = max(v[0] + v[1] for v in self.moe_cols.values())
        nxc = max(v[0] + v[1] for v in self.mix_cols.values())
        self.d_xT = self.dram_in("xT", [D, T])
        self.d_const = self.dram_in("consts", [128, ncc])
        self.d_out = self.dram_out("outT", [D, T])
        self.d_moep = self.dram_in("moep", [L, 128, nmc])
        self.d_wg = self.dram_in("w_gate", [L, NE, D, D])
        self.d_wu = self.dram_in("w_up", [L, NE, D, D])
        self.d_wd = self.dram_in("w_down", [L, NE, D, D])
        self.d_mixp = self.dram_in("mixp", [L, 128, nxc])
        self.d_win = self.dram_in("w_in_ext", [L, D, NWIN])
        self.d_wout = self.dram_in("w_out", [L, D, D])
        self.d_posb = self.dram_in("posb", [128, T], I32)
        self.d_rot = nc.dram_tensor("rotd", [2, 128, T], F32).ap()
        sA = [nc.dram_tensor(f"scrA{l}", [D, T], F32).ap() for l in range(L)]
        sB = [nc.dram_tensor(f"scrB{l}", [D, T], F32).ap() for l in range(L - 1)]

        NS = NE * CAP + 1
        self.Xd = [nc.dram_tensor(f"Xd{l}", [NS, D], BF16).ap() for l in range(L)]
        self.Yd = [nc.dram_tensor(f"Yd{l}", [NS, D], BF16).ap() for l in range(L)]
        self.cst = self.sb([128, ncc], F32, "cst")
        self.psum = [nc.alloc_psum_tensor(f"ps{i}", [128, 512], F32) for i in range(8)]
        S.add("sp", lambda e: e.dma_start(out=self.cst[:], in_=self.d_const), writes=["cst"], dma=True)
        zt = self.sb([128, D], BF16, "zt")
        zbar = self.sb([1, 8], F32, "zbar")
        S.add("pool", lambda e: e.memset(zt[:], 0.0), writes=["zt"])
        self.identb = self.sb([128, 128], BF16, "identb")
        S.add("act", lambda e: e.activation(out=self.identb[:], in_=self.col(self.cst, self.const_cols, "ident"), func=AF.Copy), reads=["cst"], writes=["identb"])
        base = self.mark()
        for l in range(L):
            src = self.d_xT if l == 0 else sB[l - 1]
            self.mixer_layer(l, src, ("B", l - 1), sA[l], ("A", l))
            if l == 0:
                self.zero_fill(zt, zbar)
            S.fence()
            self.release(base)
            last = (l == L - 1)
            self.moe_layer(l, sA[l], ("A", l), self.d_out if last else sB[l], ("B", l), last)
            S.fence()
            self.release(base)
        S.emit(final_ops=self.finals)
        return nc


    def zero_fill(self, zt, zbar):
        S = self.S
        for l in range(self.L):
            Xd_, Yd_ = self.Xd[l], self.Yd[l]
            S.add("pool", lambda e, Xd_=Xd_: e.dma_start(out=Xd_[0:1, :], in_=zt[0:1, :]), reads=["zt"], writes=[("Xdzp", l, -1)], dma=True)
            for b_ in range(NE * CAP // 128):
                S.add("pool", lambda e, Xd_=Xd_, b_=b_: e.dma_start(out=Xd_[1 + b_ * 128:1 + (b_ + 1) * 128, :], in_=zt[:]),
                      reads=["zt"], writes=[("Xdzp", l, b_)], dma=True)
            S.add("pool", lambda e, Yd_=Yd_: e.dma_start(out=Yd_[0:1, :], in_=zt[0:1, :]), reads=["zt"], writes=[("Ydz", l)], dma=True)
            S.add("pool", lambda e: e.memset(zbar[:], 0.0), reads=[("Xdzp", l, b_) for b_ in range(-1, NE * CAP // 128)], writes=[("Xdz", l), "zbar"])

    def E(self, eng, meth, reads, writes, **kw):
        return self.S.add(eng, lambda e: getattr(e, meth)(**kw), reads=reads, writes=writes)

    def nextq(self):
        self.qctr = (getattr(self, "qctr", -1) + 1) % 6
        return 2 + self.qctr

    def psq(self, i):
        return self.psum[i][:, 0:128]

    def mixer_layer(self, l, xsrc, srcres, xdst, dstres):
        S, E = self.S, self.E
        xc, cc = self.mix_cols, self.const_cols
        nxc = max(v[0] + v[1] for v in xc.values())
        self.xTb = self.sb_cached("xTb", [128, NCH, T], BF16)
        mp = self.sb([128, nxc], F32, "mixp")
        S.add("sp", lambda e: e.dma_start(out=mp[:], in_=self.d_mixp[l]), writes=["mp"], dma=True)
        win = self.sb([128, NCH, NWIN], BF16, "win")
        wout = self.sb([128, NCH, D], BF16, "wout")
        for c in range(NCH):
            S.add("pool", lambda e, c=c: e.dma_start(out=win[:, c, :], in_=self.d_win[l, c * 128:(c + 1) * 128, :]),
                  writes=["win"], dma=True)
        S.add("pool", lambda e: e.dma_start(out=wout[:], in_=self.d_wout[l].rearrange("(c p) f -> p c f", p=128)),
              writes=["wout"], dma=True)
        P = lambda name, lo=0, n=None: self.col(mp, xc, name, lo, n)
        C = lambda name, lo=0, n=None: self.col(self.cst, cc, name, lo, n)
        ident = C("ident")
        xTb = self.xTb
        TWO_PI = 2.0 * np.pi

        m0 = self.mark()
        stg = [self.sb([128, TG], F32, "xstg") for _ in range(2)]
        k = 0
        for c in range(NCH):
            for g in range(NTG):
                st = stg[k % 2]
                S.add("sp", lambda e, c=c, g=g, st=st: e.dma_start(out=st[:], in_=xsrc[c * 128:(c + 1) * 128, g * TG:(g + 1) * TG]),
                      reads=[("dram", srcres, c)], writes=[("xstg", k % 2)], dma=True)
                S.add("act", lambda e, c=c, g=g, st=st: e.activation(out=self.xTb[:, c, g * TG:(g + 1) * TG], in_=st[:], func=AF.Copy),
                      reads=[("xstg", k % 2)], writes=[("xTb", c, g)])
                k += 1
        if l == 0:
            cosT = self.sb([128, T], F32, "cosT")
            sinT = self.sb([128, T], F32, "sinT")
            posi = self.sb([128, T], I32, "posi")
            vf = self.sb([128, T], F32, "vf")
            ni = self.sb([128, T], I32, "ni")
            nf = self.sb([128, T], F32, "nf")
            mk = self.sb([128, T], F32, "mk")
            S.add("sp", lambda e: e.dma_start(out=posi[:], in_=self.d_posb), writes=["posi"], dma=True)
            for which, dst in ((0, sinT), (1, cosT)):
                E("dve", "tensor_copy", ["posi"], ["vf"], out=vf[:], in_=posi[:])
                E("dve", "tensor_scalar", ["vf", "cst"], ["vf"], out=vf[:], in0=vf[:], scalar1=C("invf")[:, 0:1],
                  scalar2=(0.25 if which else 0.0), op0=ALU.mult, op1=ALU.add)
                E("dve", "tensor_copy", ["vf"], ["ni"], out=ni[:], in_=vf[:])
                E("dve", "tensor_copy", ["ni"], ["nf"], out=nf[:], in_=ni[:])
                E("dve", "tensor_tensor", ["vf", "nf"], ["vf"], out=vf[:], in0=vf[:], in1=nf[:], op=ALU.subtract)
                E("dve", "tensor_scalar", ["vf"], ["mk"], out=mk[:], in0=vf[:], scalar1=0.5, scalar2=None, op0=ALU.is_gt)
                E("dve", "tensor_tensor", ["vf", "mk"], ["vf"], out=vf[:], in0=vf[:], in1=mk[:], op=ALU.subtract)
                E("dve", "tensor_scalar", ["vf"], ["mk"], out=mk[:], in0=vf[:], scalar1=-0.5, scalar2=None, op0=ALU.is_lt)
                E("dve", "tensor_tensor", ["vf", "mk"], ["vf"], out=vf[:], in0=vf[:], in1=mk[:], op=ALU.add)
                E("act", "activation", ["vf"], [("rot", which)], out=dst[:], in_=vf[:], func=AF.Sin, scale=TWO_PI)
            E("dve", "tensor_scalar", [("rot", 0), "cst"], [("rot", 0)], out=sinT[:], in0=sinT[:], scalar1=C("sgn")[:, 0:1],
              scalar2=None, op0=ALU.mult)
            S.add("sp", lambda e: e.dma_start(out=self.d_rot[0], in_=sinT[:]), reads=[("rot", 0)], writes=[("rotd", 0)], dma=True)
            S.add("sp", lambda e: e.dma_start(out=self.d_rot[1], in_=cosT[:]), reads=[("rot", 1)], writes=[("rotd", 1)], dma=True)
        S.fence()
        self.release(m0)
        rc = [self.sb([128, 2, 128], F32, "rc") for _ in range(2)]

        negA = self.sb([8, 1], F32, "negA")
        E("act", "activation", ["mp"], ["negA"], out=negA[:], in_=P("a_log")[0:8, 0:1], func=AF.Exp)
        E("dve", "tensor_scalar", ["negA"], ["negA"], out=negA[:], in0=negA[:], scalar1=-1.0, scalar2=None, op0=ALU.mult)
        nbgk = self.sb([64, 2], F32, "nbgk")
        E("dve", "tensor_scalar", ["mp"], ["nbgk"], out=nbgk[:], in0=P("bgk")[0:64, 0:2], scalar1=-1.0, scalar2=None, op0=ALU.mult)
        wgk = self.sb([16, 128], F32, "wgk")
        E("dve", "tensor_copy", ["mp"], ["wgk"], out=wgk[:], in_=P("w_gk2")[0:16, 0:128])

        Sret = self.sb([128, 2, 64], F32, "Sret"); Sretb = self.sb([128, 2, 64], BF16, "Sretb")
        Sssd = self.sb([128, 4, 64], F32, "Sssd"); Sssdb = self.sb([128, 4, 64], BF16, "Sssdb")
        Sgla = self.sb([64, 2, 64], F32, "Sgla"); Sglab = self.sb([64, 2, 64], BF16, "Sglab")
        for t_, nm in ((Sret, "Sret"), (Sretb, "Sretb"), (Sssd, "Sssd"), (Sssdb, "Sssdb"), (Sgla, "Sgla"), (Sglab, "Sglab")):
            for h in range(t_.shape[1]):
                wres = [(nm, 2 * h), (nm, 2 * h + 1)] if nm in ("Sret", "Sretb", "Sgla", "Sglab") else ([(nm, h), (nm, h + 4)] if nm in ("Sssd", "Sssdb") else [(nm, h)])
                E("dve", "memset", [], wres, ap=t_[:, h, :], constant=0.0)
        raw = self.sb([128, 6, 131], F32, "raw")
        E("dve", "memset", [], [("raw", r) for r in range(6)], ap=raw[:], constant=0.0)

        tmv = self.sb([128, 256], BF16, "tm_rv")
        tmg = self.sb([128, 256], F32, "tm_rg")
        tmz = self.sb([128, 512], F32, "tm_sz")
        tgv = self.sb([128, 256], BF16, "tm_gv")
        tgg = self.sb([128, 256], F32, "tm_gg")
        def per(nh, shape, dt, nm):
            return [self.sb(shape, dt, nm) for _ in range(nh)]
        t1_s = per(2, [128, 128], F32, "t1"); t2_s = per(2, [128, 128], F32, "t2")
        rq_s = per(2, [128, 128], BF16, "rq"); rqi_s = per(2, [128, 128], BF16, "rqi")
        rkb_s = per(2, [128, 128], BF16, "rkb")
        kst_s = per(2, [128, 128], BF16, "kst")
        PTr_s = per(4, [128, 128], BF16, "PTr"); PTs_s = per(1, [128, 128], BF16, "PTs"); PTg_s = per(2, [128, 128], BF16, "PTg")
        cacc = self.sb([128, 128], F32, "cacc")
        xsT = self.sb([128, 4, 128], F32, "xsT")
        BT = self.sb([128, 128], F32, "BT"); BTb = self.sb([128, 128], BF16, "BTb")
        CT = self.sb([128, 128], F32, "CT")
        xs_tok = self.sb([128, 8, 64], F32, "xs_tok")
        B_tok = self.sb([128, 2, 64], BF16, "B_tok")
        dtT = self.sb([8, 128], F32, "dtT"); aT = self.sb([8, 128], F32, "aT"); acT = self.sb([8, 128], F32, "acT")
        dt_tok = self.sb([128, 8], F32, "dt_tok"); ac_tok = self.sb([128, 8], F32, "ac_tok")
        Xm4_s = per(2, [128, 4, 128], F32, "Xm4"); PT4_s = per(2, [128, 4, 128], BF16, "PT4")
        EB4_s = per(2, [128, 4, 128], F32, "EB4"); Ct4_s = per(2, [128, 4, 128], BF16, "Ct4")
        wc4_s = per(2, [128, 4], F32, "wc4"); vh4_s = per(2, [128, 4, 64], BF16, "vh4"); vst4_s = per(2, [128, 4, 64], BF16, "vst4")
        rhsb_s = per(2, [8, 4, 128], F32, "rhsb")
        Xm_s = per(1, [128, 128], F32, "Xm")
        EB_s = per(1, [128, 128], F32, "EB")
        Ct_s = per(1, [128, 128], BF16, "Ct")
        Ctb = self.sb([128, 128], BF16, "Ctb")
        wcol_s = per(1, [128, 1], F32, "wcol")
        vh_s = per(1, [128, 64], BF16, "vh"); vst_s = per(1, [128, 64], BF16, "vst")
        gkl = self.sb([16, 128], F32, "gkl")
        la_s = per(2, [64, 128], F32, "la"); bb_s = per(2, [64, 128], F32, "bb")
        eb_s = per(2, [64, 128], F32, "eb"); enb_s = per(2, [64, 128], F32, "enb"); ek_s = per(2, [64, 128], F32, "ek")
        gq_s = per(2, [64, 128], BF16, "gq"); gk__s = per(2, [64, 128], BF16, "gk_"); gkf_s = per(2, [64, 128], F32, "gkf")
        gkh_s = per(2, [64, 128], F32, "gkh"); gkst_s = per(2, [128, 64], BF16, "gkst")
        h_tok = self.sb([128, D], F32, "h_tok")
        hT = self.sb([128, NCH, 128], BF16, "hTm")
        sq = self.sb([128, 512], F32, "sq")
        st1 = self.sb([128, 8], F32, "st1"); st2 = self.sb([128, 8], F32, "st2"); st3 = self.sb([128, 8], F32, "st3")
        yz = self.sb([128, 512], F32, "yz")
        zc = self.sb([128, NCH, 128], F32, "zc")
        zsq = self.sb([128, NCH, 128], F32, "zsq")
        lm2 = self.sb([128, 128], F32, "lm2"); lrs = self.sb([128, 128], F32, "lrs"); lta = self.sb([128, 128], F32, "lta")
        ones_row = C("ones")
        self.cbT = [self.sb([128, 128], F32, "cbT0"), self.sb([128, 128], F32, "cbT1")]

        GAM = [1.0 - 2.0 ** (-5.0 - h) for h in range(4)]
        po = [self.psum[0], self.psum[1]]

        def o_ap(lo, n):
            b_, l_ = divmod(lo, 512)
            assert l_ + n <= 512
            return po[b_][:, l_:l_ + n]

        def proj_fm(n, off, M):
            qi = self.nextq()
            ps = self.psq(qi)
            for c in range(NCH):
                E("pe", "matmul", ["win", ("xTb", c, n // 4)], [("ps", qi)], out=ps[0:M, :], lhsT=win[:, c, off:off + M],
                  rhs=xTb[:, c, n * 128:(n + 1) * 128], start=(c == 0), stop=(c == NCH - 1))
            return qi, ps[0:M, :]

        def proj_tm(n, bank, groups):
            for (off, w, dst) in groups:
                for c in range(NCH):
                    E("pe", "matmul", ["win", ("xTb", c, n // 4)], [("ps", bank)], out=self.psum[bank][:, dst:dst + w],
                      lhsT=xTb[:, c, n * 128:(n + 1) * 128], rhs=win[:, c, off:off + w], start=(c == 0), stop=(c == NCH - 1))

        import os
        for n in range(int(os.environ.get('MIX_CHUNKS', T // 128))):
            tsl = slice(n * 128, (n + 1) * 128)
            proj_tm(n, 2, [(512, 512, 0)])
            E("act", "activation", [("ps", 2)], ["tmv"], out=tmv[:], in_=self.psum[2][:, 0:256], func=AF.Copy)
            E("act", "activation", [("ps", 2)], ["tmg"], out=tmg[:], in_=self.psum[2][:, 256:512], func=AF.Silu)
            proj_tm(n, 3, [(1024, 512, 0)])
            E("act", "activation", [("ps", 3)], ["tmz"], out=tmz[:], in_=self.psum[3][:], func=AF.Silu)
            proj_tm(n, 4, [(2568, 256, 0), (2840, 256, 256)])
            E("act", "activation", [("ps", 4)], ["tgv"], out=tgv[:], in_=self.psum[4][:, 0:256], func=AF.Copy)
            E("act", "activation", [("ps", 4)], ["tgg"], out=tgg[:], in_=self.psum[4][:, 256:512], func=AF.Silu)

            rcn = rc[n % 2]
            S.add("sp", lambda e, rcn=rcn, tsl=tsl: e.dma_start(out=rcn[:], in_=self.d_rot[:, :, tsl].rearrange("w p t -> p w t")),
                  reads=[("rotd", 0), ("rotd", 1)], writes=[("rc", n % 2)], dma=True)
            def ret_stage(p2):
                t1, t2, rq, rqi, rkb, kst = t1_s[p2], t2_s[p2], rq_s[p2], rqi_s[p2], rkb_s[p2], kst_s[p2]
                for kind, offa, offb in (("q", 128 * p2, 3096 + 128 * p2), ("k", 256 + 128 * p2, 3096 + 256 + 128 * p2)):
                    qa, pa = proj_fm(n, offa, 128)
                    qb, pb = proj_fm(n, offb, 128)
                    E("dve", "tensor_tensor", [("ps", qa), ("rc", n % 2)], [("t1", p2)], out=t1[:], in0=pa, in1=rcn[:, 1, :], op=ALU.mult)
                    E("dve", "tensor_tensor", [("ps", qb), ("rc", n % 2)], [("t2", p2)], out=t2[:], in0=pb, in1=rcn[:, 0, :], op=ALU.mult)
                    if kind == "k":
                        E("dve", "tensor_tensor", [("t1", p2), ("t2", p2)], [("rkb", p2)], out=rkb[:], in0=t1[:], in1=t2[:], op=ALU.add)
                    else:
                        E("dve", "tensor_tensor", [("t1", p2), ("t2", p2)], [("rq", p2)], out=rq[:], in0=t1[:], in1=t2[:], op=ALU.add)
                        E("dve", "tensor_tensor", [("t1", p2), ("t2", p2)], [("t1", p2)], out=t1[:], in0=t1[:], in1=t2[:], op=ALU.add)
                        E("dve", "tensor_tensor", [("t1", p2), "cst"], [("rqi", p2)], out=rqi[:], in0=t1[:], in1=C("gq2")[:, p2 * 128:(p2 + 1) * 128], op=ALU.mult)
                qt = self.nextq()
                pbf = self.psum[qt][:].bitcast(BF16)
                E("pe", "transpose", [("rkb", p2), "identb"], [("ps", qt)], out=pbf[:, 0:128], in_=rkb[:], identity=self.identb[:])
                for j in range(2):
                    h = 2 * p2 + j
                    E("dve", "tensor_scalar", [("ps", qt), "cst"], [("kst", p2, j)], out=kst[:, 64 * j:64 * j + 64], in0=pbf[:, 64 * j:64 * j + 64],
                      scalar1=C("gk")[:, h:h + 1], scalar2=None, op0=ALU.mult)
                def stage_b():
                    for j in range(2):
                        h = 2 * p2 + j
                        hs = slice(64 * j, 64 * j + 64)
                        PT = PTr_s[h]
                        qs = self.nextq(); pss = self.psq(qs)
                        E("pe", "matmul", [("rkb", p2), ("rq", p2)], [("ps", qs)], out=pss, lhsT=rkb[hs, :], rhs=rq[hs, :], start=True, stop=True)
                        E("dve", "tensor_tensor", [("ps", qs), "cst"], [("PTr", h)], out=PT[:], in0=pss, in1=C("retmask")[:, h * 128:(h + 1) * 128], op=ALU.mult)
                        E("pe", "matmul", [("PTr", h), "tmv"], [("ps", 0)], out=o_ap(64 * h, 64), lhsT=PT[:], rhs=tmv[:, 64 * h:64 * h + 64], start=True, stop=False)
                        E("pe", "matmul", [("rqi", p2), ("Sretb", h)], [("ps", 0)], out=o_ap(64 * h, 64), lhsT=rqi[hs, :], rhs=Sretb[hs, p2, :], start=False, stop=True)
                    qu = self.nextq(); psu = self.psq(qu)
                    E("pe", "matmul", [("kst", p2, 0), ("kst", p2, 1), "tmv"], [("ps", qu)], out=psu, lhsT=kst[:], rhs=tmv[:, 128 * p2:128 * p2 + 128], start=True, stop=True)
                    for j in range(2):
                        h = 2 * p2 + j
                        hs = slice(64 * j, 64 * j + 64)
                        E("dve", "scalar_tensor_tensor", [("Sret", h), ("ps", qu)], [("Sret", h)], out=Sret[hs, p2, :], in0=Sret[hs, p2, :],
                          scalar=float(GAM[h] ** 128), in1=psu[hs, 64 * j:64 * j + 64], op0=ALU.mult, op1=ALU.add)
                        E("act", "activation", [("Sret", h)], [("Sretb", h)], out=Sretb[hs, p2, :], in_=Sret[hs, p2, :], func=AF.Copy)
                return stage_b

            pend = [ret_stage(0), ret_stage(1)]
            while pend:
                pend.pop(0)()

            for r in range(6):
                qx, px = proj_fm(n, 1536 + 128 * r, 128)
                E("dve", "tensor_copy", [("raw", r)], [("raw", r)], out=raw[:, r, 0:3], in_=raw[:, r, 128:131])
                E("act", "activation", [("ps", qx)], [("raw", r)], out=raw[:, r, 3:131], in_=px, func=AF.Copy)
                cw = P("conv_w")
                E("dve", "tensor_scalar", [("raw", r), "mp"], ["cacc"], out=cacc[:], in0=raw[:, r, 0:128], scalar1=cw[:, 4 * r:4 * r + 1], scalar2=None, op0=ALU.mult)
                for j in range(1, 4):
                    E("dve", "scalar_tensor_tensor", [("raw", r), "mp", "cacc"], ["cacc"], out=cacc[:], in0=raw[:, r, j:j + 128],
                      scalar=cw[:, 4 * r + j:4 * r + j + 1], in1=cacc[:], op0=ALU.mult, op1=ALU.add)
                dst = xsT[:, r, :] if r < 4 else (BT[:] if r == 4 else CT[:])
                dres = ("xsT", r) if r < 4 else ("BT" if r == 4 else "CT")
                E("act", "activation", ["cacc", "mp"], [dres], out=dst, in_=cacc[:], func=AF.Silu, bias=P("conv_b")[:, r:r + 1], scale=1.0)
            E("act", "activation", ["BT"], ["BTb"], out=BTb[:], in_=BT[:], func=AF.Copy)
            E("act", "activation", ["CT"], ["Ctb"], out=Ctb[:], in_=CT[:], func=AF.Copy)
            for r in range(5):
                qt = self.nextq(); pst = self.psq(qt)
                tsrc = xsT[:, r, :] if r < 4 else BT[:]
                sres = ("xsT", r) if r < 4 else "BT"
                E("pe", "transpose", [sres, "cst"], [("ps", qt)], out=pst, in_=tsrc, identity=ident)
                if r < 4:
                    E("act", "activation", [("ps", qt)], [("xs_tok", 2 * r), ("xs_tok", 2 * r + 1)], out=xs_tok[:, 2 * r:2 * r + 2, :],
                      in_=pst.rearrange("p (h d) -> p h d", h=2), func=AF.Copy)
                else:
                    E("act", "activation", [("ps", qt)], [("B_tok", 0), ("B_tok", 1)], out=B_tok[:], in_=pst.rearrange("p (h d) -> p h d", h=2), func=AF.Copy)
            qd, pd = proj_fm(n, 2304, 8)
            E("act", "activation", [("ps", qd), "mp"], ["dtT"], out=dtT[:], in_=pd, func=AF.Exp, bias=P("dt_bias")[0:8, 0:1], scale=1.0)
            E("act", "activation", ["dtT"], ["dtT"], out=dtT[:], in_=dtT[:], func=AF.Ln, bias=1.0, scale=1.0)
            E("dve", "tensor_scalar", ["dtT", "negA"], ["aT"], out=aT[:], in0=dtT[:], scalar1=negA[:, 0:1], scalar2=None, op0=ALU.mult)
            E("dve", "tensor_tensor_scan", ["aT", "cst"], ["acT"], out=acT[:], data0=ones_row[0:8, 0:128], data1=aT[:], initial=0.0, op0=ALU.mult, op1=ALU.add)
            for (srcT, dstt, nm) in ((dtT, dt_tok, "dt_tok"), (acT, ac_tok, "ac_tok")):
                qt = self.nextq(); pst = self.psq(qt)
                E("pe", "transpose", ["dtT" if srcT is dtT else "acT", "cst"], [("ps", qt)], out=pst[:, 0:8], in_=srcT[:], identity=ident[0:8, 0:8])
                E("act", "activation", [("ps", qt)], [nm], out=dstt[:], in_=pst[:, 0:8], func=AF.Copy)
            for g in range(2):
                gs = slice(64 * g, 64 * g + 64)
                qc = self.nextq(); pc = self.psq(qc)
                E("pe", "matmul", ["BTb", "Ctb"], [("ps", qc)], out=pc, lhsT=BTb[gs, :], rhs=Ctb[gs, :], start=True, stop=True)
                cbT = self.cbT[g]
                E("act", "activation", [("ps", qc)], [("cbT", g)], out=cbT[:], in_=pc, func=AF.Copy)
            def ssd_stage(g):
                gs = slice(64 * g, 64 * g + 64)
                Xm4, PT4, EB4, Ct4, wc4, vh4, vst4, rhsb = Xm4_s[g], PT4_s[g], EB4_s[g], Ct4_s[g], wc4_s[g], vh4_s[g], vst4_s[g], rhsb_s[g]
                acg = ac_tok[:, 4 * g:4 * g + 4].rearrange("p (j o) -> p j o", o=1)
                E("dve", "tensor_tensor", ["acT", "cst"], [("rhsb", g)], out=rhsb[:], in0=acT[:].rearrange("k (o c) -> k o c", o=1).to_broadcast([8, 4, 128]),
                  in1=C("selm")[0:8, g * 512:(g + 1) * 512].rearrange("k (j c) -> k j c", j=4), op=ALU.mult)
                qa = self.nextq()
                pab = self.psum[qa][:].rearrange("p (j c) -> p j c", j=4)
                E("pe", "matmul", ["cst", ("rhsb", g)], [("ps", qa)], out=self.psum[qa][:], lhsT=ones_row[0:8, 0:128], rhs=rhsb[:].rearrange("k j c -> k (j c)"), start=True, stop=True)
                E("dve", "tensor_tensor", [("ps", qa), "ac_tok"], [("Xm4", g)], out=Xm4[:], in0=pab, in1=acg.to_broadcast([128, 4, 128]), op=ALU.subtract)
                E("dve", "tensor_scalar", [("Xm4", g)], [("Xm4", g)], out=Xm4[:], in0=Xm4[:], scalar1=0.0, scalar2=None, op0=ALU.min)
                E("act", "activation", [("Xm4", g)], [("Xm4", g)], out=Xm4[:], in_=Xm4[:], func=AF.Exp)
                E("dve", "tensor_tensor", [("Xm4", g), "cst"], [("Xm4", g)], out=Xm4[:], in0=Xm4[:],
                  in1=C("causal").rearrange("p (o c) -> p o c", o=1).to_broadcast([128, 4, 128]), op=ALU.mult)
                E("dve", "tensor_tensor", [("Xm4", g), ("cbT", g)], [("PT4", g)], out=PT4[:], in0=Xm4[:],
                  in1=self.cbT[g][:].rearrange("p (o c) -> p o c", o=1).to_broadcast([128, 4, 128]), op=ALU.mult)
                E("act", "activation", [("ps", qa)], [("EB4", g)], out=EB4[gs, :, :], in_=pab[gs, :, :], func=AF.Exp)
                E("dve", "tensor_tensor", ["CT", ("EB4", g)], [("Ct4", g)], out=Ct4[gs, :, :],
                  in0=CT[gs, :].rearrange("p (o c) -> p o c", o=1).to_broadcast([64, 4, 128]), in1=EB4[gs, :, :], op=ALU.mult)
                E("dve", "tensor_tensor", [("ps", qa), "ac_tok"], [("wc4", g)], out=wc4[:].rearrange("p (j o) -> p j o", o=1), in0=pab[:, :, 127:128], in1=acg, op=ALU.subtract)
                E("act", "activation", [("wc4", g)], [("wc4", g)], out=wc4[:], in_=wc4[:], func=AF.Exp)
                E("dve", "tensor_tensor", [("xs_tok", 4 * g + j) for j in range(4)] + ["dt_tok"], [("vh4", g)], out=vh4[:], in0=xs_tok[:, 4 * g:4 * g + 4, :],
                  in1=dt_tok[:, 4 * g:4 * g + 4].rearrange("p (j o) -> p j o", o=1).to_broadcast([128, 4, 64]), op=ALU.mult)
                E("dve", "tensor_tensor", [("vh4", g), ("wc4", g)], [("vst4", g)], out=vst4[:], in0=vh4[:],
                  in1=wc4[:].rearrange("p (j o) -> p j o", o=1).to_broadcast([128, 4, 64]), op=ALU.mult)
                def stage_b():
                    for j in range(4):
                        h = 4 * g + j
                        E("pe", "matmul", [("PT4", g), ("vh4", g)], [("ps", 1 if h >= 4 else 0)], out=o_ap(256 + 64 * h, 64), lhsT=PT4[:, j, :], rhs=vh4[:, j, :], start=True, stop=False)
                        E("pe", "matmul", [("Ct4", g), ("Sssdb", h)], [("ps", 1 if h >= 4 else 0)], out=o_ap(256 + 64 * h, 64), lhsT=Ct4[gs, j, :], rhs=Sssdb[gs, j, :], start=False, stop=True)
                        qu = self.nextq(); psu = self.psq(qu)
                        E("pe", "matmul", [("B_tok", 0), ("B_tok", 1), ("vst4", g)], [("ps", qu)], out=psu[:, 0:64], lhsT=B_tok[:].rearrange("p g k -> p (g k)"), rhs=vst4[:, j, :], start=True, stop=True)
                        E("dve", "scalar_tensor_tensor", [("Sssd", h), ("ps", qu), ("EB4", g)], [("Sssd", h)], out=Sssd[gs, j, :], in0=Sssd[gs, j, :],
                          scalar=EB4[gs, j, 127:128], in1=psu[gs, 0:64], op0=ALU.mult, op1=ALU.add)
                        E("act", "activation", [("Sssd", h)], [("Sssdb", h)], out=Sssdb[gs, j, :], in_=Sssd[gs, j, :], func=AF.Copy)
                return stage_b

            pend = [ssd_stage(0), ssd_stage(1)]
            while pend:
                pend.pop(0)()

            qg, pg = proj_fm(n, 2824, 16)
            E("act", "activation", [("ps", qg)], ["gkl"], out=gkl[:], in_=pg, func=AF.Copy)
            def gla_stage(p2):
                la, bb, eb, enb, ek, gq, gk_, gkf, gkh, gkst = la_s[p2], bb_s[p2], eb_s[p2], enb_s[p2], ek_s[p2], gq_s[p2], gk__s[p2], gkf_s[p2], gkh_s[p2], gkst_s[p2]
                qk, pk = proj_fm(n, 2440 + 64 * p2, 64)
                E("act", "activation", [("ps", qk)], [("gkf", p2)], out=gkf[:], in_=pk, func=AF.Copy)
                qq, pq = proj_fm(n, 2312 + 64 * p2, 64)
                ql = self.nextq(); pl = self.psq(ql)
                E("pe", "matmul", ["wgk", "gkl"], [("ps", ql)], out=pl[0:64, :], lhsT=wgk[:, 64 * p2:64 * p2 + 64], rhs=gkl[:], start=True, stop=True)
                E("act", "activation", [("ps", ql), "nbgk"], [("la", p2)], out=la[:], in_=pl[0:64, :], func=AF.Exp, bias=nbgk[:, p2:p2 + 1], scale=-1.0)
                E("act", "activation", [("la", p2)], [("la", p2)], out=la[:], in_=la[:], func=AF.Ln, bias=1.0, scale=1.0)
                E("dve", "tensor_scalar", [("la", p2)], [("la", p2)], out=la[:], in0=la[:], scalar1=-1.0 / 16.0, scalar2=None, op0=ALU.mult)
                E("dve", "tensor_tensor_scan", [("la", p2), "cst"], [("bb", p2)], out=bb[:], data0=ones_row[0:64, 0:128], data1=la[:], initial=0.0, op0=ALU.mult, op1=ALU.add)
                E("act", "activation", [("bb", p2)], [("eb", p2)], out=eb[:], in_=bb[:], func=AF.Exp)
                E("act", "activation", [("bb", p2)], [("enb", p2)], out=enb[:], in_=bb[:], func=AF.Exp, scale=-1.0)
                E("act", "activation", [("bb", p2)], [("ek", p2)], out=ek[:], in_=bb[:], func=AF.Exp, scale=-1.0, bias=bb[:, 127:128])
                E("dve", "scalar_tensor_tensor", [("ps", qq), ("eb", p2)], [("gq", p2)], out=gq[:], in0=pq, scalar=float(32 ** -0.5), in1=eb[:], op0=ALU.mult, op1=ALU.mult)
                E("dve", "tensor_tensor", [("gkf", p2), ("enb", p2)], [("gk_", p2)], out=gk_[:], in0=gkf[:], in1=enb[:], op=ALU.mult)
                E("dve", "tensor_tensor", [("gkf", p2), ("ek", p2)], [("gkh", p2)], out=gkh[:], in0=gkf[:], in1=ek[:], op=ALU.mult)
                def stage_b():
                    for j in range(2):
                        h = 2 * p2 + j
                        hs = slice(32 * j, 32 * j + 32)
                        PT = PTg_s[j]
                        qs = self.nextq(); pss = self.psq(qs)
                        E("pe", "matmul", [("gk_", p2), ("gq", p2)], [("ps", qs)], out=pss, lhsT=gk_[hs, :], rhs=gq[hs, :], start=True, stop=True)
                        E("dve", "tensor_tensor", [("ps", qs), "cst"], [("PTg", j)], out=PT[:], in0=pss, in1=C("causal"), op=ALU.mult)
                        E("pe", "matmul", [("PTg", j), "tgv"], [("ps", 1)], out=o_ap(768 + 64 * h, 64), lhsT=PT[:], rhs=tgv[:, 64 * h:64 * h + 64], start=True, stop=False)
                        E("pe", "matmul", [("gq", p2), ("Sglab", h)], [("ps", 1)], out=o_ap(768 + 64 * h, 64), lhsT=gq[hs, :], rhs=Sglab[hs, p2, :], start=False, stop=True)
                    qt = self.nextq(); pst = self.psq(qt)
                    E("pe", "transpose", [("gkh", p2), "cst"], [("ps", qt)], out=pst[:, 0:64], in_=gkh[:], identity=ident[0:64, 0:64])
                    E("act", "activation", [("ps", qt)], [("gkst", p2)], out=gkst[:], in_=pst[:, 0:64], func=AF.Copy)
                    qu = self.nextq(); psu = self.psq(qu)
                    E("pe", "matmul", [("gkst", p2), "tgv"], [("ps", qu)], out=psu[0:64, 0:128], lhsT=gkst[:], rhs=tgv[:, 128 * p2:128 * p2 + 128], start=True, stop=True)
                    for j in range(2):
                        h = 2 * p2 + j
                        hs = slice(32 * j, 32 * j + 32)
                        E("dve", "scalar_tensor_tensor", [("Sgla", h), ("ps", qu), ("eb", p2)], [("Sgla", h)], out=Sgla[hs, p2, :], in0=Sgla[hs, p2, :],
                          scalar=eb[hs, 127:128], in1=psu[hs, 64 * j:64 * j + 64], op0=ALU.mult, op1=ALU.add)
                    E("act", "activation", [("Sgla", 2 * p2), ("Sgla", 2 * p2 + 1)], [("Sglab", 2 * p2), ("Sglab", 2 * p2 + 1)], out=Sglab[:, p2, :], in_=Sgla[:, p2, :], func=AF.Copy)
                return stage_b

            pend = [gla_stage(0), gla_stage(1)]
            while pend:
                pend.pop(0)()

            oret = po[0][:, 0:256]
            E("dve", "tensor_reduce", [("ps", 0)], ["st1"], out=st1[:, 0:4], in_=oret.rearrange("p (h d) -> p h d", h=4), axis=AX.X, op=ALU.add)
            E("act", "activation", [("ps", 0)], ["sq"], out=sq[:, 0:256], in_=oret, func=AF.Square)
            E("dve", "tensor_reduce", ["sq"], ["st2"], out=st2[:, 0:4], in_=sq[:, 0:256].rearrange("p (h d) -> p h d", h=4), axis=AX.X, op=ALU.add)
            E("dve", "tensor_scalar", ["st1"], ["st1"], out=st1[:, 0:4], in0=st1[:, 0:4], scalar1=1.0 / 64, scalar2=None, op0=ALU.mult)
            E("dve", "tensor_tensor", ["st1"], ["st3"], out=st3[:, 0:4], in0=st1[:, 0:4], in1=st1[:, 0:4], op=ALU.mult)
            E("dve", "scalar_tensor_tensor", ["st2", "st3"], ["st2"], out=st2[:, 0:4], in0=st2[:, 0:4], scalar=1.0 / 64, in1=st3[:, 0:4], op0=ALU.mult, op1=ALU.subtract)
            E("dve", "tensor_scalar", ["st2"], ["st2"], out=st2[:, 0:4], in0=st2[:, 0:4], scalar1=LN_EPS, scalar2=None, op0=ALU.add)
            E("act", "activation", ["st2"], ["st2"], out=st2[:, 0:4], in_=st2[:, 0:4], func=AF.Sqrt)
            E("dve", "reciprocal", ["st2"], ["st2"], out=st2[:, 0:4], in_=st2[:, 0:4])
            hr = h_tok[:, 0:256].rearrange("p (h d) -> p h d", h=4)
            E("dve", "tensor_tensor", [("ps", 0), "st1"], ["h_ret"], out=hr, in0=oret.rearrange("p (h d) -> p h d", h=4),
              in1=st1[:, 0:4].to_broadcast([128, 4, 64]) if False else st1[:, 0:4].rearrange("p (h o) -> p h o", o=1).to_broadcast([128, 4, 64]), op=ALU.subtract)
            E("dve", "tensor_tensor", ["h_ret", "st2"], ["h_ret"], out=hr, in0=hr, in1=st2[:, 0:4].rearrange("p (h o) -> p h o", o=1).to_broadcast([128, 4, 64]), op=ALU.mult)
            E("dve", "tensor_tensor", ["h_ret", "mp"], ["h_ret"], out=h_tok[:, 0:256], in0=h_tok[:, 0:256], in1=P("ret_nw"), op=ALU.mult)
            E("dve", "tensor_tensor", ["h_ret", "tmg"], ["h_ret"], out=h_tok[:, 0:256], in0=h_tok[:, 0:256], in1=tmg[:], op=ALU.mult)
            xs3 = xs_tok[:]
            E("dve", "tensor_tensor", [("xs_tok", r) for r in range(8)] + ["mp"], ["yz"], out=yz[:].rearrange("p (h d) -> p h d", h=8), in0=xs3,
              in1=P("ssd_d").rearrange("p (h o) -> p h o", o=1).to_broadcast([128, 8, 64]), op=ALU.mult)
            E("dve", "tensor_tensor", ["yz", ("ps", 0)], ["yz"], out=yz[:, 0:256], in0=yz[:, 0:256], in1=po[0][:, 256:512], op=ALU.add)
            E("dve", "tensor_tensor", ["yz", ("ps", 1)], ["yz"], out=yz[:, 256:512], in0=yz[:, 256:512], in1=po[1][:, 0:256], op=ALU.add)
            E("dve", "tensor_tensor", ["yz", "tmz"], ["yz"], out=yz[:], in0=yz[:], in1=tmz[:], op=ALU.mult)
            E("act", "activation", ["yz"], ["sq"], out=sq[:], in_=yz[:], func=AF.Square)
            E("dve", "tensor_reduce", ["sq"], ["st2"], out=st2[:, 0:2], in_=sq[:].rearrange("p (g d) -> p g d", g=2), axis=AX.X, op=ALU.add)
            E("dve", "tensor_scalar", ["st2"], ["st2"], out=st2[:, 0:2], in0=st2[:, 0:2], scalar1=1.0 / 256, scalar2=NORM_EPS, op0=ALU.mult, op1=ALU.add)
            E("act", "activation", ["st2"], ["st2"], out=st2[:, 0:2], in_=st2[:, 0:2], func=AF.Sqrt)
            E("dve", "reciprocal", ["st2"], ["st2"], out=st2[:, 0:2], in_=st2[:, 0:2])
            hs = h_tok[:, 256:768].rearrange("p (g d) -> p g d", g=2)
            E("dve", "tensor_tensor", ["yz", "st2"], ["h_ssd"], out=hs, in0=yz[:].rearrange("p (g d) -> p g d", g=2),
              in1=st2[:, 0:2].rearrange("p (g o) -> p g o", o=1).to_broadcast([128, 2, 256]), op=ALU.mult)
            E("dve", "tensor_tensor", ["h_ssd", "mp"], ["h_ssd"], out=h_tok[:, 256:768], in0=h_tok[:, 256:768], in1=P("ssd_nw"), op=ALU.mult)
            ogl = po[1][:, 256:512]
            E("act", "activation", [("ps", 1)], ["sq"], out=sq[:, 0:256], in_=ogl, func=AF.Square)
            E("dve", "tensor_reduce", ["sq"], ["st2"], out=st2[:, 0:4], in_=sq[:, 0:256].rearrange("p (h d) -> p h d", h=4), axis=AX.X, op=ALU.add)
            E("dve", "tensor_scalar", ["st2"], ["st2"], out=st2[:, 0:4], in0=st2[:, 0:4], scalar1=1.0 / 64, scalar2=NORM_EPS, op0=ALU.mult, op1=ALU.add)
            E("act", "activation", ["st2"], ["st2"], out=st2[:, 0:4], in_=st2[:, 0:4], func=AF.Sqrt)
            E("dve", "reciprocal", ["st2"], ["st2"], out=st2[:, 0:4], in_=st2[:, 0:4])
            hg = h_tok[:, 768:1024].rearrange("p (h d) -> p h d", h=4)
            E("dve", "tensor_tensor", [("ps", 1), "st2"], ["h_gla"], out=hg, in0=ogl.rearrange("p (h d) -> p h d", h=4),
              in1=st2[:, 0:4].rearrange("p (h o) -> p h o", o=1).to_broadcast([128, 4, 64]), op=ALU.mult)
            E("dve", "tensor_tensor", ["h_gla", "mp"], ["h_gla"], out=h_tok[:, 768:1024], in0=h_tok[:, 768:1024], in1=P("gla_nw"), op=ALU.mult)
            E("dve", "tensor_tensor", ["h_gla", "tgg"], ["h_gla"], out=h_tok[:, 768:1024], in0=h_tok[:, 768:1024], in1=tgg[:], op=ALU.mult)

            for ec in range(NCH):
                hres = "h_ret" if ec < 2 else ("h_ssd" if ec < 6 else "h_gla")
                qt = self.nextq(); pst = self.psq(qt)
                E("pe", "transpose", [hres, "cst"], [("ps", qt)], out=pst, in_=h_tok[:, ec * 128:(ec + 1) * 128], identity=ident)
                E("act", "activation", [("ps", qt)], [("hT", ec)], out=hT[:, ec, :], in_=pst, func=AF.Copy)
            S.add("sp", lambda e, tsl=tsl: e.dma_start(out=zc[:], in_=xsrc[:, tsl].rearrange("(c p) t -> p c t", p=128)),
                  reads=[("dram", srcres, c) for c in range(NCH)], writes=[("zc", c) for c in range(NCH)], dma=True)
            for half, (mt, mres) in enumerate(((sq, "sq"), (yz, "yz"))):
                bk = 2 + half
                pmt = self.psum[bk]
                for ec in range(NCH):
                    E("pe", "matmul", ["wout", ("hT", ec)], [("ps", bk)], out=pmt[:], lhsT=hT[:, ec, :], rhs=wout[:, ec, half * 512:(half + 1) * 512],
                      start=(ec == 0), stop=(ec == NCH - 1))
                E("act", "activation", [("ps", bk)], [mres], out=mt[:], in_=pmt[:], func=AF.Copy)
            for dc in range(NCH):
                mt, mres = (sq, "sq") if dc < 4 else (yz, "yz")
                qm = self.nextq(); pm = self.psq(qm)
                E("pe", "transpose", [mres, "cst"], [("ps", qm)], out=pm, in_=mt[:, (dc % 4) * 128:(dc % 4 + 1) * 128], identity=ident)
                E("dve", "scalar_tensor_tensor", [("zc", dc), ("ps", qm)], [("zc", dc)], out=zc[:, dc, :], in0=zc[:, dc, :], scalar=ALPHA, in1=pm, op0=ALU.mult, op1=ALU.add)
            onesm = C("onesm")
            qm_ = self.nextq(); pmn = self.psq(qm_)
            qq_ = self.nextq(); pqq = self.psq(qq_)
            for c in range(NCH):
                E("act", "activation", [("zc", c)], [("zsq", c)], out=zsq[:, c, :], in_=zc[:, c, :], func=AF.Square)
            for c in range(NCH):
                E("pe", "matmul", [("zc", c), "cst"], [("ps", qm_)], out=pmn, lhsT=onesm, rhs=zc[:, c, :], start=(c == 0), stop=(c == NCH - 1))
            for c in range(NCH):
                E("pe", "matmul", [("zsq", c), "cst"], [("ps", qq_)], out=pqq, lhsT=onesm, rhs=zsq[:, c, :], start=(c == 0), stop=(c == NCH - 1))
            E("act", "activation", [("ps", qm_)], ["lm2"], out=lm2[:], in_=pmn, func=AF.Square)
            E("dve", "tensor_tensor", [("ps", qq_), "lm2"], ["lrs"], out=lrs[:], in0=pqq, in1=lm2[:], op=ALU.subtract)
            E("dve", "tensor_scalar", ["lrs"], ["lrs"], out=lrs[:], in0=lrs[:], scalar1=LN_EPS, scalar2=None, op0=ALU.add)
            E("act", "activation", ["lrs"], ["lrs"], out=lrs[:], in_=lrs[:], func=AF.Sqrt)
            E("dve", "reciprocal", ["lrs"], ["lrs"], out=lrs[:], in_=lrs[:])
            for c in range(NCH):
                E("dve", "tensor_tensor", [("zc", c), ("ps", qm_)], ["lta"], out=lta[:], in0=zc[:, c, :], in1=pmn, op=ALU.subtract)
                E("dve", "tensor_tensor", ["lta", "lrs"], ["lta"], out=lta[:], in0=lta[:], in1=lrs[:], op=ALU.mult)
                E("act", "activation", ["lta", "mp"], [("zc", c)], out=zc[:, c, :], in_=lta[:], func=AF.Identity,
                  scale=P("ln1_g")[:, c:c + 1], bias=P("ln1_b")[:, c:c + 1])
            S.add("sp", lambda e, tsl=tsl: e.dma_start(out=xdst[:, tsl].rearrange("(c p) t -> p c t", p=128), in_=zc[:]),
                  reads=[("zc", c) for c in range(NCH)], writes=[("dramw", dstres, n)], dma=True)

    def layer_norm_fm(self, gcol, bcol, ptile, pcols, tmp_sq, tmp_a, tmp_b, stat_m2, stat_rs, tag):
        S = self.S
        onesm = self.col(self.cst, self.const_cols, "onesm")
        for g in range(NTG):
            sl = slice(g * TG, (g + 1) * TG)
            ps_m, ps_q = self.psum[6], self.psum[7]
            for c in range(NCH):
                S.add("act", lambda e, c=c, sl=sl: e.activation(out=tmp_sq[:, c, :], in_=self.xT[:, c, sl], func=AF.Square),
                      reads=[("xT", c, g)], writes=[(tag + "sq", c)])
            for c in range(NCH):
                S.add("pe", lambda e, c=c, sl=sl: e.matmul(ps_m[:], lhsT=onesm, rhs=self.xT[:, c, sl],
                                                            start=(c == 0), stop=(c == NCH - 1)),
                      reads=[("xT", c, g), "cst"], writes=[("ps", 6)])
            for c in range(NCH):
                S.add("pe", lambda e, c=c: e.matmul(ps_q[:], lhsT=onesm, rhs=tmp_sq[:, c, :],
                                                    start=(c == 0), stop=(c == NCH - 1)),
                      reads=[(tag + "sq", c), "cst"], writes=[("ps", 7)])
            S.add("act", lambda e: e.activation(out=stat_m2[:], in_=ps_m[:], func=AF.Square),
                  reads=[("ps", 6)], writes=[tag + "m2"])
            S.add("dve", lambda e: e.tensor_tensor(out=stat_rs[:], in0=ps_q[:], in1=stat_m2[:], op=ALU.subtract),
                  reads=[("ps", 7), tag + "m2"], writes=[tag + "rs"])
            S.add("dve", lambda e: e.tensor_scalar(out=stat_rs[:], in0=stat_rs[:], scalar1=LN_EPS, scalar2=None, op0=ALU.add),
                  reads=[tag + "rs"], writes=[tag + "rs"])
            S.add("act", lambda e: e.activation(out=stat_rs[:], in_=stat_rs[:], func=AF.Sqrt),
                  reads=[tag + "rs"], writes=[tag + "rs"])
            S.add("dve", lambda e: e.reciprocal(out=stat_rs[:], in_=stat_rs[:]),
                  reads=[tag + "rs"], writes=[tag + "rs"])
            for c in range(NCH):
                ta = tmp_a[c % 2]
                S.add("dve", lambda e, c=c, sl=sl, ta=ta: e.tensor_tensor(out=ta[:], in0=self.xT[:, c, sl], in1=ps_m[:], op=ALU.subtract),
                      reads=[("xT", c, g), ("ps", 6)], writes=[(tag + "ta", c % 2)])
                S.add("dve", lambda e, ta=ta: e.tensor_tensor(out=ta[:], in0=ta[:], in1=stat_rs[:], op=ALU.mult),
                      reads=[(tag + "ta", c % 2), tag + "rs"], writes=[(tag + "ta", c % 2)])
                gs = self.col(ptile, pcols, gcol, c, 1)
                bs = self.col(ptile, pcols, bcol, c, 1)
                S.add("act", lambda e, c=c, sl=sl, ta=ta, gs=gs, bs=bs: e.activation(
                    out=self.xT[:, c, sl], in_=ta[:], func=AF.Identity, scale=gs, bias=bs),
                    reads=[(tag + "ta", c % 2), tag + "p"], writes=[("xT", c, g)])

    def moe_layer(self, l, xsrc, srcres, xdst, dstres, last):
        nc, S, E = self.nc, self.S, self.E
        mc = self.moe_cols
        nmc = max(v[0] + v[1] for v in mc.values())
        tag = f"m{l}"
        Xd, Yd = self.Xd[l], self.Yd[l]
        self.xT = self.sb_cached("xT", [128, NCH, T], F32)
        for c in range(NCH):
            S.add("sp", lambda e, c=c: e.dma_start(out=self.xT[:, c, :], in_=xsrc[c * 128:(c + 1) * 128, :]),
                  reads=[("dramw", srcres, n) for n in range(T // 128)], writes=[("xT", c, g) for g in range(NTG)], dma=True)
        mp = self.sb([128, nmc], F32, "moep")
        S.add("sp", lambda e: e.dma_start(out=mp[:], in_=self.d_moep[l]), writes=[tag + "p"], dma=True)
        m_after_mp = self.mark()
        C = lambda name, lo=0, n=None: self.col(self.cst, self.const_cols, name, lo, n)
        ident, ones = C("ident"), C("ones")
        NT = T // 128
        GT = self.sb([32, T], F32, "GT")
        idx4 = self.sb([128, NT, 4], I32, "idx4")
        g4 = self.sb([128, NT, 4], F32, "g4")
        bar = self.sb([1, 8], F32, "bar")
        m_keep = self.mark()
        tot = self.sb([128, NE], F32, "tot")
        lg = self.sb([128, NE], F32, "lg"); m8 = self.sb([128, 8], F32, "m8"); nmx = self.sb([128, 1], F32, "nmx")
        msk = self.sb([128, NE], F32, "msk"); ex = self.sb([128, NE], F32, "ex"); ssum = self.sb([128, 1], F32, "ssum")
        gts = self.sb([128, NE], F32, "gts"); ptt = self.sb([128, NE], F32, "ptt"); okk = self.sb([128, NE], F32, "okk")
        s1 = self.sb([128, NE], F32, "s1"); m8b = self.sb([128, 8], F32, "m8b"); eq4 = self.sb([128, 4, NE], F32, "eq4")
        x1f = [self.sb([128, D], BF16, "x1f") for _ in range(2)]
        wr = self.col(mp, mc, "w_router"); br = self.col(mp, mc, "b_router")
        E("dve", "memset", [], [tag + "tot"], ap=tot[:], constant=0.0)
        for i in range(NT):
            ts_ = slice(i * 128, (i + 1) * 128)
            g = i // 4
            ps = self.psum[i % 2]
            for c in range(NCH):
                E("pe", "matmul", [("xT", c, g), tag + "p"], [("ps", i % 2)], out=ps[:, 0:NE], lhsT=self.xT[:, c, ts_],
                  rhs=wr[:, c * NE:(c + 1) * NE], start=(c == 0), stop=(c == NCH - 1))
            E("dve", "tensor_tensor", [("ps", i % 2), tag + "p"], [tag + "lg"], out=lg[:], in0=ps[:, 0:NE], in1=br, op=ALU.add)
            E("dve", "max", [tag + "lg"], [tag + "m8"], out=m8[:], in_=lg[:])
            E("dve", "tensor_scalar", [tag + "lg", tag + "m8"], [tag + "msk"], out=msk[:], in0=lg[:], scalar1=m8[:, 3:4], scalar2=None, op0=ALU.is_ge)
            E("dve", "tensor_scalar", [tag + "m8"], [tag + "nmx"], out=nmx[:], in0=m8[:, 0:1], scalar1=-1.0, scalar2=None, op0=ALU.mult)
            E("act", "activation", [tag + "lg", tag + "nmx"], [tag + "ex"], out=ex[:], in_=lg[:], func=AF.Exp, bias=nmx[:, 0:1], scale=1.0)
            E("dve", "tensor_tensor", [tag + "ex", tag + "msk"], [tag + "ex"], out=ex[:], in0=ex[:], in1=msk[:], op=ALU.mult)
            E("dve", "reduce_sum", [tag + "ex"], [tag + "ssum"], out=ssum[:], in_=ex[:], axis=AX.X)
            E("dve", "reciprocal", [tag + "ssum"], [tag + "ssum"], out=ssum[:], in_=ssum[:])
            E("dve", "tensor_scalar", [tag + "ex", tag + "ssum"], [tag + "gts"], out=gts[:], in0=ex[:], scalar1=ssum[:, 0:1], scalar2=None, op0=ALU.mult)
            pt = self.psum[2 + i % 2]
            E("pe", "transpose", [tag + "gts", "cst"], [("ps", 2 + i % 2)], out=pt[0:NE, 0:128], in_=gts[:], identity=ident)
            E("act", "activation", [("ps", 2 + i % 2)], [(tag + "GT", g)], out=GT[:, ts_], in_=pt[0:NE, 0:128], func=AF.Copy)
            pp = self.psum[4]
            E("pe", "matmul", [tag + "msk", "cst"], [("ps", 4)], out=pp[:, 0:NE], lhsT=C("triu"), rhs=msk[:], start=True, stop=True)
            E("dve", "tensor_tensor", [("ps", 4), tag + "tot"], [tag + "ptt"], out=ptt[:], in0=pp[:, 0:NE], in1=tot[:], op=ALU.add)
            pq = self.psum[5]
            E("pe", "matmul", [tag + "msk", "cst"], [("ps", 5)], out=pq[:, 0:NE], lhsT=ones, rhs=msk[:], start=True, stop=True)
            E("dve", "tensor_tensor", [("ps", 5), tag + "tot"], [tag + "tot"], out=tot[:], in0=pq[:, 0:NE], in1=tot[:], op=ALU.add)
            E("dve", "tensor_scalar", [tag + "ptt"], [tag + "okk"], out=okk[:], in0=ptt[:], scalar1=float(CAP), scalar2=None, op0=ALU.is_lt)
            E("dve", "tensor_tensor", [tag + "okk", tag + "msk"], [tag + "okk"], out=okk[:], in0=okk[:], in1=msk[:], op=ALU.mult)
            E("dve", "tensor_tensor", [tag + "ptt", "cst"], [tag + "s1"], out=s1[:], in0=ptt[:], in1=C("ec1"), op=ALU.add)
            E("dve", "tensor_tensor", [tag + "s1", tag + "okk"], [tag + "s1"], out=s1[:], in0=s1[:], in1=okk[:], op=ALU.mult)
            E("dve", "max", [tag + "s1"], [tag + "m8b"], out=m8b[:], in_=s1[:])
            E("dve", "tensor_copy", [tag + "m8b"], [(tag + "idx", i)], out=idx4[:, i, :], in_=m8b[:, 0:4])
            E("dve", "tensor_tensor", [tag + "s1", tag + "m8b"], [tag + "eq4"], out=eq4[:],
              in0=s1[:].rearrange("p (o e) -> p o e", o=1).to_broadcast([128, 4, NE]),
              in1=m8b[:, 0:4].rearrange("p (k o) -> p k o", o=1).to_broadcast([128, 4, NE]), op=ALU.is_equal)
            E("dve", "tensor_tensor", [tag + "eq4", tag + "gts"], [tag + "eq4"], out=eq4[:], in0=eq4[:],
              in1=gts[:].rearrange("p (o e) -> p o e", o=1).to_broadcast([128, 4, NE]), op=ALU.mult)
            E("dve", "tensor_reduce", [tag + "eq4"], [(tag + "g4", i)], out=g4[:, i, :], in_=eq4[:], axis=AX.X, op=ALU.add)
            xf = x1f[i % 2]
            for c in range(NCH):
                bk = 6 + c // 4
                E("pe", "transpose", [("xT", c, g), "cst"], [("ps", bk)], out=self.psum[bk][:, (c % 4) * 128:(c % 4 + 1) * 128],
                  in_=self.xT[:, c, ts_], identity=ident)
            for hb in range(2):
                E("act", "activation", [("ps", 6 + hb)], [(tag + "x1f", i % 2)], out=xf[:, hb * 512:(hb + 1) * 512], in_=self.psum[6 + hb][:], func=AF.Copy)
            for k in range(4):
                S.add("pool", lambda e, xf=xf, i=i, k=k: e.indirect_dma_start(
                    out=Xd, out_offset=bass.IndirectOffsetOnAxis(ap=idx4[:, i, k:k + 1], axis=0), in_=xf[:], in_offset=None),
                    reads=[(tag + "x1f", i % 2), (tag + "idx", i), ("Xdz", l)], writes=[(tag + "Xd", i, k)], dma=True)
        bd = self.col(mp, mc, "b_down")
        for c in range(NCH):
            for g in range(NTG):
                sl = slice(g * TG, (g + 1) * TG)
                bk = (c * NTG + g) % 2
                ps = self.psum[bk]
                E("pe", "matmul", [tag + "p", (tag + "GT", g)], [("ps", bk)], out=ps[:], lhsT=bd[0:NE, c * 128:(c + 1) * 128], rhs=GT[:, sl], start=True, stop=True)
                E("dve", "scalar_tensor_tensor", [("xT", c, g), ("ps", bk)], [("xT", c, g)], out=self.xT[:, c, sl], in0=self.xT[:, c, sl],
                  scalar=ALPHA, in1=ps[:], op0=ALU.mult, op1=ALU.add)
        E("dve", "memset", [(tag + "Xd", i, k) for i in range(NT) for k in range(4)], [tag + "Xd_ready", tag + "bar"], ap=bar[:], constant=0.0)
        NSLOT, QW = 4, 512
        ring = [self.sb([128, NCH, QW], BF16, "wr") for _ in range(NSLOT)]
        xe = self.sb([128, CAP // 128, D], BF16, "xe")
        xeT = self.sb([128, NCH, CAP], BF16, "xeT")
        hT = self.sb([128, NCH, CAP], BF16, "hT")
        ye = [self.sb([128, CAP // 128, D], BF16, "ye") for _ in range(2)]
        tmpg = [self.sb([128, CAP], F32, "tg") for _ in range(2)]
        tmps = [self.sb([128, CAP], BF16, "ts") for _ in range(2)]
        tmpu = [self.sb([128, CAP], BF16, "tu") for _ in range(2)]
        slot_ctr = [0]

        def load_q(dram_w, e_, q):
            s = slot_ctr[0] % NSLOT
            slot_ctr[0] += 1
            wsrc = dram_w[l, e_, :, q * QW:(q + 1) * QW].rearrange("(c p) f -> p c f", p=128)
            S.add("pool", lambda e, s=s, wsrc=wsrc: e.dma_start(out=ring[s][:], in_=wsrc), writes=[(tag + "ring", s)], dma=True)
            return s

        bgc = self.col(mp, mc, "b_gate"); buc = self.col(mp, mc, "b_up")
        import os
        nexp = int(os.environ.get("N_EXPERTS", NE))
        tcnt = 0
        for ex_ in range(nexp):
            r0 = 1 + ex_ * CAP
            S.add("sp", lambda e, r0=r0: e.dma_start(out=xe[:], in_=Xd[r0:r0 + CAP, :].rearrange("(s p) d -> p s d", p=128)),
                  reads=[tag + "Xd_ready"], writes=[tag + "xe"], dma=True)
            for c in range(NCH):
                bk = 6 + c % 2
                pbf = self.psum[bk][:].bitcast(BF16)
                for st in range(CAP // 128):
                    E("pe", "transpose", [tag + "xe", "identb"], [("ps", bk)], out=pbf[:, st * 128:(st + 1) * 128],
                      in_=xe[:, st, c * 128:(c + 1) * 128], identity=self.identb[:])
                E("act", "activation", [("ps", bk)], [(tag + "xeT", c)], out=xeT[:, c, :], in_=pbf[:, 0:CAP], func=AF.Copy)
            sg = su = None
            for f in range(NCH):
                if f % 4 == 0:
                    sg = load_q(self.d_wg, ex_, f // 4)
                    su = load_q(self.d_wu, ex_, f // 4)
                fi = f % 4
                k = tcnt % 2
                tcnt += 1
                pg, pu = self.psum[k], self.psum[2 + k]
                for c in range(NCH):
                    E("pe", "matmul", [(tag + "ring", sg), (tag + "xeT", c)], [("ps", k)], out=pg[:, 0:CAP], lhsT=ring[sg][:, c, fi * 128:(fi + 1) * 128],
                      rhs=xeT[:, c, :], start=(c == 0), stop=(c == NCH - 1))
                for c in range(NCH):
                    E("pe", "matmul", [(tag + "ring", su), (tag + "xeT", c)], [("ps", 2 + k)], out=pu[:, 0:CAP], lhsT=ring[su][:, c, fi * 128:(fi + 1) * 128],
                      rhs=xeT[:, c, :], start=(c == 0), stop=(c == NCH - 1))
                bg1 = bgc[:, ex_ * 8 + f:ex_ * 8 + f + 1]
                bu1 = buc[:, ex_ * 8 + f:ex_ * 8 + f + 1]
                tg_, ts2, tu_ = tmpg[k], tmps[k], tmpu[k]
                E("dve", "tensor_scalar", [("ps", k), tag + "p"], [(tag + "tg", k)], out=tg_[:], in0=pg[:, 0:CAP], scalar1=bg1, scalar2=7.0, op0=ALU.add, op1=ALU.min)
                E("act", "activation", [(tag + "tg", k)], [(tag + "ts", k)], out=ts2[:], in_=tg_[:], func=AF.Sigmoid, scale=1.702)
                E("dve", "tensor_scalar", [("ps", 2 + k), tag + "p"], [(tag + "tu", k)], out=tu_[:], in0=pu[:, 0:CAP], scalar1=bu1, scalar2=7.0, op0=ALU.add, op1=ALU.min)
                E("dve", "tensor_scalar", [(tag + "tu", k)], [(tag + "tu", k)], out=tu_[:], in0=tu_[:], scalar1=-7.0, scalar2=1.0, op0=ALU.max, op1=ALU.add)
                E("dve", "tensor_tensor", [(tag + "tg", k), (tag + "ts", k)], [(tag + "ts", k)], out=ts2[:], in0=tg_[:], in1=ts2[:], op=ALU.mult)
                E("dve", "tensor_tensor", [(tag + "tu", k), (tag + "ts", k)], [(tag + "hT", f)], out=hT[:, f, :], in0=tu_[:], in1=ts2[:], op=ALU.mult)
            yy = ye[ex_ % 2]
            ycnt = 0
            for half in range(2):
                sd = load_q(self.d_wd, ex_, half)
                for st in range(CAP // 128):
                    bk = 4 + ycnt % 2
                    ycnt += 1
                    py = self.psum[bk]
                    for f in range(NCH):
                        E("pe", "matmul", [(tag + "ring", sd), (tag + "hT", f)], [("ps", bk)], out=py[:], lhsT=hT[:, f, st * 128:(st + 1) * 128],
                          rhs=ring[sd][:, f, :], start=(f == 0), stop=(f == NCH - 1))
                    E("act", "activation", [("ps", bk)], [(tag + "ye", ex_ % 2)], out=yy[:, st, half * 512:(half + 1) * 512], in_=py[:], func=AF.Copy)
            S.add("sp", lambda e, r0=r0, yy=yy: e.dma_start(out=Yd[r0:r0 + CAP, :].rearrange("(s p) d -> p s d", p=128), in_=yy[:]),
                  reads=[(tag + "ye", ex_ % 2)], writes=[(tag + "Yd", ex_)], dma=True)
        E("dve", "memset", [(tag + "Yd", e_) for e_ in range(nexp)] + [("Ydz", l)], [tag + "Yd_ready", tag + "bar"], ap=bar[:], constant=0.0)
        S.fence()
        self.release(m_keep)
        yg = [self.sb([128, D], BF16, "yg") for _ in range(4)]
        acc = [self.sb([128, D], F32, "acc") for _ in range(2)]
        for i in range(NT):
            ts_ = slice(i * 128, (i + 1) * 128)
            g = i // 4
            for k in range(4):
                S.add("pool", lambda e, i=i, k=k: e.indirect_dma_start(
                    out=yg[k][:], out_offset=None, in_=Yd, in_offset=bass.IndirectOffsetOnAxis(ap=idx4[:, i, k:k + 1], axis=0)),
                    reads=[tag + "Yd_ready", (tag + "idx", i)], writes=[(tag + "yg", k)], dma=True)
            ac = acc[i % 2]
            E("dve", "tensor_scalar", [(tag + "yg", 0), (tag + "g4", i)], [(tag + "acc", i % 2)], out=ac[:], in0=yg[0][:], scalar1=g4[:, i, 0:1], scalar2=None, op0=ALU.mult)
            for k in range(1, 4):
                E("dve", "scalar_tensor_tensor", [(tag + "yg", k), (tag + "g4", i), (tag + "acc", i % 2)], [(tag + "acc", i % 2)], out=ac[:], in0=yg[k][:],
                  scalar=g4[:, i, k:k + 1], in1=ac[:], op0=ALU.mult, op1=ALU.add)
            for c in range(NCH):
                bk = 6 + c // 4
                E("pe", "transpose", [(tag + "acc", i % 2), "cst"], [("ps", bk)], out=self.psum[bk][:, (c % 4) * 128:(c % 4 + 1) * 128],
                  in_=ac[:, c * 128:(c + 1) * 128], identity=ident)
            for hb in range(2):
                E("dve", "tensor_tensor", [("ps", 6 + hb)] + [("xT", 4 * hb + cc_, g) for cc_ in range(4)], [("xT", 4 * hb + cc_, g) for cc_ in range(4)],
                  out=self.xT[:, 4 * hb:4 * hb + 4, ts_], in0=self.xT[:, 4 * hb:4 * hb + 4, ts_],
                  in1=self.psum[6 + hb][:].rearrange("p (c t) -> p c t", c=4), op=ALU.add)
        S.fence()
        m = self.mark()
        tmp_sq = self.sb([128, NCH, TG], F32, "lnsq")
        tmp_a = [self.sb([128, TG], F32, "lna") for _ in range(2)]
        st_m2 = self.sb([128, TG], F32, "lnm2")
        st_rs = self.sb([128, TG], F32, "lnrs")
        self.layer_norm_fm("ln2_g", "ln2_b", mp, mc, tmp_sq, tmp_a, None, st_m2, st_rs, tag)
        self.release(m)
        for c in range(NCH):
            op = S.add("sp", lambda e, c=c: e.dma_start(out=xdst[c * 128:(c + 1) * 128, :], in_=self.xT[:, c, :]),
                       reads=[("xT", c, g) for g in range(NTG)], writes=[("dram", dstres, c)], dma=True)
            if last:
                self.finals.append(op)


_CACHE = {}


def kernel(**inputs):
    x = np.asarray(inputs["x"], np.float32)
    pos = np.asarray(inputs["positions"], np.int32)
    B = x.shape[0]
    L = DEPTH
    cp = const_pack()
    xps = [mix_pack(l, inputs) for l in range(L)]
    mps = [moe_pack(l, inputs) for l in range(L)]
    if "full" not in _CACHE:
        _CACHE["full"] = Builder(L, moe_cols=mps[0].cols, const_cols=cp.cols, mode="full", mix_cols=xps[0].cols).build()
    nc = _CACHE["full"]
    shared = {
        "consts": cp.array(),
        "mixp": np.stack([p.array() for p in xps]),
        "moep": np.stack([p.array() for p in mps]),
        "w_in_ext": np.stack([w_in_ext(inputs["w_in"][l]) for l in range(L)]),
        "w_out": np.ascontiguousarray(inputs["w_out"], np.float32),
        "w_gate": np.ascontiguousarray(inputs["w_gate"], np.float32),
        "w_up": np.ascontiguousarray(inputs["w_up"], np.float32),
        "w_down": np.ascontiguousarray(inputs["w_down"], np.float32),
    }
    in_maps = []
    for b in range(B):
        m = dict(shared)
        m["xT"] = np.ascontiguousarray(x[b].T)
        m["posb"] = np.ascontiguousarray(np.broadcast_to(pos[b][None, :], (128, T))).astype(np.int32)
        in_maps.append(m)
    res = run_bass_kernel_spmd(nc, in_maps, core_ids=list(range(B)))
    return np.stack([r["outT"].T for r in res.results]).astype(np.float32)
```

```python
import contextlib
import numpy as np
import concourse.bass as bass
import concourse.mybir as mybir
from concourse.bass_utils import run_bass_kernel_spmd

F32 = mybir.dt.float32
BF16 = mybir.dt.bfloat16
I32 = mybir.dt.int32
AF = mybir.ActivationFunctionType
ALU = mybir.AluOpType
AX = mybir.AxisListType

D = 1024
T = 2048
DEPTH = 2
NE = 32
ALPHA = (2.0 * DEPTH) ** 0.25
LN_EPS = 1e-5
NORM_EPS = 1e-6
NCH = 8
NTG = 4
TG = 512

ENGS = ("pe", "act", "dve", "pool", "sp")
EPOCH = 8000
NDMASEM = 10


class Op:
    __slots__ = ("eng", "fn", "dma", "deps", "idx", "sig", "dsem", "dval", "nsig", "prewait")

    def __init__(self, eng, fn, dma):
        self.eng = eng
        self.fn = fn
        self.dma = dma
        self.deps = {}
        self.sig = False
        self.nsig = None
        self.dsem = None
        self.dval = None
        self.prewait = None


class Sched:
    def __init__(self, nc):
        self.nc = nc
        self.ops = []
        self.state = {}
        self.dma_count = {e: 0 for e in ENGS}
        self.dma_hist = {e: [] for e in ENGS}
        self.fence_op = None
        self.fenced = set()
        self.last = {e: None for e in ENGS}

    def _dep(self, op, prod, kind):
        if prod is None or prod is op:
            return
        if prod.dma:
            op.deps[("d", id(prod))] = prod
            return
        if prod.eng == op.eng and not op.dma:
            if op.eng == "pe":
                return
        cur = op.deps.get(prod.eng)
        if cur is None or cur.idx < prod.idx:
            op.deps[prod.eng] = prod

    def add(self, eng, fn, reads=(), writes=(), dma=False):
        op = Op(eng, fn, dma)
        op.idx = len(self.ops)
        if self.fence_op is not None and eng not in self.fenced:
            self.fenced.add(eng)
            for f in self.fence_op:
                self._dep(op, f, "raw" if f.eng != eng else "waw")
        for r in reads:
            st = self.state.get(r)
            if st is None:
                st = self.state[r] = [None, []]
            self._dep(op, st[0], "raw")
            if isinstance(r, tuple) and r[0] == "ps":
                for rd in st[1]:
                    if rd.eng != eng:
                        self._dep(op, rd, "war")
        for w in writes:
            st = self.state.get(w)
            if st is None:
                st = self.state[w] = [None, []]
            self._dep(op, st[0], "waw")
            for rd in st[1]:
                self._dep(op, rd, "war")
        for r in reads:
            self.state[r][1].append(op)
        for w in writes:
            st = self.state[w]
            st[0] = op
            st[1] = []
        if dma:
            k = self.dma_count[eng]
            self.dma_count[eng] = k + 1
            op.dsem = k % NDMASEM
            op.dval = 16 * (k // NDMASEM + 1)
            hist = self.dma_hist[eng]
            if k >= NDMASEM:
                op.prewait = hist[k - NDMASEM]
            hist.append(op)
        else:
            self.last[eng] = op
        self.ops.append(op)
        return op

    def fence(self):
        prods = [o for o in self.last.values() if o is not None]
        for e in ENGS:
            prods.extend(self.dma_hist[e][-NDMASEM:])
        self.fence_op = prods
        self.fenced = set()

    def emit(self, final_ops=()):
        nc = self.nc
        for op in self.ops:
            for p in op.deps.values():
                if not p.dma:
                    p.sig = True
        counts = {e: 0 for e in ENGS}
        for op in self.ops:
            if op.sig and not op.dma:
                counts[op.eng] += 1
                op.nsig = counts[op.eng]
        with contextlib.ExitStack() as es:
            csem = {}
            for e in ENGS:
                n_ep = counts[e] // EPOCH + 1
                csem[e] = [es.enter_context(nc.semaphore(f"c_{e}_{i}")) for i in range(n_ep)]
            dsem = {}
            for e in ENGS:
                if self.dma_count[e]:
                    dsem[e] = [es.enter_context(nc.semaphore(f"d_{e}_{i}"))
                               for i in range(min(NDMASEM, self.dma_count[e]))]
            block = es.enter_context(nc.Block())
            per_eng = {e: [o for o in self.ops if o.eng == e] for e in ENGS}

            def wait_for(engine, p, waited):
                if p.dma:
                    key, val = (p.eng, "d", p.dsem), p.dval
                else:
                    ep, v = divmod(p.nsig - 1, EPOCH)
                    if waited.get((p.eng, "ep"), -1) > ep:
                        return
                    key, val = (p.eng, "c", ep), v + 1
                if waited.get(key, 0) >= val:
                    return
                waited[key] = val
                if p.dma:
                    engine.wait_ge(dsem[p.eng][p.dsem], val)
                else:
                    waited[(p.eng, "ep")] = max(waited.get((p.eng, "ep"), -1), ep)
                    engine.wait_ge(csem[p.eng][ep], val)

            def body(ename, tail):
                def f(engine):
                    waited = {}
                    for op in per_eng[ename]:
                        if op.prewait is not None:
                            wait_for(engine, op.prewait, waited)
                        for p in op.deps.values():
                            wait_for(engine, p, waited)
                        ins = op.fn(engine)
                        if op.dma:
                            ins.then_inc(dsem[ename][op.dsem], 16)
                        elif op.sig:
                            ep, v = divmod(op.nsig - 1, EPOCH)
                            ins.then_inc(csem[ename][ep], 1)
                    if tail:
                        for p in final_ops:
                            wait_for(engine, p, waited)
                return f

            block.sync(body("sp", True))
            block.tensor(body("pe", False))
            block.scalar(body("act", False))
            block.vector(body("dve", False))
            block.gpsimd(body("pool", False))


def _fm(v):
    v = np.asarray(v, np.float32)
    return np.ascontiguousarray(v.reshape(-1, 128).T)


class Pack:
    def __init__(self):
        self.cols = {}
        self.n = 0
        self.parts = []

    def put(self, name, arr):
        arr = np.asarray(arr, np.float32)
        if arr.ndim == 1:
            arr = arr[:, None]
        p, w = arr.shape
        full = np.zeros((128, w), np.float32)
        full[:p] = arr
        self.cols[name] = (self.n, w)
        self.n += w
        self.parts.append(full)

    def array(self):
        return np.ascontiguousarray(np.concatenate(self.parts, axis=1))


def moe_pack(l, inp):
    pk = Pack()
    pk.put("ln2_g", _fm(inp["ln2_g"][l]))
    pk.put("ln2_b", _fm(inp["ln2_b"][l]))
    pk.put("b_gate", inp["b_gate"][l].reshape(NE, 8, 128).transpose(2, 0, 1).reshape(128, NE * 8))
    pk.put("b_up", inp["b_up"][l].reshape(NE, 8, 128).transpose(2, 0, 1).reshape(128, NE * 8))
    pk.put("w_router", inp["w_router"][l].reshape(8, 128, NE).transpose(1, 0, 2).reshape(128, 8 * NE))
    pk.put("b_router", np.broadcast_to(inp["b_router"][l][None, :], (128, NE)))
    pk.put("b_down", inp["b_down"][l])
    return pk


NWIN = 3096 + 512
CAP = 512


def w_in_ext(w_in_l):
    w = np.asarray(w_in_l, np.float32)
    idx = []
    for base in (0, 256):
        for h in range(4):
            idx += list(range(base + 64 * h + 32, base + 64 * h + 64)) + list(range(base + 64 * h, base + 64 * h + 32))
    return np.ascontiguousarray(np.concatenate([w, w[:, idx]], axis=1))


def mix_pack(l, inp):
    pk = Pack()
    pk.put("ln1_g", _fm(inp["ln1_g"][l]))
    pk.put("ln1_b", _fm(inp["ln1_b"][l]))
    cw = np.asarray(inp["ssd_conv_w"][l], np.float32)
    pk.put("conv_w", cw.reshape(4, 6, 128).transpose(2, 1, 0).reshape(128, 24))
    pk.put("conv_b", np.asarray(inp["ssd_conv_b"][l], np.float32).reshape(6, 128).T)
    pk.put("dt_bias", np.asarray(inp["ssd_dt_bias"][l], np.float32).reshape(8, 1))
    pk.put("a_log", np.asarray(inp["ssd_a_log"][l], np.float32).reshape(8, 1))
    pk.put("bgk", np.asarray(inp["gla_b_gk2"][l], np.float32).reshape(2, 64).T)
    pk.put("w_gk2", np.asarray(inp["gla_w_gk2"][l], np.float32))
    pk.put("ret_nw", np.broadcast_to(np.asarray(inp["ret_norm_w"][l], np.float32)[None, :], (128, 256)))
    pk.put("ssd_nw", np.broadcast_to(np.asarray(inp["ssd_norm_w"][l], np.float32)[None, :], (128, 512)))
    pk.put("gla_nw", np.broadcast_to(np.asarray(inp["gla_norm_w"][l], np.float32)[None, :], (128, 256)))
    pk.put("ssd_d", np.broadcast_to(np.asarray(inp["ssd_d"][l], np.float32)[None, :], (128, 8)))
    return pk


def const_pack():
    pk = Pack()
    pk.put("ident", np.eye(128, dtype=np.float32))
    pk.put("onesm", np.full((128, 128), 1.0 / D, np.float32))
    pk.put("ones", np.ones((128, 128), np.float32))
    s_ = np.arange(128)[:, None].astype(np.float64)
    c_ = np.arange(128)[None, :].astype(np.float64)
    causal = (c_ >= s_).astype(np.float64)
    pk.put("causal", causal)
    gam = [1.0 - 2.0 ** (-5.0 - h) for h in range(4)]
    pk.put("retmask", np.concatenate([np.where(c_ >= s_, gam[h] ** np.maximum(c_ - s_, 0.0), 0.0) * 0.125 for h in range(4)], axis=1))
    pk.put("gq", np.concatenate([np.broadcast_to(gam[h] ** (c_ + 1.0), (64, 128)) for h in range(4)], axis=1))
    pk.put("gk", np.concatenate([gam[h] ** (127.0 - s_) * 0.125 for h in range(4)], axis=1))
    d_ = np.arange(128) % 64
    invf = (10000.0 ** (-(d_ % 32).astype(np.float64) / 32.0)).astype(np.float32).astype(np.float64) / (2.0 * np.pi)
    pk.put("invf", invf.reshape(128, 1))
    pk.put("sgn", np.where(d_ < 32, -1.0, 1.0).reshape(128, 1))
    pk.put("gq2", np.concatenate([np.concatenate([np.broadcast_to(gam[2 * p2 + j] ** (c_ + 1.0), (64, 128)) for j in range(2)], axis=0) for p2 in range(2)], axis=1))
    pk.put("triu", (s_ < c_).astype(np.float64))
    pk.put("ec1", np.broadcast_to((np.arange(NE) * CAP + 1.0)[None, :], (128, NE)))
    sel = np.zeros((8, 8 * 128))
    for h in range(8):
        sel[h, h * 128:(h + 1) * 128] = 1.0
    pk.put("sel", sel)
    selm = np.zeros((8, 2 * 4 * 128))
    for g_ in range(2):
        for j_ in range(4):
            selm[4 * g_ + j_, g_ * 512 + j_ * 128:g_ * 512 + (j_ + 1) * 128] = 1.0
    pk.put("selm", selm)
    return pk


class Builder:
    def __init__(self, nlayers, moe_cols, const_cols, mode="full", n_experts=NE, e_lo=0, e_hi=NE, init=True, ln=True,
                 mix_cols=None):
        self.L = nlayers
        self.mode = mode
        self.n_experts = n_experts
        self.e_lo, self.e_hi, self.init, self.ln = e_lo, e_hi, init, ln
        self.mix_cols = mix_cols
        self.moe_cols = moe_cols
        self.const_cols = const_cols
        self.nc = bass.Bass("TRN2", target_bir_lowering=False)
        self.S = Sched(self.nc)
        self.sb_off = (self.nc.sbuf_base + 31) // 32 * 32
        self.sb_top = self.nc.sbuf_top
        self.uid = 0
        self.finals = []

    def sb(self, shape, dt, name=None):
        nbytes = int(np.prod(shape[1:])) * (4 if dt in (F32, I32) else 2)
        nbytes = (nbytes + 31) // 32 * 32
        off = self.sb_off
        assert off + nbytes <= self.sb_top, f"SBUF overflow allocating {name} {shape}: {off + nbytes - self.sb_top}"
        self.sb_off += nbytes
        self.uid += 1
        return self.nc.alloc_sbuf_tensor_at(f"{name or 't'}{self.uid}", list(shape), dt, offset=off)

    def sb_cached(self, key, shape, dt):
        if not hasattr(self, "_cached"):
            self._cached = {}
        if key in self._cached:
            t, off, nbytes = self._cached[key]
            assert off == self.sb_off, (key, off, self.sb_off)
            self.sb_off += nbytes
            return t
        off = self.sb_off
        t = self.sb(shape, dt, key)
        self._cached[key] = (t, off, self.sb_off - off)
        return t

    def mark(self):
        return self.sb_off

    def release(self, m):
        self.sb_off = m

    def dram_in(self, name, shape, dt=F32):
        return self.nc.dram_tensor(name, list(shape), dt, kind="ExternalInput").ap()

    def dram_out(self, name, shape, dt=F32):
        return self.nc.dram_tensor(name, list(shape), dt, kind="ExternalOutput").ap()

    def col(self, tile, cols, name, lo=0, n=None):
        c0, w = cols[name]
        if n is None:
            n = w - lo
        return tile[:, c0 + lo:c0 + lo + n]

    def build(self):
        nc, S, L = self.nc, self.S, self.L
        ncc = max(v[0] + v[1] for v in self.const_cols.values())
        nmc = max(v[0] + v[1] for v in self.moe_cols.values())
        nxc = max(v[0] + v[1] for v in self.mix_cols.values())
        self.d_xT = self.dram_in("xT", [D, T])
        self.d_const = self.dram_in("consts", [128, ncc])
        self.d_out = self.dram_out("outT", [D, T])
        self.d_moep = self.dram_in("moep", [L, 128, nmc])
        self.d_wg = self.dram_in("w_gate", [L, NE, D, D])
        self.d_wu = self.dram_in("w_up", [L, NE, D, D])
        self.d_wd = self.dram_in("w_down", [L, NE, D, D])
        self.d_mixp = self.dram_in("mixp", [L, 128, nxc])
        self.d_win = self.dram_in("w_in_ext", [L, D, NWIN])
        self.d_wout = self.dram_in("w_out", [L, D, D])
        self.d_posb = self.dram_in("posb", [128, T], I32)
        self.d_rot = nc.dram_tensor("rotd", [2, 128, T], F32).ap()
        sA = [nc.dram_tensor(f"scrA{l}", [D, T], F32).ap() for l in range(L)]
        sB = [nc.dram_tensor(f"scrB{l}", [D, T], F32).ap() for l in range(L - 1)]

        NS = NE * CAP + 1
        self.Xd = [nc.dram_tensor(f"Xd{l}", [NS, D], BF16).ap() for l in range(L)]
        self.Yd = [nc.dram_tensor(f"Yd{l}", [NS, D], BF16).ap() for l in range(L)]
        self.cst = self.sb([128, ncc], F32, "cst")
        self.psum = [nc.alloc_psum_tensor(f"ps{i}", [128, 512], F32) for i in range(8)]
        S.add("sp", lambda e: e.dma_start(out=self.cst[:], in_=self.d_const), writes=["cst"], dma=True)
        zt = self.sb([128, D], BF16, "zt")
        zbar = self.sb([1, 8], F32, "zbar")
        S.add("pool", lambda e: e.memset(zt[:], 0.0), writes=["zt"])
        self.identb = self.sb([128, 128], BF16, "identb")
        S.add("act", lambda e: e.activation(out=self.identb[:], in_=self.col(self.cst, self.const_cols, "ident"), func=AF.Copy), reads=["cst"], writes=["identb"])
        base = self.mark()
        for l in range(L):
            src = self.d_xT if l == 0 else sB[l - 1]
            self.mixer_layer(l, src, ("B", l - 1), sA[l], ("A", l))
            if l == 0:
                self.zero_fill(zt, zbar)
            S.fence()
            self.release(base)
            last = (l == L - 1)
            self.moe_layer(l, sA[l], ("A", l), self.d_out if last else sB[l], ("B", l), last)
            S.fence()
            self.release(base)
        S.emit(final_ops=self.finals)
        return nc


    def zero_fill(self, zt, zbar):
        S = self.S
        for l in range(self.L):
            Xd_, Yd_ = self.Xd[l], self.Yd[l]
            S.add("pool", lambda e, Xd_=Xd_: e.dma_start(out=Xd_[0:1, :], in_=zt[0:1, :]), reads=["zt"], writes=[("Xdzp", l, -1)], dma=True)
            for b_ in range(NE * CAP // 128):
                S.add("pool", lambda e, Xd_=Xd_, b_=b_: e.dma_start(out=Xd_[1 + b_ * 128:1 + (b_ + 1) * 128, :], in_=zt[:]),
                      reads=["zt"], writes=[("Xdzp", l, b_)], dma=True)
            S.add("pool", lambda e, Yd_=Yd_: e.dma_start(out=Yd_[0:1, :], in_=zt[0:1, :]), reads=["zt"], writes=[("Ydz", l)], dma=True)
            S.add("pool", lambda e: e.memset(zbar[:], 0.0), reads=[("Xdzp", l, b_) for b_ in range(-1, NE * CAP // 128)], writes=[("Xdz", l), "zbar"])

    def E(self, eng, meth, reads, writes, **kw):
        return self.S.add(eng, lambda e: getattr(e, meth)(**kw), reads=reads, writes=writes)

    def nextq(self):
        self.qctr = (getattr(self, "qctr", -1) + 1) % 6
        return 2 + self.qctr

    def psq(self, i):
        return self.psum[i][:, 0:128]

    def mixer_layer(self, l, xsrc, srcres, xdst, dstres):
        S, E = self.S, self.E
        xc, cc = self.mix_cols, self.const_cols
        nxc = max(v[0] + v[1] for v in xc.values())
        self.xTb = self.sb_cached("xTb", [128, NCH, T], BF16)
        mp = self.sb([128, nxc], F32, "mixp")
        S.add("sp", lambda e: e.dma_start(out=mp[:], in_=self.d_mixp[l]), writes=["mp"], dma=True)
        win = self.sb([128, NCH, NWIN], BF16, "win")
        wout = self.sb([128, NCH, D], BF16, "wout")
        for c in range(NCH):
            S.add("pool", lambda e, c=c: e.dma_start(out=win[:, c, :], in_=self.d_win[l, c * 128:(c + 1) * 128, :]),
                  writes=["win"], dma=True)
        S.add("pool", lambda e: e.dma_start(out=wout[:], in_=self.d_wout[l].rearrange("(c p) f -> p c f", p=128)),
              writes=["wout"], dma=True)
        P = lambda name, lo=0, n=None: self.col(mp, xc, name, lo, n)
        C = lambda name, lo=0, n=None: self.col(self.cst, cc, name, lo, n)
        ident = C("ident")
        xTb = self.xTb
        TWO_PI = 2.0 * np.pi

        m0 = self.mark()
        stg = [self.sb([128, TG], F32, "xstg") for _ in range(2)]
        k = 0
        for c in range(NCH):
            for g in range(NTG):
                st = stg[k % 2]
                S.add("sp", lambda e, c=c, g=g, st=st: e.dma_start(out=st[:], in_=xsrc[c * 128:(c + 1) * 128, g * TG:(g + 1) * TG]),
                      reads=[("dram", srcres, c)], writes=[("xstg", k % 2)], dma=True)
                S.add("act", lambda e, c=c, g=g, st=st: e.activation(out=self.xTb[:, c, g * TG:(g + 1) * TG], in_=st[:], func=AF.Copy),
                      reads=[("xstg", k % 2)], writes=[("xTb", c, g)])
                k += 1
        if l == 0:
            cosT = self.sb([128, T], F32, "cosT")
            sinT = self.sb([128, T], F32, "sinT")
            posi = self.sb([128, T], I32, "posi")
            vf = self.sb([128, T], F32, "vf")
            ni = self.sb([128, T], I32, "ni")
            nf = self.sb([128, T], F32, "nf")
            mk = self.sb([128, T], F32, "mk")
            S.add("sp", lambda e: e.dma_start(out=posi[:], in_=self.d_posb), writes=["posi"], dma=True)
            for which, dst in ((0, sinT), (1, cosT)):
                E("dve", "tensor_copy", ["posi"], ["vf"], out=vf[:], in_=posi[:])
                E("dve", "tensor_scalar", ["vf", "cst"], ["vf"], out=vf[:], in0=vf[:], scalar1=C("invf")[:, 0:1],
                  scalar2=(0.25 if which else 0.0), op0=ALU.mult, op1=ALU.add)
                E("dve", "tensor_copy", ["vf"], ["ni"], out=ni[:], in_=vf[:])
                E("dve", "tensor_copy", ["ni"], ["nf"], out=nf[:], in_=ni[:])
                E("dve", "tensor_tensor", ["vf", "nf"], ["vf"], out=vf[:], in0=vf[:], in1=nf[:], op=ALU.subtract)
                E("dve", "tensor_scalar", ["vf"], ["mk"], out=mk[:], in0=vf[:], scalar1=0.5, scalar2=None, op0=ALU.is_gt)
                E("dve", "tensor_tensor", ["vf", "mk"], ["vf"], out=vf[:], in0=vf[:], in1=mk[:], op=ALU.subtract)
                E("dve", "tensor_scalar", ["vf"], ["mk"], out=mk[:], in0=vf[:], scalar1=-0.5, scalar2=None, op0=ALU.is_lt)
                E("dve", "tensor_tensor", ["vf", "mk"], ["vf"], out=vf[:], in0=vf[:], in1=mk[:], op=ALU.add)
                E("act", "activation", ["vf"], [("rot", which)], out=dst[:], in_=vf[:], func=AF.Sin, scale=TWO_PI)
            E("dve", "tensor_scalar", [("rot", 0), "cst"], [("rot", 0)], out=sinT[:], in0=sinT[:], scalar1=C("sgn")[:, 0:1],
              scalar2=None, op0=ALU.mult)
            S.add("sp", lambda e: e.dma_start(out=self.d_rot[0], in_=sinT[:]), reads=[("rot", 0)], writes=[("rotd", 0)], dma=True)
            S.add("sp", lambda e: e.dma_start(out=self.d_rot[1], in_=cosT[:]), reads=[("rot", 1)], writes=[("rotd", 1)], dma=True)
        S.fence()
        self.release(m0)
        rc = [self.sb([128, 2, 128], F32, "rc") for _ in range(2)]

        negA = self.sb([8, 1], F32, "negA")
        E("act", "activation", ["mp"], ["negA"], out=negA[:], in_=P("a_log")[0:8, 0:1], func=AF.Exp)
        E("dve", "tensor_scalar", ["negA"], ["negA"], out=negA[:], in0=negA[:], scalar1=-1.0, scalar2=None, op0=ALU.mult)
        nbgk = self.sb([64, 2], F32, "nbgk")
        E("dve", "tensor_scalar", ["mp"], ["nbgk"], out=nbgk[:], in0=P("bgk")[0:64, 0:2], scalar1=-1.0, scalar2=None, op0=ALU.mult)
        wgk = self.sb([16, 128], F32, "wgk")
        E("dve", "tensor_copy", ["mp"], ["wgk"], out=wgk[:], in_=P("w_gk2")[0:16, 0:128])

        Sret = self.sb([128, 2, 64], F32, "Sret"); Sretb = self.sb([128, 2, 64], BF16, "Sretb")
        Sssd = self.sb([128, 4, 64], F32, "Sssd"); Sssdb = self.sb([128, 4, 64], BF16, "Sssdb")
        Sgla = self.sb([64, 2, 64], F32, "Sgla"); Sglab = self.sb([64, 2, 64], BF16, "Sglab")
        for t_, nm in ((Sret, "Sret"), (Sretb, "Sretb"), (Sssd, "Sssd"), (Sssdb, "Sssdb"), (Sgla, "Sgla"), (Sglab, "Sglab")):
            for h in range(t_.shape[1]):
                wres = [(nm, 2 * h), (nm, 2 * h + 1)] if nm in ("Sret", "Sretb", "Sgla", "Sglab") else ([(nm, h), (nm, h + 4)] if nm in ("Sssd", "Sssdb") else [(nm, h)])
                E("dve", "memset", [], wres, ap=t_[:, h, :], constant=0.0)
        raw = self.sb([128, 6, 131], F32, "raw")
        E("dve", "memset", [], [("raw", r) for r in range(6)], ap=raw[:], constant=0.0)

        tmv = self.sb([128, 256], BF16, "tm_rv")
        tmg = self.sb([128, 256], F32, "tm_rg")
        tmz = self.sb([128, 512], F32, "tm_sz")
        tgv = self.sb([128, 256], BF16, "tm_gv")
        tgg = self.sb([128, 256], F32, "tm_gg")
        def per(nh, shape, dt, nm):
            return [self.sb(shape, dt, nm) for _ in range(nh)]
        t1_s = per(2, [128, 128], F32, "t1"); t2_s = per(2, [128, 128], F32, "t2")
        rq_s = per(2, [128, 128], BF16, "rq"); rqi_s = per(2, [128, 128], BF16, "rqi")
        rkb_s = per(2, [128, 128], BF16, "rkb")
        kst_s = per(2, [128, 128], BF16, "kst")
        PTr_s = per(4, [128, 128], BF16, "PTr"); PTs_s = per(1, [128, 128], BF16, "PTs"); PTg_s = per(2, [128, 128], BF16, "PTg")
        cacc = self.sb([128, 128], F32, "cacc")
        xsT = self.sb([128, 4, 128], F32, "xsT")
        BT = self.sb([128, 128], F32, "BT"); BTb = self.sb([128, 128], BF16, "BTb")
        CT = self.sb([128, 128], F32, "CT")
        xs_tok = self.sb([128, 8, 64], F32, "xs_tok")
        B_tok = self.sb([128, 2, 64], BF16, "B_tok")
        dtT = self.sb([8, 128], F32, "dtT"); aT = self.sb([8, 128], F32, "aT"); acT = self.sb([8, 128], F32, "acT")
        dt_tok = self.sb([128, 8], F32, "dt_tok"); ac_tok = self.sb([128, 8], F32, "ac_tok")
        Xm4_s = per(2, [128, 4, 128], F32, "Xm4"); PT4_s = per(2, [128, 4, 128], BF16, "PT4")
        EB4_s = per(2, [128, 4, 128], F32, "EB4"); Ct4_s = per(2, [128, 4, 128], BF16, "Ct4")
        wc4_s = per(2, [128, 4], F32, "wc4"); vh4_s = per(2, [128, 4, 64], BF16, "vh4"); vst4_s = per(2, [128, 4, 64], BF16, "vst4")
        rhsb_s = per(2, [8, 4, 128], F32, "rhsb")
        Xm_s = per(1, [128, 128], F32, "Xm")
        EB_s = per(1, [128, 128], F32, "EB")
        Ct_s = per(1, [128, 128], BF16, "Ct")
        Ctb = self.sb([128, 128], BF16, "Ctb")
        wcol_s = per(1, [128, 1], F32, "wcol")
        vh_s = per(1, [128, 64], BF16, "vh"); vst_s = per(1, [128, 64], BF16, "vst")
        gkl = self.sb([16, 128], F32, "gkl")
        la_s = per(2, [64, 128], F32, "la"); bb_s = per(2, [64, 128], F32, "bb")
        eb_s = per(2, [64, 128], F32, "eb"); enb_s = per(2, [64, 128], F32, "enb"); ek_s = per(2, [64, 128], F32, "ek")
        gq_s = per(2, [64, 128], BF16, "gq"); gk__s = per(2, [64, 128], BF16, "gk_"); gkf_s = per(2, [64, 128], F32, "gkf")
        gkh_s = per(2, [64, 128], F32, "gkh"); gkst_s = per(2, [128, 64], BF16, "gkst")
        h_tok = self.sb([128, D], F32, "h_tok")
        h_tokb = self.sb([128, D], BF16, "h_tokb")
        hT = self.sb([128, NCH, 128], BF16, "hTm")
        sq = self.sb([128, 512], F32, "sq")
        st1 = self.sb([128, 8], F32, "st1"); st2 = self.sb([128, 8], F32, "st2"); st3 = self.sb([128, 8], F32, "st3")
        yz = self.sb([128, 512], F32, "yz")
        zc = self.sb([128, NCH, 128], F32, "zc")
        zsq = self.sb([128, NCH, 128], F32, "zsq")
        lm2 = self.sb([128, 128], F32, "lm2"); lrs = self.sb([128, 128], F32, "lrs"); lta = self.sb([128, 128], F32, "lta")
        ones_row = C("ones")
        self.cbT = [self.sb([128, 128], F32, "cbT0"), self.sb([128, 128], F32, "cbT1")]

        GAM = [1.0 - 2.0 ** (-5.0 - h) for h in range(4)]
        po = [self.psum[0], self.psum[1]]

        def o_ap(lo, n):
            b_, l_ = divmod(lo, 512)
            assert l_ + n <= 512
            return po[b_][:, l_:l_ + n]

        def proj_fm(n, off, M):
            qi = self.nextq()
            ps = self.psq(qi)
            for c in range(NCH):
                E("pe", "matmul", ["win", ("xTb", c, n // 4)], [("ps", qi)], out=ps[0:M, :], lhsT=win[:, c, off:off + M],
                  rhs=xTb[:, c, n * 128:(n + 1) * 128], start=(c == 0), stop=(c == NCH - 1))
            return qi, ps[0:M, :]

        def proj_tm(n, bank, groups):
            for (off, w, dst) in groups:
                for c in range(NCH):
                    E("pe", "matmul", ["win", ("xTb", c, n // 4)], [("ps", bank)], out=self.psum[bank][:, dst:dst + w],
                      lhsT=xTb[:, c, n * 128:(n + 1) * 128], rhs=win[:, c, off:off + w], start=(c == 0), stop=(c == NCH - 1))

        import os
        for n in range(int(os.environ.get('MIX_CHUNKS', T // 128))):
            tsl = slice(n * 128, (n + 1) * 128)
            proj_tm(n, 2, [(512, 512, 0)])
            E("act", "activation", [("ps", 2)], ["tmv"], out=tmv[:], in_=self.psum[2][:, 0:256], func=AF.Copy)
            E("act", "activation", [("ps", 2)], ["tmg"], out=tmg[:], in_=self.psum[2][:, 256:512], func=AF.Silu)
            proj_tm(n, 3, [(1024, 512, 0)])
            E("act", "activation", [("ps", 3)], ["tmz"], out=tmz[:], in_=self.psum[3][:], func=AF.Silu)
            proj_tm(n, 4, [(2568, 256, 0), (2840, 256, 256)])
            E("act", "activation", [("ps", 4)], ["tgv"], out=tgv[:], in_=self.psum[4][:, 0:256], func=AF.Copy)
            E("act", "activation", [("ps", 4)], ["tgg"], out=tgg[:], in_=self.psum[4][:, 256:512], func=AF.Silu)

            rcn = rc[n % 2]
            S.add("sp", lambda e, rcn=rcn, tsl=tsl: e.dma_start(out=rcn[:], in_=self.d_rot[:, :, tsl].rearrange("w p t -> p w t")),
                  reads=[("rotd", 0), ("rotd", 1)], writes=[("rc", n % 2)], dma=True)
            def ret_stage(p2):
                t1, t2, rq, rqi, rkb, kst = t1_s[p2], t2_s[p2], rq_s[p2], rqi_s[p2], rkb_s[p2], kst_s[p2]
                for kind, offa, offb in (("q", 128 * p2, 3096 + 128 * p2), ("k", 256 + 128 * p2, 3096 + 256 + 128 * p2)):
                    qa, pa = proj_fm(n, offa, 128)
                    qb, pb = proj_fm(n, offb, 128)
                    E("dve", "tensor_tensor", [("ps", qa), ("rc", n % 2)], [("t1", p2)], out=t1[:], in0=pa, in1=rcn[:, 1, :], op=ALU.mult)
                    E("dve", "tensor_tensor", [("ps", qb), ("rc", n % 2)], [("t2", p2)], out=t2[:], in0=pb, in1=rcn[:, 0, :], op=ALU.mult)
                    if kind == "k":
                        E("dve", "tensor_tensor", [("t1", p2), ("t2", p2)], [("rkb", p2)], out=rkb[:], in0=t1[:], in1=t2[:], op=ALU.add)
                    else:
                        E("dve", "tensor_tensor", [("t1", p2), ("t2", p2)], [("rq", p2)], out=rq[:], in0=t1[:], in1=t2[:], op=ALU.add)
                        E("dve", "tensor_tensor", [("t1", p2), ("t2", p2)], [("t1", p2)], out=t1[:], in0=t1[:], in1=t2[:], op=ALU.add)
                        E("dve", "tensor_tensor", [("t1", p2), "cst"], [("rqi", p2)], out=rqi[:], in0=t1[:], in1=C("gq2")[:, p2 * 128:(p2 + 1) * 128], op=ALU.mult)
                qt = self.nextq()
                pbf = self.psum[qt][:].bitcast(BF16)
                E("pe", "transpose", [("rkb", p2), "identb"], [("ps", qt)], out=pbf[:, 0:128], in_=rkb[:], identity=self.identb[:])
                for j in range(2):
                    h = 2 * p2 + j
                    E("dve", "tensor_scalar", [("ps", qt), "cst"], [("kst", p2, j)], out=kst[:, 64 * j:64 * j + 64], in0=pbf[:, 64 * j:64 * j + 64],
                      scalar1=C("gk")[:, h:h + 1], scalar2=None, op0=ALU.mult)
                def stage_b():
                    for j in range(2):
                        h = 2 * p2 + j
                        hs = slice(64 * j, 64 * j + 64)
                        PT = PTr_s[h]
                        qs = self.nextq(); pss = self.psq(qs)
                        E("pe", "matmul", [("rkb", p2), ("rq", p2)], [("ps", qs)], out=pss, lhsT=rkb[hs, :], rhs=rq[hs, :], start=True, stop=True)
                        E("dve", "tensor_tensor", [("ps", qs), "cst"], [("PTr", h)], out=PT[:], in0=pss, in1=C("retmask")[:, h * 128:(h + 1) * 128], op=ALU.mult)
                        E("pe", "matmul", [("PTr", h), "tmv"], [("ps", 0)], out=o_ap(64 * h, 64), lhsT=PT[:], rhs=tmv[:, 64 * h:64 * h + 64], start=True, stop=False)
                        E("pe", "matmul", [("rqi", p2), ("Sretb", h)], [("ps", 0)], out=o_ap(64 * h, 64), lhsT=rqi[hs, :], rhs=Sretb[hs, p2, :], start=False, stop=True)
                    qu = self.nextq(); psu = self.psq(qu)
                    E("pe", "matmul", [("kst", p2, 0), ("kst", p2, 1), "tmv"], [("ps", qu)], out=psu, lhsT=kst[:], rhs=tmv[:, 128 * p2:128 * p2 + 128], start=True, stop=True)
                    for j in range(2):
                        h = 2 * p2 + j
                        hs = slice(64 * j, 64 * j + 64)
                        E("dve", "scalar_tensor_tensor", [("Sret", h), ("ps", qu)], [("Sret", h)], out=Sret[hs, p2, :], in0=Sret[hs, p2, :],
                          scalar=float(GAM[h] ** 128), in1=psu[hs, 64 * j:64 * j + 64], op0=ALU.mult, op1=ALU.add)
                        E("act", "activation", [("Sret", h)], [("Sretb", h)], out=Sretb[hs, p2, :], in_=Sret[hs, p2, :], func=AF.Copy)
                return stage_b

            pend = [ret_stage(0), ret_stage(1)]
            while pend:
                pend.pop(0)()

            for r in range(6):
                qx, px = proj_fm(n, 1536 + 128 * r, 128)
                E("dve", "tensor_copy", [("raw", r)], [("raw", r)], out=raw[:, r, 0:3], in_=raw[:, r, 128:131])
                E("act", "activation", [("ps", qx)], [("raw", r)], out=raw[:, r, 3:131], in_=px, func=AF.Copy)
                cw = P("conv_w")
                E("dve", "tensor_scalar", [("raw", r), "mp"], ["cacc"], out=cacc[:], in0=raw[:, r, 0:128], scalar1=cw[:, 4 * r:4 * r + 1], scalar2=None, op0=ALU.mult)
                for j in range(1, 4):
                    E("dve", "scalar_tensor_tensor", [("raw", r), "mp", "cacc"], ["cacc"], out=cacc[:], in0=raw[:, r, j:j + 128],
                      scalar=cw[:, 4 * r + j:4 * r + j + 1], in1=cacc[:], op0=ALU.mult, op1=ALU.add)
                dst = xsT[:, r, :] if r < 4 else (BT[:] if r == 4 else CT[:])
                dres = ("xsT", r) if r < 4 else ("BT" if r == 4 else "CT")
                E("act", "activation", ["cacc", "mp"], [dres], out=dst, in_=cacc[:], func=AF.Silu, bias=P("conv_b")[:, r:r + 1], scale=1.0)
            E("act", "activation", ["BT"], ["BTb"], out=BTb[:], in_=BT[:], func=AF.Copy)
            E("act", "activation", ["CT"], ["Ctb"], out=Ctb[:], in_=CT[:], func=AF.Copy)
            for r in range(5):
                qt = self.nextq(); pst = self.psq(qt)
                tsrc = xsT[:, r, :] if r < 4 else BT[:]
                sres = ("xsT", r) if r < 4 else "BT"
                E("pe", "transpose", [sres, "cst"], [("ps", qt)], out=pst, in_=tsrc, identity=ident)
                if r < 4:
                    E("act", "activation", [("ps", qt)], [("xs_tok", 2 * r), ("xs_tok", 2 * r + 1)], out=xs_tok[:, 2 * r:2 * r + 2, :],
                      in_=pst.rearrange("p (h d) -> p h d", h=2), func=AF.Copy)
                else:
                    E("act", "activation", [("ps", qt)], [("B_tok", 0), ("B_tok", 1)], out=B_tok[:], in_=pst.rearrange("p (h d) -> p h d", h=2), func=AF.Copy)
            qd, pd = proj_fm(n, 2304, 8)
            E("act", "activation", [("ps", qd), "mp"], ["dtT"], out=dtT[:], in_=pd, func=AF.Exp, bias=P("dt_bias")[0:8, 0:1], scale=1.0)
            E("act", "activation", ["dtT"], ["dtT"], out=dtT[:], in_=dtT[:], func=AF.Ln, bias=1.0, scale=1.0)
            E("dve", "tensor_scalar", ["dtT", "negA"], ["aT"], out=aT[:], in0=dtT[:], scalar1=negA[:, 0:1], scalar2=None, op0=ALU.mult)
            E("dve", "tensor_tensor_scan", ["aT", "cst"], ["acT"], out=acT[:], data0=ones_row[0:8, 0:128], data1=aT[:], initial=0.0, op0=ALU.mult, op1=ALU.add)
            for (srcT, dstt, nm) in ((dtT, dt_tok, "dt_tok"), (acT, ac_tok, "ac_tok")):
                qt = self.nextq(); pst = self.psq(qt)
                E("pe", "transpose", ["dtT" if srcT is dtT else "acT", "cst"], [("ps", qt)], out=pst[:, 0:8], in_=srcT[:], identity=ident[0:8, 0:8])
                E("act", "activation", [("ps", qt)], [nm], out=dstt[:], in_=pst[:, 0:8], func=AF.Copy)
            for g in range(2):
                gs = slice(64 * g, 64 * g + 64)
                qc = self.nextq(); pc = self.psq(qc)
                E("pe", "matmul", ["BTb", "Ctb"], [("ps", qc)], out=pc, lhsT=BTb[gs, :], rhs=Ctb[gs, :], start=True, stop=True)
                cbT = self.cbT[g]
                E("act", "activation", [("ps", qc)], [("cbT", g)], out=cbT[:], in_=pc, func=AF.Copy)
            def ssd_stage(g):
                gs = slice(64 * g, 64 * g + 64)
                Xm4, PT4, EB4, Ct4, wc4, vh4, vst4, rhsb = Xm4_s[g], PT4_s[g], EB4_s[g], Ct4_s[g], wc4_s[g], vh4_s[g], vst4_s[g], rhsb_s[g]
                acg = ac_tok[:, 4 * g:4 * g + 4].rearrange("p (j o) -> p j o", o=1)
                E("dve", "tensor_tensor", ["acT", "cst"], [("rhsb", g)], out=rhsb[:], in0=acT[:].rearrange("k (o c) -> k o c", o=1).to_broadcast([8, 4, 128]),
                  in1=C("selm")[0:8, g * 512:(g + 1) * 512].rearrange("k (j c) -> k j c", j=4), op=ALU.mult)
                qa = self.nextq()
                pab = self.psum[qa][:].rearrange("p (j c) -> p j c", j=4)
                E("pe", "matmul", ["cst", ("rhsb", g)], [("ps", qa)], out=self.psum[qa][:], lhsT=ones_row[0:8, 0:128], rhs=rhsb[:].rearrange("k j c -> k (j c)"), start=True, stop=True)
                E("dve", "tensor_tensor", [("ps", qa), "ac_tok"], [("Xm4", g)], out=Xm4[:], in0=pab, in1=acg.to_broadcast([128, 4, 128]), op=ALU.subtract)
                E("dve", "tensor_scalar", [("Xm4", g)], [("Xm4", g)], out=Xm4[:], in0=Xm4[:], scalar1=0.0, scalar2=None, op0=ALU.min)
                E("act", "activation", [("Xm4", g)], [("Xm4", g)], out=Xm4[:], in_=Xm4[:], func=AF.Exp)
                E("dve", "tensor_tensor", [("Xm4", g), "cst"], [("Xm4", g)], out=Xm4[:], in0=Xm4[:],
                  in1=C("causal").rearrange("p (o c) -> p o c", o=1).to_broadcast([128, 4, 128]), op=ALU.mult)
                E("dve", "tensor_tensor", [("Xm4", g), ("cbT", g)], [("PT4", g)], out=PT4[:], in0=Xm4[:],
                  in1=self.cbT[g][:].rearrange("p (o c) -> p o c", o=1).to_broadcast([128, 4, 128]), op=ALU.mult)
                E("act", "activation", [("ps", qa)], [("EB4", g)], out=EB4[gs, :, :], in_=pab[gs, :, :], func=AF.Exp)
                E("dve", "tensor_tensor", ["CT", ("EB4", g)], [("Ct4", g)], out=Ct4[gs, :, :],
                  in0=CT[gs, :].rearrange("p (o c) -> p o c", o=1).to_broadcast([64, 4, 128]), in1=EB4[gs, :, :], op=ALU.mult)
                E("dve", "tensor_tensor", [("ps", qa), "ac_tok"], [("wc4", g)], out=wc4[:].rearrange("p (j o) -> p j o", o=1), in0=pab[:, :, 127:128], in1=acg, op=ALU.subtract)
                E("act", "activation", [("wc4", g)], [("wc4", g)], out=wc4[:], in_=wc4[:], func=AF.Exp)
                E("dve", "tensor_tensor", [("xs_tok", 4 * g + j) for j in range(4)] + ["dt_tok"], [("vh4", g)], out=vh4[:], in0=xs_tok[:, 4 * g:4 * g + 4, :],
                  in1=dt_tok[:, 4 * g:4 * g + 4].rearrange("p (j o) -> p j o", o=1).to_broadcast([128, 4, 64]), op=ALU.mult)
                E("dve", "tensor_tensor", [("vh4", g), ("wc4", g)], [("vst4", g)], out=vst4[:], in0=vh4[:],
                  in1=wc4[:].rearrange("p (j o) -> p j o", o=1).to_broadcast([128, 4, 64]), op=ALU.mult)
                def stage_b():
                    for j in range(4):
                        h = 4 * g + j
                        E("pe", "matmul", [("PT4", g), ("vh4", g)], [("ps", 1 if h >= 4 else 0)], out=o_ap(256 + 64 * h, 64), lhsT=PT4[:, j, :], rhs=vh4[:, j, :], start=True, stop=False)
                        E("pe", "matmul", [("Ct4", g), ("Sssdb", h)], [("ps", 1 if h >= 4 else 0)], out=o_ap(256 + 64 * h, 64), lhsT=Ct4[gs, j, :], rhs=Sssdb[gs, j, :], start=False, stop=True)
                        qu = self.nextq(); psu = self.psq(qu)
                        E("pe", "matmul", [("B_tok", 0), ("B_tok", 1), ("vst4", g)], [("ps", qu)], out=psu[:, 0:64], lhsT=B_tok[:].rearrange("p g k -> p (g k)"), rhs=vst4[:, j, :], start=True, stop=True)
                        E("dve", "scalar_tensor_tensor", [("Sssd", h), ("ps", qu), ("EB4", g)], [("Sssd", h)], out=Sssd[gs, j, :], in0=Sssd[gs, j, :],
                          scalar=EB4[gs, j, 127:128], in1=psu[gs, 0:64], op0=ALU.mult, op1=ALU.add)
                        E("act", "activation", [("Sssd", h)], [("Sssdb", h)], out=Sssdb[gs, j, :], in_=Sssd[gs, j, :], func=AF.Copy)
                return stage_b

            pend = [ssd_stage(0), ssd_stage(1)]
            while pend:
                pend.pop(0)()

            qg, pg = proj_fm(n, 2824, 16)
            E("act", "activation", [("ps", qg)], ["gkl"], out=gkl[:], in_=pg, func=AF.Copy)
            def gla_stage(p2):
                la, bb, eb, enb, ek, gq, gk_, gkf, gkh, gkst = la_s[p2], bb_s[p2], eb_s[p2], enb_s[p2], ek_s[p2], gq_s[p2], gk__s[p2], gkf_s[p2], gkh_s[p2], gkst_s[p2]
                qk, pk = proj_fm(n, 2440 + 64 * p2, 64)
                E("act", "activation", [("ps", qk)], [("gkf", p2)], out=gkf[:], in_=pk, func=AF.Copy)
                qq, pq = proj_fm(n, 2312 + 64 * p2, 64)
                ql = self.nextq(); pl = self.psq(ql)
                E("pe", "matmul", ["wgk", "gkl"], [("ps", ql)], out=pl[0:64, :], lhsT=wgk[:, 64 * p2:64 * p2 + 64], rhs=gkl[:], start=True, stop=True)
                E("act", "activation", [("ps", ql), "nbgk"], [("la", p2)], out=la[:], in_=pl[0:64, :], func=AF.Exp, bias=nbgk[:, p2:p2 + 1], scale=-1.0)
                E("act", "activation", [("la", p2)], [("la", p2)], out=la[:], in_=la[:], func=AF.Ln, bias=1.0, scale=1.0)
                E("dve", "tensor_scalar", [("la", p2)], [("la", p2)], out=la[:], in0=la[:], scalar1=-1.0 / 16.0, scalar2=None, op0=ALU.mult)
                E("dve", "tensor_tensor_scan", [("la", p2), "cst"], [("bb", p2)], out=bb[:], data0=ones_row[0:64, 0:128], data1=la[:], initial=0.0, op0=ALU.mult, op1=ALU.add)
                E("act", "activation", [("bb", p2)], [("eb", p2)], out=eb[:], in_=bb[:], func=AF.Exp)
                E("act", "activation", [("bb", p2)], [("enb", p2)], out=enb[:], in_=bb[:], func=AF.Exp, scale=-1.0)
                E("act", "activation", [("bb", p2)], [("ek", p2)], out=ek[:], in_=bb[:], func=AF.Exp, scale=-1.0, bias=bb[:, 127:128])
                E("dve", "scalar_tensor_tensor", [("ps", qq), ("eb", p2)], [("gq", p2)], out=gq[:], in0=pq, scalar=float(32 ** -0.5), in1=eb[:], op0=ALU.mult, op1=ALU.mult)
                E("dve", "tensor_tensor", [("gkf", p2), ("enb", p2)], [("gk_", p2)], out=gk_[:], in0=gkf[:], in1=enb[:], op=ALU.mult)
                E("dve", "tensor_tensor", [("gkf", p2), ("ek", p2)], [("gkh", p2)], out=gkh[:], in0=gkf[:], in1=ek[:], op=ALU.mult)
                def stage_b():
                    for j in range(2):
                        h = 2 * p2 + j
                        hs = slice(32 * j, 32 * j + 32)
                        PT = PTg_s[j]
                        qs = self.nextq(); pss = self.psq(qs)
                        E("pe", "matmul", [("gk_", p2), ("gq", p2)], [("ps", qs)], out=pss, lhsT=gk_[hs, :], rhs=gq[hs, :], start=True, stop=True)
                        E("dve", "tensor_tensor", [("ps", qs), "cst"], [("PTg", j)], out=PT[:], in0=pss, in1=C("causal"), op=ALU.mult)
                        E("pe", "matmul", [("PTg", j), "tgv"], [("ps", 1)], out=o_ap(768 + 64 * h, 64), lhsT=PT[:], rhs=tgv[:, 64 * h:64 * h + 64], start=True, stop=False)
                        E("pe", "matmul", [("gq", p2), ("Sglab", h)], [("ps", 1)], out=o_ap(768 + 64 * h, 64), lhsT=gq[hs, :], rhs=Sglab[hs, p2, :], start=False, stop=True)
                    qt = self.nextq(); pst = self.psq(qt)
                    E("pe", "transpose", [("gkh", p2), "cst"], [("ps", qt)], out=pst[:, 0:64], in_=gkh[:], identity=ident[0:64, 0:64])
                    E("act", "activation", [("ps", qt)], [("gkst", p2)], out=gkst[:], in_=pst[:, 0:64], func=AF.Copy)
                    qu = self.nextq(); psu = self.psq(qu)
                    E("pe", "matmul", [("gkst", p2), "tgv"], [("ps", qu)], out=psu[0:64, 0:128], lhsT=gkst[:], rhs=tgv[:, 128 * p2:128 * p2 + 128], start=True, stop=True)
                    for j in range(2):
                        h = 2 * p2 + j
                        hs = slice(32 * j, 32 * j + 32)
                        E("dve", "scalar_tensor_tensor", [("Sgla", h), ("ps", qu), ("eb", p2)], [("Sgla", h)], out=Sgla[hs, p2, :], in0=Sgla[hs, p2, :],
                          scalar=eb[hs, 127:128], in1=psu[hs, 64 * j:64 * j + 64], op0=ALU.mult, op1=ALU.add)
                    E("act", "activation", [("Sgla", 2 * p2), ("Sgla", 2 * p2 + 1)], [("Sglab", 2 * p2), ("Sglab", 2 * p2 + 1)], out=Sglab[:, p2, :], in_=Sgla[:, p2, :], func=AF.Copy)
                return stage_b

            pend = [gla_stage(0), gla_stage(1)]
            while pend:
                pend.pop(0)()

            oret = po[0][:, 0:256]
            E("dve", "tensor_reduce", [("ps", 0)], ["st1"], out=st1[:, 0:4], in_=oret.rearrange("p (h d) -> p h d", h=4), axis=AX.X, op=ALU.add)
            E("act", "activation", [("ps", 0)], ["sq"], out=sq[:, 0:256], in_=oret, func=AF.Square)
            E("dve", "tensor_reduce", ["sq"], ["st2"], out=st2[:, 0:4], in_=sq[:, 0:256].rearrange("p (h d) -> p h d", h=4), axis=AX.X, op=ALU.add)
            E("dve", "tensor_scalar", ["st1"], ["st1"], out=st1[:, 0:4], in0=st1[:, 0:4], scalar1=1.0 / 64, scalar2=None, op0=ALU.mult)
            E("dve", "tensor_tensor", ["st1"], ["st3"], out=st3[:, 0:4], in0=st1[:, 0:4], in1=st1[:, 0:4], op=ALU.mult)
            E("dve", "scalar_tensor_tensor", ["st2", "st3"], ["st2"], out=st2[:, 0:4], in0=st2[:, 0:4], scalar=1.0 / 64, in1=st3[:, 0:4], op0=ALU.mult, op1=ALU.subtract)
            E("dve", "tensor_scalar", ["st2"], ["st2"], out=st2[:, 0:4], in0=st2[:, 0:4], scalar1=LN_EPS, scalar2=None, op0=ALU.add)
            E("act", "activation", ["st2"], ["st2"], out=st2[:, 0:4], in_=st2[:, 0:4], func=AF.Sqrt)
            E("dve", "reciprocal", ["st2"], ["st2"], out=st2[:, 0:4], in_=st2[:, 0:4])
            hr = h_tok[:, 0:256].rearrange("p (h d) -> p h d", h=4)
            E("dve", "tensor_tensor", [("ps", 0), "st1"], ["h_ret"], out=hr, in0=oret.rearrange("p (h d) -> p h d", h=4),
              in1=st1[:, 0:4].to_broadcast([128, 4, 64]) if False else st1[:, 0:4].rearrange("p (h o) -> p h o", o=1).to_broadcast([128, 4, 64]), op=ALU.subtract)
            E("dve", "tensor_tensor", ["h_ret", "st2"], ["h_ret"], out=hr, in0=hr, in1=st2[:, 0:4].rearrange("p (h o) -> p h o", o=1).to_broadcast([128, 4, 64]), op=ALU.mult)
            E("dve", "tensor_tensor", ["h_ret", "mp"], ["h_ret"], out=h_tok[:, 0:256], in0=h_tok[:, 0:256], in1=P("ret_nw"), op=ALU.mult)
            E("dve", "tensor_tensor", ["h_ret", "tmg"], ["hb_ret"], out=h_tokb[:, 0:256], in0=h_tok[:, 0:256], in1=tmg[:], op=ALU.mult)
            xs3 = xs_tok[:]
            E("dve", "tensor_tensor", [("xs_tok", r) for r in range(8)] + ["mp"], ["yz"], out=yz[:].rearrange("p (h d) -> p h d", h=8), in0=xs3,
              in1=P("ssd_d").rearrange("p (h o) -> p h o", o=1).to_broadcast([128, 8, 64]), op=ALU.mult)
            E("dve", "tensor_tensor", ["yz", ("ps", 0)], ["yz"], out=yz[:, 0:256], in0=yz[:, 0:256], in1=po[0][:, 256:512], op=ALU.add)
            E("dve", "tensor_tensor", ["yz", ("ps", 1)], ["yz"], out=yz[:, 256:512], in0=yz[:, 256:512], in1=po[1][:, 0:256], op=ALU.add)
            E("dve", "tensor_tensor", ["yz", "tmz"], ["yz"], out=yz[:], in0=yz[:], in1=tmz[:], op=ALU.mult)
            E("act", "activation", ["yz"], ["sq"], out=sq[:], in_=yz[:], func=AF.Square)
            E("dve", "tensor_reduce", ["sq"], ["st2"], out=st2[:, 0:2], in_=sq[:].rearrange("p (g d) -> p g d", g=2), axis=AX.X, op=ALU.add)
            E("dve", "tensor_scalar", ["st2"], ["st2"], out=st2[:, 0:2], in0=st2[:, 0:2], scalar1=1.0 / 256, scalar2=NORM_EPS, op0=ALU.mult, op1=ALU.add)
            E("act", "activation", ["st2"], ["st2"], out=st2[:, 0:2], in_=st2[:, 0:2], func=AF.Sqrt)
            E("dve", "reciprocal", ["st2"], ["st2"], out=st2[:, 0:2], in_=st2[:, 0:2])
            hs = h_tok[:, 256:768].rearrange("p (g d) -> p g d", g=2)
            E("dve", "tensor_tensor", ["yz", "st2"], ["h_ssd"], out=hs, in0=yz[:].rearrange("p (g d) -> p g d", g=2),
              in1=st2[:, 0:2].rearrange("p (g o) -> p g o", o=1).to_broadcast([128, 2, 256]), op=ALU.mult)
            E("dve", "tensor_tensor", ["h_ssd", "mp"], ["hb_ssd"], out=h_tokb[:, 256:768], in0=h_tok[:, 256:768], in1=P("ssd_nw"), op=ALU.mult)
            ogl = po[1][:, 256:512]
            E("act", "activation", [("ps", 1)], ["sq"], out=sq[:, 0:256], in_=ogl, func=AF.Square)
            E("dve", "tensor_reduce", ["sq"], ["st2"], out=st2[:, 0:4], in_=sq[:, 0:256].rearrange("p (h d) -> p h d", h=4), axis=AX.X, op=ALU.add)
            E("dve", "tensor_scalar", ["st2"], ["st2"], out=st2[:, 0:4], in0=st2[:, 0:4], scalar1=1.0 / 64, scalar2=NORM_EPS, op0=ALU.mult, op1=ALU.add)
            E("act", "activation", ["st2"], ["st2"], out=st2[:, 0:4], in_=st2[:, 0:4], func=AF.Sqrt)
            E("dve", "reciprocal", ["st2"], ["st2"], out=st2[:, 0:4], in_=st2[:, 0:4])
            hg = h_tok[:, 768:1024].rearrange("p (h d) -> p h d", h=4)
            E("dve", "tensor_tensor", [("ps", 1), "st2"], ["h_gla"], out=hg, in0=ogl.rearrange("p (h d) -> p h d", h=4),
              in1=st2[:, 0:4].rearrange("p (h o) -> p h o", o=1).to_broadcast([128, 4, 64]), op=ALU.mult)
            E("dve", "tensor_tensor", ["h_gla", "mp"], ["h_gla"], out=h_tok[:, 768:1024], in0=h_tok[:, 768:1024], in1=P("gla_nw"), op=ALU.mult)
            E("dve", "tensor_tensor", ["h_gla", "tgg"], ["hb_gla"], out=h_tokb[:, 768:1024], in0=h_tok[:, 768:1024], in1=tgg[:], op=ALU.mult)

            for ec in range(NCH):
                hres = "hb_ret" if ec < 2 else ("hb_ssd" if ec < 6 else "hb_gla")
                qt = self.nextq()
                pbf_ = self.psum[qt][:].bitcast(BF16)
                E("pe", "transpose", [hres, "identb"], [("ps", qt)], out=pbf_[:, 0:128], in_=h_tokb[:, ec * 128:(ec + 1) * 128], identity=self.identb[:])
                E("act", "activation", [("ps", qt)], [("hT", ec)], out=hT[:, ec, :], in_=pbf_[:, 0:128], func=AF.Copy)
            S.add("sp", lambda e, tsl=tsl: e.dma_start(out=zc[:], in_=xsrc[:, tsl].rearrange("(c p) t -> p c t", p=128)),
                  reads=[("dram", srcres, c) for c in range(NCH)], writes=[("zc", c) for c in range(NCH)], dma=True)
            for half, (mt, mres) in enumerate(((sq, "sq"), (yz, "yz"))):
                bk = 2 + half
                pmt = self.psum[bk]
                for ec in range(NCH):
                    E("pe", "matmul", ["wout", ("hT", ec)], [("ps", bk)], out=pmt[:], lhsT=hT[:, ec, :], rhs=wout[:, ec, half * 512:(half + 1) * 512],
                      start=(ec == 0), stop=(ec == NCH - 1))
                E("act", "activation", [("ps", bk)], [mres], out=mt[:], in_=pmt[:], func=AF.Copy)
            for dc in range(NCH):
                mt, mres = (sq, "sq") if dc < 4 else (yz, "yz")
                qm = self.nextq(); pm = self.psq(qm)
                E("pe", "transpose", [mres, "cst"], [("ps", qm)], out=pm, in_=mt[:, (dc % 4) * 128:(dc % 4 + 1) * 128], identity=ident)
                E("dve", "scalar_tensor_tensor", [("zc", dc), ("ps", qm)], [("zc", dc)], out=zc[:, dc, :], in0=zc[:, dc, :], scalar=ALPHA, in1=pm, op0=ALU.mult, op1=ALU.add)
            onesm = C("onesm")
            qm_ = self.nextq(); pmn = self.psq(qm_)
            qq_ = self.nextq(); pqq = self.psq(qq_)
            for c in range(NCH):
                E("act", "activation", [("zc", c)], [("zsq", c)], out=zsq[:, c, :], in_=zc[:, c, :], func=AF.Square)
            for c in range(NCH):
                E("pe", "matmul", [("zc", c), "cst"], [("ps", qm_)], out=pmn, lhsT=onesm, rhs=zc[:, c, :], start=(c == 0), stop=(c == NCH - 1))
            for c in range(NCH):
                E("pe", "matmul", [("zsq", c), "cst"], [("ps", qq_)], out=pqq, lhsT=onesm, rhs=zsq[:, c, :], start=(c == 0), stop=(c == NCH - 1))
            E("act", "activation", [("ps", qm_)], ["lm2"], out=lm2[:], in_=pmn, func=AF.Square)
            E("dve", "tensor_tensor", [("ps", qq_), "lm2"], ["lrs"], out=lrs[:], in0=pqq, in1=lm2[:], op=ALU.subtract)
            E("dve", "tensor_scalar", ["lrs"], ["lrs"], out=lrs[:], in0=lrs[:], scalar1=LN_EPS, scalar2=None, op0=ALU.add)
            E("act", "activation", ["lrs"], ["lrs"], out=lrs[:], in_=lrs[:], func=AF.Sqrt)
            E("dve", "reciprocal", ["lrs"], ["lrs"], out=lrs[:], in_=lrs[:])
            for c in range(NCH):
                E("dve", "tensor_tensor", [("zc", c), ("ps", qm_)], ["lta"], out=lta[:], in0=zc[:, c, :], in1=pmn, op=ALU.subtract)
                E("dve", "tensor_tensor", ["lta", "lrs"], ["lta"], out=lta[:], in0=lta[:], in1=lrs[:], op=ALU.mult)
                E("act", "activation", ["lta", "mp"], [("zc", c)], out=zc[:, c, :], in_=lta[:], func=AF.Identity,
                  scale=P("ln1_g")[:, c:c + 1], bias=P("ln1_b")[:, c:c + 1])
            S.add("sp", lambda e, tsl=tsl: e.dma_start(out=xdst[:, tsl].rearrange("(c p) t -> p c t", p=128), in_=zc[:]),
                  reads=[("zc", c) for c in range(NCH)], writes=[("dramw", dstres, n)], dma=True)

    def layer_norm_fm(self, gcol, bcol, ptile, pcols, tmp_sq, tmp_a, tmp_b, stat_m2, stat_rs, tag):
        S = self.S
        onesm = self.col(self.cst, self.const_cols, "onesm")
        for g in range(NTG):
            sl = slice(g * TG, (g + 1) * TG)
            ps_m, ps_q = self.psum[6], self.psum[7]
            for c in range(NCH):
                S.add("act", lambda e, c=c, sl=sl: e.activation(out=tmp_sq[:, c, :], in_=self.xT[:, c, sl], func=AF.Square),
                      reads=[("xT", c, g)], writes=[(tag + "sq", c)])
            for c in range(NCH):
                S.add("pe", lambda e, c=c, sl=sl: e.matmul(ps_m[:], lhsT=onesm, rhs=self.xT[:, c, sl],
                                                            start=(c == 0), stop=(c == NCH - 1)),
                      reads=[("xT", c, g), "cst"], writes=[("ps", 6)])
            for c in range(NCH):
                S.add("pe", lambda e, c=c: e.matmul(ps_q[:], lhsT=onesm, rhs=tmp_sq[:, c, :],
                                                    start=(c == 0), stop=(c == NCH - 1)),
                      reads=[(tag + "sq", c), "cst"], writes=[("ps", 7)])
            S.add("act", lambda e: e.activation(out=stat_m2[:], in_=ps_m[:], func=AF.Square),
                  reads=[("ps", 6)], writes=[tag + "m2"])
            S.add("dve", lambda e: e.tensor_tensor(out=stat_rs[:], in0=ps_q[:], in1=stat_m2[:], op=ALU.subtract),
                  reads=[("ps", 7), tag + "m2"], writes=[tag + "rs"])
            S.add("dve", lambda e: e.tensor_scalar(out=stat_rs[:], in0=stat_rs[:], scalar1=LN_EPS, scalar2=None, op0=ALU.add),
                  reads=[tag + "rs"], writes=[tag + "rs"])
            S.add("act", lambda e: e.activation(out=stat_rs[:], in_=stat_rs[:], func=AF.Sqrt),
                  reads=[tag + "rs"], writes=[tag + "rs"])
            S.add("dve", lambda e: e.reciprocal(out=stat_rs[:], in_=stat_rs[:]),
                  reads=[tag + "rs"], writes=[tag + "rs"])
            for c in range(NCH):
                ta = tmp_a[c % 2]
                S.add("dve", lambda e, c=c, sl=sl, ta=ta: e.tensor_tensor(out=ta[:], in0=self.xT[:, c, sl], in1=ps_m[:], op=ALU.subtract),
                      reads=[("xT", c, g), ("ps", 6)], writes=[(tag + "ta", c % 2)])
                S.add("dve", lambda e, ta=ta: e.tensor_tensor(out=ta[:], in0=ta[:], in1=stat_rs[:], op=ALU.mult),
                      reads=[(tag + "ta", c % 2), tag + "rs"], writes=[(tag + "ta", c % 2)])
                gs = self.col(ptile, pcols, gcol, c, 1)
                bs = self.col(ptile, pcols, bcol, c, 1)
                S.add("act", lambda e, c=c, sl=sl, ta=ta, gs=gs, bs=bs: e.activation(
                    out=self.xT[:, c, sl], in_=ta[:], func=AF.Identity, scale=gs, bias=bs),
                    reads=[(tag + "ta", c % 2), tag + "p"], writes=[("xT", c, g)])

    def moe_layer(self, l, xsrc, srcres, xdst, dstres, last):
        nc, S, E = self.nc, self.S, self.E
        mc = self.moe_cols
        nmc = max(v[0] + v[1] for v in mc.values())
        tag = f"m{l}"
        Xd, Yd = self.Xd[l], self.Yd[l]
        self.xT = self.sb_cached("xT", [128, NCH, T], F32)
        for c in range(NCH):
            S.add("sp", lambda e, c=c: e.dma_start(out=self.xT[:, c, :], in_=xsrc[c * 128:(c + 1) * 128, :]),
                  reads=[("dramw", srcres, n) for n in range(T // 128)], writes=[("xT", c, g) for g in range(NTG)], dma=True)
        mp = self.sb([128, nmc], F32, "moep")
        S.add("sp", lambda e: e.dma_start(out=mp[:], in_=self.d_moep[l]), writes=[tag + "p"], dma=True)
        m_after_mp = self.mark()
        C = lambda name, lo=0, n=None: self.col(self.cst, self.const_cols, name, lo, n)
        ident, ones = C("ident"), C("ones")
        NT = T // 128
        GT = self.sb([32, T], F32, "GT")
        idx4 = self.sb([128, NT, 4], I32, "idx4")
        g4 = self.sb([128, NT, 4], F32, "g4")
        bar = self.sb([1, 8], F32, "bar")
        m_keep = self.mark()
        tot = self.sb([128, NE], F32, "tot")
        lg = self.sb([128, NE], F32, "lg"); m8 = self.sb([128, 8], F32, "m8"); nmx = self.sb([128, 1], F32, "nmx")
        msk = self.sb([128, NE], F32, "msk"); ex = self.sb([128, NE], F32, "ex"); ssum = self.sb([128, 1], F32, "ssum")
        gts = self.sb([128, NE], F32, "gts"); ptt = self.sb([128, NE], F32, "ptt"); okk = self.sb([128, NE], F32, "okk")
        s1 = self.sb([128, NE], F32, "s1"); m8b = self.sb([128, 8], F32, "m8b"); eq4 = self.sb([128, 4, NE], F32, "eq4")
        x1f = [self.sb([128, D], BF16, "x1f") for _ in range(2)]
        wr = self.col(mp, mc, "w_router"); br = self.col(mp, mc, "b_router")
        E("dve", "memset", [], [tag + "tot"], ap=tot[:], constant=0.0)
        for i in range(NT):
            ts_ = slice(i * 128, (i + 1) * 128)
            g = i // 4
            ps = self.psum[i % 2]
            for c in range(NCH):
                E("pe", "matmul", [("xT", c, g), tag + "p"], [("ps", i % 2)], out=ps[:, 0:NE], lhsT=self.xT[:, c, ts_],
                  rhs=wr[:, c * NE:(c + 1) * NE], start=(c == 0), stop=(c == NCH - 1))
            E("dve", "tensor_tensor", [("ps", i % 2), tag + "p"], [tag + "lg"], out=lg[:], in0=ps[:, 0:NE], in1=br, op=ALU.add)
            E("dve", "max", [tag + "lg"], [tag + "m8"], out=m8[:], in_=lg[:])
            E("dve", "tensor_scalar", [tag + "lg", tag + "m8"], [tag + "msk"], out=msk[:], in0=lg[:], scalar1=m8[:, 3:4], scalar2=None, op0=ALU.is_ge)
            E("dve", "tensor_scalar", [tag + "m8"], [tag + "nmx"], out=nmx[:], in0=m8[:, 0:1], scalar1=-1.0, scalar2=None, op0=ALU.mult)
            E("act", "activation", [tag + "lg", tag + "nmx"], [tag + "ex"], out=ex[:], in_=lg[:], func=AF.Exp, bias=nmx[:, 0:1], scale=1.0)
            E("dve", "tensor_tensor", [tag + "ex", tag + "msk"], [tag + "ex"], out=ex[:], in0=ex[:], in1=msk[:], op=ALU.mult)
            E("dve", "reduce_sum", [tag + "ex"], [tag + "ssum"], out=ssum[:], in_=ex[:], axis=AX.X)
            E("dve", "reciprocal", [tag + "ssum"], [tag + "ssum"], out=ssum[:], in_=ssum[:])
            E("dve", "tensor_scalar", [tag + "ex", tag + "ssum"], [tag + "gts"], out=gts[:], in0=ex[:], scalar1=ssum[:, 0:1], scalar2=None, op0=ALU.mult)
            pt = self.psum[2 + i % 2]
            E("pe", "transpose", [tag + "gts", "cst"], [("ps", 2 + i % 2)], out=pt[0:NE, 0:128], in_=gts[:], identity=ident)
            E("act", "activation", [("ps", 2 + i % 2)], [(tag + "GT", g)], out=GT[:, ts_], in_=pt[0:NE, 0:128], func=AF.Copy)
            pp = self.psum[4]
            E("pe", "matmul", [tag + "msk", "cst"], [("ps", 4)], out=pp[:, 0:NE], lhsT=C("triu"), rhs=msk[:], start=True, stop=True)
            E("dve", "tensor_tensor", [("ps", 4), tag + "tot"], [tag + "ptt"], out=ptt[:], in0=pp[:, 0:NE], in1=tot[:], op=ALU.add)
            pq = self.psum[5]
            E("pe", "matmul", [tag + "msk", "cst"], [("ps", 5)], out=pq[:, 0:NE], lhsT=ones, rhs=msk[:], start=True, stop=True)
            E("dve", "tensor_tensor", [("ps", 5), tag + "tot"], [tag + "tot"], out=tot[:], in0=pq[:, 0:NE], in1=tot[:], op=ALU.add)
            E("dve", "tensor_scalar", [tag + "ptt"], [tag + "okk"], out=okk[:], in0=ptt[:], scalar1=float(CAP), scalar2=None, op0=ALU.is_lt)
            E("dve", "tensor_tensor", [tag + "okk", tag + "msk"], [tag + "okk"], out=okk[:], in0=okk[:], in1=msk[:], op=ALU.mult)
            E("dve", "tensor_tensor", [tag + "ptt", "cst"], [tag + "s1"], out=s1[:], in0=ptt[:], in1=C("ec1"), op=ALU.add)
            E("dve", "tensor_tensor", [tag + "s1", tag + "okk"], [tag + "s1"], out=s1[:], in0=s1[:], in1=okk[:], op=ALU.mult)
            E("dve", "max", [tag + "s1"], [tag + "m8b"], out=m8b[:], in_=s1[:])
            E("dve", "tensor_copy", [tag + "m8b"], [(tag + "idx", i)], out=idx4[:, i, :], in_=m8b[:, 0:4])
            E("dve", "tensor_tensor", [tag + "s1", tag + "m8b"], [tag + "eq4"], out=eq4[:],
              in0=s1[:].rearrange("p (o e) -> p o e", o=1).to_broadcast([128, 4, NE]),
              in1=m8b[:, 0:4].rearrange("p (k o) -> p k o", o=1).to_broadcast([128, 4, NE]), op=ALU.is_equal)
            E("dve", "tensor_tensor", [tag + "eq4", tag + "gts"], [tag + "eq4"], out=eq4[:], in0=eq4[:],
              in1=gts[:].rearrange("p (o e) -> p o e", o=1).to_broadcast([128, 4, NE]), op=ALU.mult)
            E("dve", "tensor_reduce", [tag + "eq4"], [(tag + "g4", i)], out=g4[:, i, :], in_=eq4[:], axis=AX.X, op=ALU.add)
            xf = x1f[i % 2]
            for c in range(NCH):
                bk = 6 + c // 4
                E("pe", "transpose", [("xT", c, g), "cst"], [("ps", bk)], out=self.psum[bk][:, (c % 4) * 128:(c % 4 + 1) * 128],
                  in_=self.xT[:, c, ts_], identity=ident)
            for hb in range(2):
                E("act", "activation", [("ps", 6 + hb)], [(tag + "x1f", i % 2)], out=xf[:, hb * 512:(hb + 1) * 512], in_=self.psum[6 + hb][:], func=AF.Copy)
            for k in range(4):
                S.add("pool", lambda e, xf=xf, i=i, k=k: e.indirect_dma_start(
                    out=Xd, out_offset=bass.IndirectOffsetOnAxis(ap=idx4[:, i, k:k + 1], axis=0), in_=xf[:], in_offset=None),
                    reads=[(tag + "x1f", i % 2), (tag + "idx", i), ("Xdz", l)], writes=[(tag + "Xd", i, k)], dma=True)
        bd = self.col(mp, mc, "b_down")
        for c in range(NCH):
            for g in range(NTG):
                sl = slice(g * TG, (g + 1) * TG)
                bk = (c * NTG + g) % 2
                ps = self.psum[bk]
                E("pe", "matmul", [tag + "p", (tag + "GT", g)], [("ps", bk)], out=ps[:], lhsT=bd[0:NE, c * 128:(c + 1) * 128], rhs=GT[:, sl], start=True, stop=True)
                E("dve", "scalar_tensor_tensor", [("xT", c, g), ("ps", bk)], [("xT", c, g)], out=self.xT[:, c, sl], in0=self.xT[:, c, sl],
                  scalar=ALPHA, in1=ps[:], op0=ALU.mult, op1=ALU.add)
        E("dve", "memset", [(tag + "Xd", i, k) for i in range(NT) for k in range(4)], [tag + "Xd_ready", tag + "bar"], ap=bar[:], constant=0.0)
        NSLOT, QW = 4, 512
        ring = [self.sb([128, NCH, QW], BF16, "wr") for _ in range(NSLOT)]
        xe = self.sb([128, CAP // 128, D], BF16, "xe")
        xeT = self.sb([128, NCH, CAP], BF16, "xeT")
        hT = self.sb([128, NCH, CAP], BF16, "hT")
        ye = [self.sb([128, CAP // 128, D], BF16, "ye") for _ in range(2)]
        tmpg = [self.sb([128, CAP], F32, "tg") for _ in range(2)]
        tmps = [self.sb([128, CAP], BF16, "ts") for _ in range(2)]
        tmpu = [self.sb([128, CAP], BF16, "tu") for _ in range(2)]
        slot_ctr = [0]

        def load_q(dram_w, e_, q):
            s = slot_ctr[0] % NSLOT
            slot_ctr[0] += 1
            wsrc = dram_w[l, e_, :, q * QW:(q + 1) * QW].rearrange("(c p) f -> p c f", p=128)
            S.add("pool", lambda e, s=s, wsrc=wsrc: e.dma_start(out=ring[s][:], in_=wsrc), writes=[(tag + "ring", s)], dma=True)
            return s

        bgc = self.col(mp, mc, "b_gate"); buc = self.col(mp, mc, "b_up")
        import os
        nexp = int(os.environ.get("N_EXPERTS", NE))
        tcnt = 0
        for ex_ in range(nexp):
            r0 = 1 + ex_ * CAP
            S.add("sp", lambda e, r0=r0: e.dma_start(out=xe[:], in_=Xd[r0:r0 + CAP, :].rearrange("(s p) d -> p s d", p=128)),
                  reads=[tag + "Xd_ready"], writes=[tag + "xe"], dma=True)
            for c in range(NCH):
                bk = 6 + c % 2
                pbf = self.psum[bk][:].bitcast(BF16)
                for st in range(CAP // 128):
                    E("pe", "transpose", [tag + "xe", "identb"], [("ps", bk)], out=pbf[:, st * 128:(st + 1) * 128],
                      in_=xe[:, st, c * 128:(c + 1) * 128], identity=self.identb[:])
                E("act", "activation", [("ps", bk)], [(tag + "xeT", c)], out=xeT[:, c, :], in_=pbf[:, 0:CAP], func=AF.Copy)
            sg = su = None
            for f in range(NCH):
                if f % 4 == 0:
                    sg = load_q(self.d_wg, ex_, f // 4)
                    su = load_q(self.d_wu, ex_, f // 4)
                fi = f % 4
                k = tcnt % 2
                tcnt += 1
                pg, pu = self.psum[k], self.psum[2 + k]
                for c in range(NCH):
                    E("pe", "matmul", [(tag + "ring", sg), (tag + "xeT", c)], [("ps", k)], out=pg[:, 0:CAP], lhsT=ring[sg][:, c, fi * 128:(fi + 1) * 128],
                      rhs=xeT[:, c, :], start=(c == 0), stop=(c == NCH - 1))
                for c in range(NCH):
                    E("pe", "matmul", [(tag + "ring", su), (tag + "xeT", c)], [("ps", 2 + k)], out=pu[:, 0:CAP], lhsT=ring[su][:, c, fi * 128:(fi + 1) * 128],
                      rhs=xeT[:, c, :], start=(c == 0), stop=(c == NCH - 1))
                bg1 = bgc[:, ex_ * 8 + f:ex_ * 8 + f + 1]
                bu1 = buc[:, ex_ * 8 + f:ex_ * 8 + f + 1]
                tg_, ts2, tu_ = tmpg[k], tmps[k], tmpu[k]
                E("dve", "tensor_scalar", [("ps", k), tag + "p"], [(tag + "tg", k)], out=tg_[:], in0=pg[:, 0:CAP], scalar1=bg1, scalar2=7.0, op0=ALU.add, op1=ALU.min)
                E("act", "activation", [(tag + "tg", k)], [(tag + "ts", k)], out=ts2[:], in_=tg_[:], func=AF.Sigmoid, scale=1.702)
                E("dve", "tensor_scalar", [("ps", 2 + k), tag + "p"], [(tag + "tu", k)], out=tu_[:], in0=pu[:, 0:CAP], scalar1=bu1, scalar2=7.0, op0=ALU.add, op1=ALU.min)
                E("dve", "tensor_scalar", [(tag + "tu", k)], [(tag + "tu", k)], out=tu_[:], in0=tu_[:], scalar1=-7.0, scalar2=1.0, op0=ALU.max, op1=ALU.add)
                E("dve", "tensor_tensor", [(tag + "tg", k), (tag + "ts", k)], [(tag + "ts", k)], out=ts2[:], in0=tg_[:], in1=ts2[:], op=ALU.mult)
                E("dve", "tensor_tensor", [(tag + "tu", k), (tag + "ts", k)], [(tag + "hT", f)], out=hT[:, f, :], in0=tu_[:], in1=ts2[:], op=ALU.mult)
            yy = ye[ex_ % 2]
            ycnt = 0
            for half in range(2):
                sd = load_q(self.d_wd, ex_, half)
                for st in range(CAP // 128):
                    bk = 4 + ycnt % 2
                    ycnt += 1
                    py = self.psum[bk]
                    for f in range(NCH):
                        E("pe", "matmul", [(tag + "ring", sd), (tag + "hT", f)], [("ps", bk)], out=py[:], lhsT=hT[:, f, st * 128:(st + 1) * 128],
                          rhs=ring[sd][:, f, :], start=(f == 0), stop=(f == NCH - 1))
                    E("act", "activation", [("ps", bk)], [(tag + "ye", ex_ % 2)], out=yy[:, st, half * 512:(half + 1) * 512], in_=py[:], func=AF.Copy)
            S.add("sp", lambda e, r0=r0, yy=yy: e.dma_start(out=Yd[r0:r0 + CAP, :].rearrange("(s p) d -> p s d", p=128), in_=yy[:]),
                  reads=[(tag + "ye", ex_ % 2)], writes=[(tag + "Yd", ex_)], dma=True)
        E("dve", "memset", [(tag + "Yd", e_) for e_ in range(nexp)] + [("Ydz", l)], [tag + "Yd_ready", tag + "bar"], ap=bar[:], constant=0.0)
        S.fence()
        self.release(m_keep)
        yg = [self.sb([128, D], BF16, "yg") for _ in range(4)]
        acc = [self.sb([128, D], F32, "acc") for _ in range(2)]
        for i in range(NT):
            ts_ = slice(i * 128, (i + 1) * 128)
            g = i // 4
            for k in range(4):
                S.add("pool", lambda e, i=i, k=k: e.indirect_dma_start(
                    out=yg[k][:], out_offset=None, in_=Yd, in_offset=bass.IndirectOffsetOnAxis(ap=idx4[:, i, k:k + 1], axis=0)),
                    reads=[tag + "Yd_ready", (tag + "idx", i)], writes=[(tag + "yg", k)], dma=True)
            ac = acc[i % 2]
            E("dve", "tensor_scalar", [(tag + "yg", 0), (tag + "g4", i)], [(tag + "acc", i % 2)], out=ac[:], in0=yg[0][:], scalar1=g4[:, i, 0:1], scalar2=None, op0=ALU.mult)
            for k in range(1, 4):
                E("dve", "scalar_tensor_tensor", [(tag + "yg", k), (tag + "g4", i), (tag + "acc", i % 2)], [(tag + "acc", i % 2)], out=ac[:], in0=yg[k][:],
                  scalar=g4[:, i, k:k + 1], in1=ac[:], op0=ALU.mult, op1=ALU.add)
            for c in range(NCH):
                bk = 6 + c // 4
                E("pe", "transpose", [(tag + "acc", i % 2), "cst"], [("ps", bk)], out=self.psum[bk][:, (c % 4) * 128:(c % 4 + 1) * 128],
                  in_=ac[:, c * 128:(c + 1) * 128], identity=ident)
            for hb in range(2):
                E("dve", "tensor_tensor", [("ps", 6 + hb)] + [("xT", 4 * hb + cc_, g) for cc_ in range(4)], [("xT", 4 * hb + cc_, g) for cc_ in range(4)],
                  out=self.xT[:, 4 * hb:4 * hb + 4, ts_], in0=self.xT[:, 4 * hb:4 * hb + 4, ts_],
                  in1=self.psum[6 + hb][:].rearrange("p (c t) -> p c t", c=4), op=ALU.add)
        S.fence()
        m = self.mark()
        tmp_sq = self.sb([128, NCH, TG], F32, "lnsq")
        tmp_a = [self.sb([128, TG], F32, "lna") for _ in range(2)]
        st_m2 = self.sb([128, TG], F32, "lnm2")
        st_rs = self.sb([128, TG], F32, "lnrs")
        self.layer_norm_fm("ln2_g", "ln2_b", mp, mc, tmp_sq, tmp_a, None, st_m2, st_rs, tag)
        self.release(m)
        for c in range(NCH):
            op = S.add("sp", lambda e, c=c: e.dma_start(out=xdst[c * 128:(c + 1) * 128, :], in_=self.xT[:, c, :]),
                       reads=[("xT", c, g) for g in range(NTG)], writes=[("dram", dstres, c)], dma=True)
            if last:
                self.finals.append(op)


_CACHE = {}


def kernel(**inputs):
    x = np.asarray(inputs["x"], np.float32)
    pos = np.asarray(inputs["positions"], np.int32)
    B = x.shape[0]
    L = DEPTH
    cp = const_pack()
    xps = [mix_pack(l, inputs) for l in range(L)]
    mps = [moe_pack(l, inputs) for l in range(L)]
    if "full" not in _CACHE:
        _CACHE["full"] = Builder(L, moe_cols=mps[0].cols, const_cols=cp.cols, mode="full", mix_cols=xps[0].cols).build()
    nc = _CACHE["full"]
    shared = {
        "consts": cp.array(),
        "mixp": np.stack([p.array() for p in xps]),
        "moep": np.stack([p.array() for p in mps]),
        "w_in_ext": np.stack([w_in_ext(inputs["w_in"][l]) for l in range(L)]),
        "w_out": np.ascontiguousarray(inputs["w_out"], np.float32),
        "w_gate": np.ascontiguousarray(inputs["w_gate"], np.float32),
        "w_up": np.ascontiguousarray(inputs["w_up"], np.float32),
        "w_down": np.ascontiguousarray(inputs["w_down"], np.float32),
    }
    in_maps = []
    for b in range(B):
        m = dict(shared)
        m["xT"] = np.ascontiguousarray(x[b].T)
        m["posb"] = np.ascontiguousarray(np.broadcast_to(pos[b][None, :], (128, T))).astype(np.int32)
        in_maps.append(m)
    res = run_bass_kernel_spmd(nc, in_maps, core_ids=list(range(B)))
    return np.stack([r["outT"].T for r in res.results]).astype(np.float32)
```

```python
import contextlib
import numpy as np
import concourse.bass as bass
import concourse.mybir as mybir
from concourse.bass_utils import run_bass_kernel_spmd

F32 = mybir.dt.float32
BF16 = mybir.dt.bfloat16
I32 = mybir.dt.int32
AF = mybir.ActivationFunctionType
ALU = mybir.AluOpType
AX = mybir.AxisListType

D = 1024
T = 2048
DEPTH = 2
NE = 32
ALPHA = (2.0 * DEPTH) ** 0.25
LN_EPS = 1e-5
NORM_EPS = 1e-6
NCH = 8
NTG = 4
TG = 512

ENGS = ("pe", "act", "dve", "pool", "sp")
EPOCH = 8000
NDMASEM = 10


class Op:
    __slots__ = ("eng", "fn", "dma", "deps", "idx", "sig", "dsem", "dval", "nsig", "prewait")

    def __init__(self, eng, fn, dma):
        self.eng = eng
        self.fn = fn
        self.dma = dma
        self.deps = {}
        self.sig = False
        self.nsig = None
        self.dsem = None
        self.dval = None
        self.prewait = None


class Sched:
    def __init__(self, nc):
        self.nc = nc
        self.ops = []
        self.state = {}
        self.dma_count = {e: 0 for e in ENGS}
        self.dma_hist = {e: [] for e in ENGS}
        self.fence_op = None
        self.fenced = set()
        self.last = {e: None for e in ENGS}

    def _dep(self, op, prod, kind):
        if prod is None or prod is op:
            return
        if prod.dma:
            op.deps[("d", id(prod))] = prod
            return
        if prod.eng == op.eng and not op.dma:
            if op.eng == "pe":
                return
        cur = op.deps.get(prod.eng)
        if cur is None or cur.idx < prod.idx:
            op.deps[prod.eng] = prod

    def add(self, eng, fn, reads=(), writes=(), dma=False):
        op = Op(eng, fn, dma)
        op.idx = len(self.ops)
        if self.fence_op is not None and eng not in self.fenced:
            self.fenced.add(eng)
            for f in self.fence_op:
                self._dep(op, f, "raw" if f.eng != eng else "waw")
        for r in reads:
            st = self.state.get(r)
            if st is None:
                st = self.state[r] = [None, []]
            self._dep(op, st[0], "raw")
            if isinstance(r, tuple) and r[0] == "ps":
                for rd in st[1]:
                    if rd.eng != eng:
                        self._dep(op, rd, "war")
        for w in writes:
            st = self.state.get(w)
            if st is None:
                st = self.state[w] = [None, []]
            self._dep(op, st[0], "waw")
            for rd in st[1]:
                self._dep(op, rd, "war")
        for r in reads:
            self.state[r][1].append(op)
        for w in writes:
            st = self.state[w]
            st[0] = op
            st[1] = []
        if dma:
            k = self.dma_count[eng]
            self.dma_count[eng] = k + 1
            op.dsem = k % NDMASEM
            op.dval = 16 * (k // NDMASEM + 1)
            hist = self.dma_hist[eng]
            if k >= NDMASEM:
                op.prewait = hist[k - NDMASEM]
            hist.append(op)
        else:
            self.last[eng] = op
        self.ops.append(op)
        return op

    def fence(self):
        prods = [o for o in self.last.values() if o is not None]
        for e in ENGS:
            prods.extend(self.dma_hist[e][-NDMASEM:])
        self.fence_op = prods
        self.fenced = set()

    def emit(self, final_ops=()):
        nc = self.nc
        for op in self.ops:
            for p in op.deps.values():
                if not p.dma:
                    p.sig = True
        counts = {e: 0 for e in ENGS}
        for op in self.ops:
            if op.sig and not op.dma:
                counts[op.eng] += 1
                op.nsig = counts[op.eng]
        with contextlib.ExitStack() as es:
            csem = {}
            for e in ENGS:
                n_ep = counts[e] // EPOCH + 1
                csem[e] = [es.enter_context(nc.semaphore(f"c_{e}_{i}")) for i in range(n_ep)]
            dsem = {}
            for e in ENGS:
                if self.dma_count[e]:
                    dsem[e] = [es.enter_context(nc.semaphore(f"d_{e}_{i}"))
                               for i in range(min(NDMASEM, self.dma_count[e]))]
            block = es.enter_context(nc.Block())
            per_eng = {e: [o for o in self.ops if o.eng == e] for e in ENGS}

            def wait_for(engine, p, waited):
                if p.dma:
                    key, val = (p.eng, "d", p.dsem), p.dval
                else:
                    ep, v = divmod(p.nsig - 1, EPOCH)
                    if waited.get((p.eng, "ep"), -1) > ep:
                        return
                    key, val = (p.eng, "c", ep), v + 1
                if waited.get(key, 0) >= val:
                    return
                waited[key] = val
                if p.dma:
                    engine.wait_ge(dsem[p.eng][p.dsem], val)
                else:
                    waited[(p.eng, "ep")] = max(waited.get((p.eng, "ep"), -1), ep)
                    engine.wait_ge(csem[p.eng][ep], val)

            def body(ename, tail):
                def f(engine):
                    waited = {}
                    for op in per_eng[ename]:
                        if op.prewait is not None:
                            wait_for(engine, op.prewait, waited)
                        for p in op.deps.values():
                            wait_for(engine, p, waited)
                        ins = op.fn(engine)
                        if op.dma:
                            ins.then_inc(dsem[ename][op.dsem], 16)
                        elif op.sig:
                            ep, v = divmod(op.nsig - 1, EPOCH)
                            ins.then_inc(csem[ename][ep], 1)
                    if tail:
                        for p in final_ops:
                            wait_for(engine, p, waited)
                return f

            block.sync(body("sp", True))
            block.tensor(body("pe", False))
            block.scalar(body("act", False))
            block.vector(body("dve", False))
            block.gpsimd(body("pool", False))


def _fm(v):
    v = np.asarray(v, np.float32)
    return np.ascontiguousarray(v.reshape(-1, 128).T)


class Pack:
    def __init__(self):
        self.cols = {}
        self.n = 0
        self.parts = []

    def put(self, name, arr):
        arr = np.asarray(arr, np.float32)
        if arr.ndim == 1:
            arr = arr[:, None]
        p, w = arr.shape
        full = np.zeros((128, w), np.float32)
        full[:p] = arr
        self.cols[name] = (self.n, w)
        self.n += w
        self.parts.append(full)

    def array(self):
        return np.ascontiguousarray(np.concatenate(self.parts, axis=1))


def moe_pack(l, inp):
    pk = Pack()
    pk.put("ln2_g", _fm(inp["ln2_g"][l]))
    pk.put("ln2_b", _fm(inp["ln2_b"][l]))
    pk.put("b_gate", inp["b_gate"][l].reshape(NE, 8, 128).transpose(2, 0, 1).reshape(128, NE * 8))
    pk.put("b_up", inp["b_up"][l].reshape(NE, 8, 128).transpose(2, 0, 1).reshape(128, NE * 8))
    pk.put("w_router", inp["w_router"][l].reshape(8, 128, NE).transpose(1, 0, 2).reshape(128, 8 * NE))
    pk.put("b_router", np.broadcast_to(inp["b_router"][l][None, :], (128, NE)))
    pk.put("b_down", inp["b_down"][l])
    return pk


NWIN = 3096 + 512
CAP = 512


def w_in_ext(w_in_l):
    w = np.asarray(w_in_l, np.float32)
    idx = []
    for base in (0, 256):
        for h in range(4):
            idx += list(range(base + 64 * h + 32, base + 64 * h + 64)) + list(range(base + 64 * h, base + 64 * h + 32))
    return np.ascontiguousarray(np.concatenate([w, w[:, idx]], axis=1))


def mix_pack(l, inp):
    pk = Pack()
    pk.put("ln1_g", _fm(inp["ln1_g"][l]))
    pk.put("ln1_b", _fm(inp["ln1_b"][l]))
    cw = np.asarray(inp["ssd_conv_w"][l], np.float32)
    pk.put("conv_w", cw.reshape(4, 6, 128).transpose(2, 1, 0).reshape(128, 24))
    pk.put("conv_b", np.asarray(inp["ssd_conv_b"][l], np.float32).reshape(6, 128).T)
    pk.put("dt_bias", np.asarray(inp["ssd_dt_bias"][l], np.float32).reshape(8, 1))
    pk.put("a_log", np.asarray(inp["ssd_a_log"][l], np.float32).reshape(8, 1))
    pk.put("bgk", np.asarray(inp["gla_b_gk2"][l], np.float32).reshape(2, 64).T)
    pk.put("w_gk2", np.asarray(inp["gla_w_gk2"][l], np.float32))
    pk.put("ret_nw", np.broadcast_to(np.asarray(inp["ret_norm_w"][l], np.float32)[None, :], (128, 256)))
    pk.put("ssd_nw", np.broadcast_to(np.asarray(inp["ssd_norm_w"][l], np.float32)[None, :], (128, 512)))
    pk.put("gla_nw", np.broadcast_to(np.asarray(inp["gla_norm_w"][l], np.float32)[None, :], (128, 256)))
    pk.put("ssd_d", np.broadcast_to(np.asarray(inp["ssd_d"][l], np.float32)[None, :], (128, 8)))
    return pk


def const_pack():
    pk = Pack()
    pk.put("ident", np.eye(128, dtype=np.float32))
    pk.put("onesm", np.full((128, 128), 1.0 / D, np.float32))
    pk.put("ones", np.ones((128, 128), np.float32))
    s_ = np.arange(128)[:, None].astype(np.float64)
    c_ = np.arange(128)[None, :].astype(np.float64)
    causal = (c_ >= s_).astype(np.float64)
    pk.put("causal", causal)
    gam = [1.0 - 2.0 ** (-5.0 - h) for h in range(4)]
    pk.put("retmask", np.concatenate([np.where(c_ >= s_, gam[h] ** np.maximum(c_ - s_, 0.0), 0.0) * 0.125 for h in range(4)], axis=1))
    pk.put("gq", np.concatenate([np.broadcast_to(gam[h] ** (c_ + 1.0), (64, 128)) for h in range(4)], axis=1))
    pk.put("gk", np.concatenate([gam[h] ** (127.0 - s_) * 0.125 for h in range(4)], axis=1))
    d_ = np.arange(128) % 64
    invf = (10000.0 ** (-(d_ % 32).astype(np.float64) / 32.0)).astype(np.float32).astype(np.float64) / (2.0 * np.pi)
    pk.put("invf", invf.reshape(128, 1))
    pk.put("sgn", np.where(d_ < 32, -1.0, 1.0).reshape(128, 1))
    pk.put("gq2", np.concatenate([np.concatenate([np.broadcast_to(gam[2 * p2 + j] ** (c_ + 1.0), (64, 128)) for j in range(2)], axis=0) for p2 in range(2)], axis=1))
    pk.put("triu", (s_ < c_).astype(np.float64))
    pk.put("ec1", np.broadcast_to((np.arange(NE) * CAP + 1.0)[None, :], (128, NE)))
    sel = np.zeros((8, 8 * 128))
    for h in range(8):
        sel[h, h * 128:(h + 1) * 128] = 1.0
    pk.put("sel", sel)
    selm = np.zeros((8, 2 * 4 * 128))
    for g_ in range(2):
        for j_ in range(4):
            selm[4 * g_ + j_, g_ * 512 + j_ * 128:g_ * 512 + (j_ + 1) * 128] = 1.0
    pk.put("selm", selm)
    return pk


class Builder:
    def __init__(self, nlayers, moe_cols, const_cols, mode="full", n_experts=NE, e_lo=0, e_hi=NE, init=True, ln=True,
                 mix_cols=None):
        self.L = nlayers
        self.mode = mode
        self.n_experts = n_experts
        self.e_lo, self.e_hi, self.init, self.ln = e_lo, e_hi, init, ln
        self.mix_cols = mix_cols
        self.moe_cols = moe_cols
        self.const_cols = const_cols
        self.nc = bass.Bass("TRN2", target_bir_lowering=False)
        self.S = Sched(self.nc)
        self.sb_off = (self.nc.sbuf_base + 31) // 32 * 32
        self.sb_top = self.nc.sbuf_top
        self.uid = 0
        self.finals = []

    def sb(self, shape, dt, name=None):
        nbytes = int(np.prod(shape[1:])) * (4 if dt in (F32, I32) else 2)
        nbytes = (nbytes + 31) // 32 * 32
        off = self.sb_off
        assert off + nbytes <= self.sb_top, f"SBUF overflow allocating {name} {shape}: {off + nbytes - self.sb_top}"
        self.sb_off += nbytes
        self.uid += 1
        return self.nc.alloc_sbuf_tensor_at(f"{name or 't'}{self.uid}", list(shape), dt, offset=off)

    def sb_cached(self, key, shape, dt):
        if not hasattr(self, "_cached"):
            self._cached = {}
        if key in self._cached:
            t, off, nbytes = self._cached[key]
            assert off == self.sb_off, (key, off, self.sb_off)
            self.sb_off += nbytes
            return t
        off = self.sb_off
        t = self.sb(shape, dt, key)
        self._cached[key] = (t, off, self.sb_off - off)
        return t

    def mark(self):
        return self.sb_off

    def release(self, m):
        self.sb_off = m

    def dram_in(self, name, shape, dt=F32):
        return self.nc.dram_tensor(name, list(shape), dt, kind="ExternalInput").ap()

    def dram_out(self, name, shape, dt=F32):
        return self.nc.dram_tensor(name, list(shape), dt, kind="ExternalOutput").ap()

    def col(self, tile, cols, name, lo=0, n=None):
        c0, w = cols[name]
        if n is None:
            n = w - lo
        return tile[:, c0 + lo:c0 + lo + n]

    def build(self):
        nc, S, L = self.nc, self.S, self.L
        ncc = max(v[0] + v[1] for v in self.const_cols.values())
        nmc = max(v[0] + v[1] for v in self.moe_cols.values())
        nxc = max(v[0] + v[1] for v in self.mix_cols.values())
        self.d_xT = self.dram_in("xT", [D, T])
        self.d_const = self.dram_in("consts", [128, ncc])
        self.d_out = self.dram_out("outT", [D, T])
        self.d_moep = self.dram_in("moep", [L, 128, nmc])
        self.d_wg = self.dram_in("w_gate", [L, NE, D, D])
        self.d_wu = self.dram_in("w_up", [L, NE, D, D])
        self.d_wd = self.dram_in("w_down", [L, NE, D, D])
        self.d_mixp = self.dram_in("mixp", [L, 128, nxc])
        self.d_win = self.dram_in("w_in_ext", [L, D, NWIN])
        self.d_wout = self.dram_in("w_out", [L, D, D])
        self.d_posb = self.dram_in("posb", [128, T], I32)
        self.d_rot = nc.dram_tensor("rotd", [2, 128, T], F32).ap()
        sA = [nc.dram_tensor(f"scrA{l}", [D, T], F32).ap() for l in range(L)]
        sB = [nc.dram_tensor(f"scrB{l}", [D, T], F32).ap() for l in range(L - 1)]

        NS = NE * CAP + 1
        self.Xd = [nc.dram_tensor(f"Xd{l}", [NS, D], BF16).ap() for l in range(L)]
        self.Yd = [nc.dram_tensor(f"Yd{l}", [NS, D], BF16).ap() for l in range(L)]
        self.cst = self.sb([128, ncc], F32, "cst")
        self.psum = [nc.alloc_psum_tensor(f"ps{i}", [128, 512], F32) for i in range(8)]
        S.add("sp", lambda e: e.dma_start(out=self.cst[:], in_=self.d_const), writes=["cst"], dma=True)
        zt = self.sb([128, D], BF16, "zt")
        zbar = self.sb([1, 8], F32, "zbar")
        S.add("pool", lambda e: e.memset(zt[:], 0.0), writes=["zt"])
        self.identb = self.sb([128, 128], BF16, "identb")
        S.add("act", lambda e: e.activation(out=self.identb[:], in_=self.col(self.cst, self.const_cols, "ident"), func=AF.Copy), reads=["cst"], writes=["identb"])
        base = self.mark()
        for l in range(L):
            src = self.d_xT if l == 0 else sB[l - 1]
            self.mixer_layer(l, src, ("B", l - 1), sA[l], ("A", l))
            if l == 0:
                self.zero_fill(zt, zbar)
            S.fence()
            self.release(base)
            last = (l == L - 1)
            self.moe_layer(l, sA[l], ("A", l), self.d_out if last else sB[l], ("B", l), last)
            S.fence()
            self.release(base)
        S.emit(final_ops=self.finals)
        return nc


    def zero_fill(self, zt, zbar):
        S = self.S
        for l in range(self.L):
            Xd_, Yd_ = self.Xd[l], self.Yd[l]
            S.add("pool", lambda e, Xd_=Xd_: e.dma_start(out=Xd_[0:1, :], in_=zt[0:1, :]), reads=["zt"], writes=[("Xdzp", l, -1)], dma=True)
            for b_ in range(NE * CAP // 128):
                S.add("pool", lambda e, Xd_=Xd_, b_=b_: e.dma_start(out=Xd_[1 + b_ * 128:1 + (b_ + 1) * 128, :], in_=zt[:]),
                      reads=["zt"], writes=[("Xdzp", l, b_)], dma=True)
            S.add("pool", lambda e, Yd_=Yd_: e.dma_start(out=Yd_[0:1, :], in_=zt[0:1, :]), reads=["zt"], writes=[("Ydz", l)], dma=True)
            S.add("pool", lambda e: e.memset(zbar[:], 0.0), reads=[("Xdzp", l, b_) for b_ in range(-1, NE * CAP // 128)], writes=[("Xdz", l), "zbar"])

    def E(self, eng, meth, reads, writes, **kw):
        return self.S.add(eng, lambda e: getattr(e, meth)(**kw), reads=reads, writes=writes)

    def nextq(self):
        self.qctr = (getattr(self, "qctr", -1) + 1) % 6
        return 2 + self.qctr

    def psq(self, i):
        return self.psum[i][:, 0:128]

    def mixer_layer(self, l, xsrc, srcres, xdst, dstres):
        S, E = self.S, self.E
        xc, cc = self.mix_cols, self.const_cols
        nxc = max(v[0] + v[1] for v in xc.values())
        self.xTb = self.sb_cached("xTb", [128, NCH, T], BF16)
        mp = self.sb([128, nxc], F32, "mixp")
        S.add("sp", lambda e: e.dma_start(out=mp[:], in_=self.d_mixp[l]), writes=["mp"], dma=True)
        win = self.sb([128, NCH, NWIN], BF16, "win")
        wout = self.sb([128, NCH, D], BF16, "wout")
        for c in range(NCH):
            S.add("pool", lambda e, c=c: e.dma_start(out=win[:, c, :], in_=self.d_win[l, c * 128:(c + 1) * 128, :]),
                  writes=["win"], dma=True)
        S.add("pool", lambda e: e.dma_start(out=wout[:], in_=self.d_wout[l].rearrange("(c p) f -> p c f", p=128)),
              writes=["wout"], dma=True)
        P = lambda name, lo=0, n=None: self.col(mp, xc, name, lo, n)
        C = lambda name, lo=0, n=None: self.col(self.cst, cc, name, lo, n)
        ident = C("ident")
        xTb = self.xTb
        TWO_PI = 2.0 * np.pi

        m0 = self.mark()
        stg = [self.sb([128, TG], F32, "xstg") for _ in range(2)]
        k = 0
        for c in range(NCH):
            for g in range(NTG):
                st = stg[k % 2]
                S.add("sp", lambda e, c=c, g=g, st=st: e.dma_start(out=st[:], in_=xsrc[c * 128:(c + 1) * 128, g * TG:(g + 1) * TG]),
                      reads=[("dram", srcres, c)], writes=[("xstg", k % 2)], dma=True)
                S.add("act", lambda e, c=c, g=g, st=st: e.activation(out=self.xTb[:, c, g * TG:(g + 1) * TG], in_=st[:], func=AF.Copy),
                      reads=[("xstg", k % 2)], writes=[("xTb", c, g)])
                k += 1
        if l == 0:
            cosT = self.sb([128, T], F32, "cosT")
            sinT = self.sb([128, T], F32, "sinT")
            posi = self.sb([128, T], I32, "posi")
            vf = self.sb([128, T], F32, "vf")
            ni = self.sb([128, T], I32, "ni")
            nf = self.sb([128, T], F32, "nf")
            mk = self.sb([128, T], F32, "mk")
            S.add("sp", lambda e: e.dma_start(out=posi[:], in_=self.d_posb), writes=["posi"], dma=True)
            for which, dst in ((0, sinT), (1, cosT)):
                E("dve", "tensor_copy", ["posi"], ["vf"], out=vf[:], in_=posi[:])
                E("dve", "tensor_scalar", ["vf", "cst"], ["vf"], out=vf[:], in0=vf[:], scalar1=C("invf")[:, 0:1],
                  scalar2=(0.25 if which else 0.0), op0=ALU.mult, op1=ALU.add)
                E("dve", "tensor_copy", ["vf"], ["ni"], out=ni[:], in_=vf[:])
                E("dve", "tensor_copy", ["ni"], ["nf"], out=nf[:], in_=ni[:])
                E("dve", "tensor_tensor", ["vf", "nf"], ["vf"], out=vf[:], in0=vf[:], in1=nf[:], op=ALU.subtract)
                E("dve", "tensor_scalar", ["vf"], ["mk"], out=mk[:], in0=vf[:], scalar1=0.5, scalar2=None, op0=ALU.is_gt)
                E("dve", "tensor_tensor", ["vf", "mk"], ["vf"], out=vf[:], in0=vf[:], in1=mk[:], op=ALU.subtract)
                E("dve", "tensor_scalar", ["vf"], ["mk"], out=mk[:], in0=vf[:], scalar1=-0.5, scalar2=None, op0=ALU.is_lt)
                E("dve", "tensor_tensor", ["vf", "mk"], ["vf"], out=vf[:], in0=vf[:], in1=mk[:], op=ALU.add)
                E("act", "activation", ["vf"], [("rot", which)], out=dst[:], in_=vf[:], func=AF.Sin, scale=TWO_PI)
            E("dve", "tensor_scalar", [("rot", 0), "cst"], [("rot", 0)], out=sinT[:], in0=sinT[:], scalar1=C("sgn")[:, 0:1],
              scalar2=None, op0=ALU.mult)
            S.add("sp", lambda e: e.dma_start(out=self.d_rot[0], in_=sinT[:]), reads=[("rot", 0)], writes=[("rotd", 0)], dma=True)
            S.add("sp", lambda e: e.dma_start(out=self.d_rot[1], in_=cosT[:]), reads=[("rot", 1)], writes=[("rotd", 1)], dma=True)
        S.fence()
        self.release(m0)
        rc = [self.sb([128, 2, 128], F32, "rc") for _ in range(2)]

        negA = self.sb([8, 1], F32, "negA")
        E("act", "activation", ["mp"], ["negA"], out=negA[:], in_=P("a_log")[0:8, 0:1], func=AF.Exp)
        E("dve", "tensor_scalar", ["negA"], ["negA"], out=negA[:], in0=negA[:], scalar1=-1.0, scalar2=None, op0=ALU.mult)
        nbgk = self.sb([64, 2], F32, "nbgk")
        E("dve", "tensor_scalar", ["mp"], ["nbgk"], out=nbgk[:], in0=P("bgk")[0:64, 0:2], scalar1=-1.0, scalar2=None, op0=ALU.mult)
        wgk = self.sb([16, 128], F32, "wgk")
        E("dve", "tensor_copy", ["mp"], ["wgk"], out=wgk[:], in_=P("w_gk2")[0:16, 0:128])

        Sret = self.sb([128, 2, 64], F32, "Sret"); Sretb = self.sb([128, 2, 64], BF16, "Sretb")
        Sssd = self.sb([128, 4, 64], F32, "Sssd"); Sssdb = self.sb([128, 4, 64], BF16, "Sssdb")
        Sgla = self.sb([64, 2, 64], F32, "Sgla"); Sglab = self.sb([64, 2, 64], BF16, "Sglab")
        for t_, nm in ((Sret, "Sret"), (Sretb, "Sretb"), (Sssd, "Sssd"), (Sssdb, "Sssdb"), (Sgla, "Sgla"), (Sglab, "Sglab")):
            for h in range(t_.shape[1]):
                wres = [(nm, 2 * h), (nm, 2 * h + 1)] if nm in ("Sret", "Sretb", "Sgla", "Sglab") else ([(nm, h), (nm, h + 4)] if nm in ("Sssd", "Sssdb") else [(nm, h)])
                E("dve", "memset", [], wres, ap=t_[:, h, :], constant=0.0)
        raw = self.sb([128, 6, 131], F32, "raw")
        E("dve", "memset", [], [("raw", r) for r in range(6)], ap=raw[:], constant=0.0)

        tmv = self.sb([128, 256], BF16, "tm_rv")
        tmg = self.sb([128, 256], F32, "tm_rg")
        tmz = self.sb([128, 512], F32, "tm_sz")
        tgv = self.sb([128, 256], BF16, "tm_gv")
        tgg = self.sb([128, 256], F32, "tm_gg")
        def per(nh, shape, dt, nm):
            return [self.sb(shape, dt, nm) for _ in range(nh)]
        t1_s = per(2, [128, 128], F32, "t1"); t2_s = per(2, [128, 128], F32, "t2")
        rq_s = per(2, [128, 128], BF16, "rq"); rqi_s = per(2, [128, 128], BF16, "rqi")
        rkb_s = per(2, [128, 128], BF16, "rkb")
        kst_s = per(2, [128, 128], BF16, "kst")
        PTr_s = per(4, [128, 128], BF16, "PTr"); PTs_s = per(1, [128, 128], BF16, "PTs"); PTg_s = per(2, [128, 128], BF16, "PTg")
        cacc = self.sb([128, 128], F32, "cacc")
        xsT = self.sb([128, 4, 128], F32, "xsT")
        BT = self.sb([128, 128], F32, "BT"); BTb = self.sb([128, 128], BF16, "BTb")
        CT = self.sb([128, 128], F32, "CT")
        xs_tok = self.sb([128, 8, 64], F32, "xs_tok")
        B_tok = self.sb([128, 2, 64], BF16, "B_tok")
        dtT = self.sb([8, 128], F32, "dtT"); aT = self.sb([8, 128], F32, "aT"); acT = self.sb([8, 128], F32, "acT")
        dt_tok = self.sb([128, 8], F32, "dt_tok"); ac_tok = self.sb([128, 8], F32, "ac_tok")
        Xm4_s = per(2, [128, 4, 128], F32, "Xm4"); PT4_s = per(2, [128, 4, 128], BF16, "PT4")
        EB4_s = per(2, [128, 4, 128], F32, "EB4"); Ct4_s = per(2, [128, 4, 128], BF16, "Ct4")
        wc4_s = per(2, [128, 4], F32, "wc4"); vh4_s = per(2, [128, 4, 64], BF16, "vh4"); vst4_s = per(2, [128, 4, 64], BF16, "vst4")
        rhsb_s = per(2, [8, 4, 128], F32, "rhsb")
        Xm_s = per(1, [128, 128], F32, "Xm")
        EB_s = per(1, [128, 128], F32, "EB")
        Ct_s = per(1, [128, 128], BF16, "Ct")
        Ctb = self.sb([128, 128], BF16, "Ctb")
        wcol_s = per(1, [128, 1], F32, "wcol")
        vh_s = per(1, [128, 64], BF16, "vh"); vst_s = per(1, [128, 64], BF16, "vst")
        gkl = self.sb([16, 128], F32, "gkl")
        la_s = per(2, [64, 128], F32, "la"); bb_s = per(2, [64, 128], F32, "bb")
        eb_s = per(2, [64, 128], F32, "eb"); enb_s = per(2, [64, 128], F32, "enb"); ek_s = per(2, [64, 128], F32, "ek")
        gq_s = per(2, [64, 128], BF16, "gq"); gk__s = per(2, [64, 128], BF16, "gk_"); gkf_s = per(2, [64, 128], F32, "gkf")
        gkh_s = per(2, [64, 128], F32, "gkh"); gkst_s = per(2, [128, 64], BF16, "gkst")
        h_tok = self.sb([128, D], F32, "h_tok")
        hT = self.sb([128, NCH, 128], BF16, "hTm")
        sq = self.sb([128, 512], F32, "sq")
        st1 = self.sb([128, 8], F32, "st1"); st2 = self.sb([128, 8], F32, "st2"); st3 = self.sb([128, 8], F32, "st3")
        yz = self.sb([128, 512], F32, "yz")
        zc = self.sb([128, NCH, 128], F32, "zc")
        zsq = self.sb([128, NCH, 128], F32, "zsq")
        lm2 = self.sb([128, 128], F32, "lm2"); lrs = self.sb([128, 128], F32, "lrs"); lta = self.sb([128, 128], F32, "lta")
        ones_row = C("ones")
        self.cbT = [self.sb([128, 128], F32, "cbT0"), self.sb([128, 128], F32, "cbT1")]

        GAM = [1.0 - 2.0 ** (-5.0 - h) for h in range(4)]
        po = [self.psum[0], self.psum[1]]

        def o_ap(lo, n):
            b_, l_ = divmod(lo, 512)
            assert l_ + n <= 512
            return po[b_][:, l_:l_ + n]

        def proj_fm(n, off, M):
            qi = self.nextq()
            ps = self.psq(qi)
            for c in range(NCH):
                E("pe", "matmul", ["win", ("xTb", c, n // 4)], [("ps", qi)], out=ps[0:M, :], lhsT=win[:, c, off:off + M],
                  rhs=xTb[:, c, n * 128:(n + 1) * 128], start=(c == 0), stop=(c == NCH - 1))
            return qi, ps[0:M, :]

        def proj_tm(n, bank, groups):
            for (off, w, dst) in groups:
                for c in range(NCH):
                    E("pe", "matmul", ["win", ("xTb", c, n // 4)], [("ps", bank)], out=self.psum[bank][:, dst:dst + w],
                      lhsT=xTb[:, c, n * 128:(n + 1) * 128], rhs=win[:, c, off:off + w], start=(c == 0), stop=(c == NCH - 1))

        import os
        for n in range(int(os.environ.get('MIX_CHUNKS', T // 128))):
            tsl = slice(n * 128, (n + 1) * 128)
            proj_tm(n, 2, [(512, 512, 0)])
            E("act", "activation", [("ps", 2)], ["tmv"], out=tmv[:], in_=self.psum[2][:, 0:256], func=AF.Copy)
            E("act", "activation", [("ps", 2)], ["tmg"], out=tmg[:], in_=self.psum[2][:, 256:512], func=AF.Silu)
            proj_tm(n, 3, [(1024, 512, 0)])
            E("act", "activation", [("ps", 3)], ["tmz"], out=tmz[:], in_=self.psum[3][:], func=AF.Silu)
            proj_tm(n, 4, [(2568, 256, 0), (2840, 256, 256)])
            E("act", "activation", [("ps", 4)], ["tgv"], out=tgv[:], in_=self.psum[4][:, 0:256], func=AF.Copy)
            E("act", "activation", [("ps", 4)], ["tgg"], out=tgg[:], in_=self.psum[4][:, 256:512], func=AF.Silu)

            rcn = rc[n % 2]
            S.add("sp", lambda e, rcn=rcn, tsl=tsl: e.dma_start(out=rcn[:], in_=self.d_rot[:, :, tsl].rearrange("w p t -> p w t")),
                  reads=[("rotd", 0), ("rotd", 1)], writes=[("rc", n % 2)], dma=True)
            def ret_stage(p2):
                t1, t2, rq, rqi, rkb, kst = t1_s[p2], t2_s[p2], rq_s[p2], rqi_s[p2], rkb_s[p2], kst_s[p2]
                for kind, offa, offb in (("q", 128 * p2, 3096 + 128 * p2), ("k", 256 + 128 * p2, 3096 + 256 + 128 * p2)):
                    qa, pa = proj_fm(n, offa, 128)
                    qb, pb = proj_fm(n, offb, 128)
                    E("dve", "tensor_tensor", [("ps", qa), ("rc", n % 2)], [("t1", p2)], out=t1[:], in0=pa, in1=rcn[:, 1, :], op=ALU.mult)
                    E("dve", "tensor_tensor", [("ps", qb), ("rc", n % 2)], [("t2", p2)], out=t2[:], in0=pb, in1=rcn[:, 0, :], op=ALU.mult)
                    if kind == "k":
                        E("dve", "tensor_tensor", [("t1", p2), ("t2", p2)], [("rkb", p2)], out=rkb[:], in0=t1[:], in1=t2[:], op=ALU.add)
                    else:
                        E("dve", "tensor_tensor", [("t1", p2), ("t2", p2)], [("rq", p2)], out=rq[:], in0=t1[:], in1=t2[:], op=ALU.add)
                        E("dve", "tensor_tensor", [("t1", p2), ("t2", p2)], [("t1", p2)], out=t1[:], in0=t1[:], in1=t2[:], op=ALU.add)
                        E("dve", "tensor_tensor", [("t1", p2), "cst"], [("rqi", p2)], out=rqi[:], in0=t1[:], in1=C("gq2")[:, p2 * 128:(p2 + 1) * 128], op=ALU.mult)
                qt = self.nextq()
                pbf = self.psum[qt][:].bitcast(BF16)
                E("pe", "transpose", [("rkb", p2), "identb"], [("ps", qt)], out=pbf[:, 0:128], in_=rkb[:], identity=self.identb[:])
                for j in range(2):
                    h = 2 * p2 + j
                    E("dve", "tensor_scalar", [("ps", qt), "cst"], [("kst", p2, j)], out=kst[:, 64 * j:64 * j + 64], in0=pbf[:, 64 * j:64 * j + 64],
                      scalar1=C("gk")[:, h:h + 1], scalar2=None, op0=ALU.mult)
                def stage_b():
                    for j in range(2):
                        h = 2 * p2 + j
                        hs = slice(64 * j, 64 * j + 64)
                        PT = PTr_s[h]
                        qs = self.nextq(); pss = self.psq(qs)
                        E("pe", "matmul", [("rkb", p2), ("rq", p2)], [("ps", qs)], out=pss, lhsT=rkb[hs, :], rhs=rq[hs, :], start=True, stop=True)
                        E("dve", "tensor_tensor", [("ps", qs), "cst"], [("PTr", h)], out=PT[:], in0=pss, in1=C("retmask")[:, h * 128:(h + 1) * 128], op=ALU.mult)
                        E("pe", "matmul", [("PTr", h), "tmv"], [("ps", 0)], out=o_ap(64 * h, 64), lhsT=PT[:], rhs=tmv[:, 64 * h:64 * h + 64], start=True, stop=False)
                        E("pe", "matmul", [("rqi", p2), ("Sretb", h)], [("ps", 0)], out=o_ap(64 * h, 64), lhsT=rqi[hs, :], rhs=Sretb[hs, p2, :], start=False, stop=True)
                    qu = self.nextq(); psu = self.psq(qu)
                    E("pe", "matmul", [("kst", p2, 0), ("kst", p2, 1), "tmv"], [("ps", qu)], out=psu, lhsT=kst[:], rhs=tmv[:, 128 * p2:128 * p2 + 128], start=True, stop=True)
                    for j in range(2):
                        h = 2 * p2 + j
                        hs = slice(64 * j, 64 * j + 64)
                        E("dve", "scalar_tensor_tensor", [("Sret", h), ("ps", qu)], [("Sret", h)], out=Sret[hs, p2, :], in0=Sret[hs, p2, :],
                          scalar=float(GAM[h] ** 128), in1=psu[hs, 64 * j:64 * j + 64], op0=ALU.mult, op1=ALU.add)
                        E("act", "activation", [("Sret", h)], [("Sretb", h)], out=Sretb[hs, p2, :], in_=Sret[hs, p2, :], func=AF.Copy)
                return stage_b

            pend = [ret_stage(0), ret_stage(1)]
            while pend:
                pend.pop(0)()

            for r in range(6):
                qx, px = proj_fm(n, 1536 + 128 * r, 128)
                E("dve", "tensor_copy", [("raw", r)], [("raw", r)], out=raw[:, r, 0:3], in_=raw[:, r, 128:131])
                E("act", "activation", [("ps", qx)], [("raw", r)], out=raw[:, r, 3:131], in_=px, func=AF.Copy)
                cw = P("conv_w")
                E("dve", "tensor_scalar", [("raw", r), "mp"], ["cacc"], out=cacc[:], in0=raw[:, r, 0:128], scalar1=cw[:, 4 * r:4 * r + 1], scalar2=None, op0=ALU.mult)
                for j in range(1, 4):
                    E("dve", "scalar_tensor_tensor", [("raw", r), "mp", "cacc"], ["cacc"], out=cacc[:], in0=raw[:, r, j:j + 128],
                      scalar=cw[:, 4 * r + j:4 * r + j + 1], in1=cacc[:], op0=ALU.mult, op1=ALU.add)
                dst = xsT[:, r, :] if r < 4 else (BT[:] if r == 4 else CT[:])
                dres = ("xsT", r) if r < 4 else ("BT" if r == 4 else "CT")
                E("act", "activation", ["cacc", "mp"], [dres], out=dst, in_=cacc[:], func=AF.Silu, bias=P("conv_b")[:, r:r + 1], scale=1.0)
            E("act", "activation", ["BT"], ["BTb"], out=BTb[:], in_=BT[:], func=AF.Copy)
            E("act", "activation", ["CT"], ["Ctb"], out=Ctb[:], in_=CT[:], func=AF.Copy)
            for r in range(5):
                qt = self.nextq(); pst = self.psq(qt)
                tsrc = xsT[:, r, :] if r < 4 else BT[:]
                sres = ("xsT", r) if r < 4 else "BT"
                E("pe", "transpose", [sres, "cst"], [("ps", qt)], out=pst, in_=tsrc, identity=ident)
                if r < 4:
                    E("act", "activation", [("ps", qt)], [("xs_tok", 2 * r), ("xs_tok", 2 * r + 1)], out=xs_tok[:, 2 * r:2 * r + 2, :],
                      in_=pst.rearrange("p (h d) -> p h d", h=2), func=AF.Copy)
                else:
                    E("act", "activation", [("ps", qt)], [("B_tok", 0), ("B_tok", 1)], out=B_tok[:], in_=pst.rearrange("p (h d) -> p h d", h=2), func=AF.Copy)
            qd, pd = proj_fm(n, 2304, 8)
            E("act", "activation", [("ps", qd), "mp"], ["dtT"], out=dtT[:], in_=pd, func=AF.Exp, bias=P("dt_bias")[0:8, 0:1], scale=1.0)
            E("act", "activation", ["dtT"], ["dtT"], out=dtT[:], in_=dtT[:], func=AF.Ln, bias=1.0, scale=1.0)
            E("dve", "tensor_scalar", ["dtT", "negA"], ["aT"], out=aT[:], in0=dtT[:], scalar1=negA[:, 0:1], scalar2=None, op0=ALU.mult)
            E("dve", "tensor_tensor_scan", ["aT", "cst"], ["acT"], out=acT[:], data0=ones_row[0:8, 0:128], data1=aT[:], initial=0.0, op0=ALU.mult, op1=ALU.add)
            for (srcT, dstt, nm) in ((dtT, dt_tok, "dt_tok"), (acT, ac_tok, "ac_tok")):
                qt = self.nextq(); pst = self.psq(qt)
                E("pe", "transpose", ["dtT" if srcT is dtT else "acT", "cst"], [("ps", qt)], out=pst[:, 0:8], in_=srcT[:], identity=ident[0:8, 0:8])
                E("act", "activation", [("ps", qt)], [nm], out=dstt[:], in_=pst[:, 0:8], func=AF.Copy)
            for g in range(2):
                gs = slice(64 * g, 64 * g + 64)
                qc = self.nextq(); pc = self.psq(qc)
                E("pe", "matmul", ["BTb", "Ctb"], [("ps", qc)], out=pc, lhsT=BTb[gs, :], rhs=Ctb[gs, :], start=True, stop=True)
                cbT = self.cbT[g]
                E("act", "activation", [("ps", qc)], [("cbT", g)], out=cbT[:], in_=pc, func=AF.Copy)
            def ssd_stage(g):
                gs = slice(64 * g, 64 * g + 64)
                Xm4, PT4, EB4, Ct4, wc4, vh4, vst4, rhsb = Xm4_s[g], PT4_s[g], EB4_s[g], Ct4_s[g], wc4_s[g], vh4_s[g], vst4_s[g], rhsb_s[g]
                acg = ac_tok[:, 4 * g:4 * g + 4].rearrange("p (j o) -> p j o", o=1)
                E("dve", "tensor_tensor", ["acT", "cst"], [("rhsb", g)], out=rhsb[:], in0=acT[:].rearrange("k (o c) -> k o c", o=1).to_broadcast([8, 4, 128]),
                  in1=C("selm")[0:8, g * 512:(g + 1) * 512].rearrange("k (j c) -> k j c", j=4), op=ALU.mult)
                qa = self.nextq()
                pab = self.psum[qa][:].rearrange("p (j c) -> p j c", j=4)
                E("pe", "matmul", ["cst", ("rhsb", g)], [("ps", qa)], out=self.psum[qa][:], lhsT=ones_row[0:8, 0:128], rhs=rhsb[:].rearrange("k j c -> k (j c)"), start=True, stop=True)
                E("dve", "tensor_tensor", [("ps", qa), "ac_tok"], [("Xm4", g)], out=Xm4[:], in0=pab, in1=acg.to_broadcast([128, 4, 128]), op=ALU.subtract)
                E("dve", "tensor_scalar", [("Xm4", g)], [("Xm4", g)], out=Xm4[:], in0=Xm4[:], scalar1=0.0, scalar2=None, op0=ALU.min)
                E("act", "activation", [("Xm4", g)], [("Xm4", g)], out=Xm4[:], in_=Xm4[:], func=AF.Exp)
                E("dve", "tensor_tensor", [("Xm4", g), "cst"], [("Xm4", g)], out=Xm4[:], in0=Xm4[:],
                  in1=C("causal").rearrange("p (o c) -> p o c", o=1).to_broadcast([128, 4, 128]), op=ALU.mult)
                E("dve", "tensor_tensor", [("Xm4", g), ("cbT", g)], [("PT4", g)], out=PT4[:], in0=Xm4[:],
                  in1=self.cbT[g][:].rearrange("p (o c) -> p o c", o=1).to_broadcast([128, 4, 128]), op=ALU.mult)
                E("act", "activation", [("ps", qa)], [("EB4", g)], out=EB4[gs, :, :], in_=pab[gs, :, :], func=AF.Exp)
                E("dve", "tensor_tensor", ["CT", ("EB4", g)], [("Ct4", g)], out=Ct4[gs, :, :],
                  in0=CT[gs, :].rearrange("p (o c) -> p o c", o=1).to_broadcast([64, 4, 128]), in1=EB4[gs, :, :], op=ALU.mult)
                E("dve", "tensor_tensor", [("ps", qa), "ac_tok"], [("wc4", g)], out=wc4[:].rearrange("p (j o) -> p j o", o=1), in0=pab[:, :, 127:128], in1=acg, op=ALU.subtract)
                E("act", "activation", [("wc4", g)], [("wc4", g)], out=wc4[:], in_=wc4[:], func=AF.Exp)
                E("dve", "tensor_tensor", [("xs_tok", 4 * g + j) for j in range(4)] + ["dt_tok"], [("vh4", g)], out=vh4[:], in0=xs_tok[:, 4 * g:4 * g + 4, :],
                  in1=dt_tok[:, 4 * g:4 * g + 4].rearrange("p (j o) -> p j o", o=1).to_broadcast([128, 4, 64]), op=ALU.mult)
                E("dve", "tensor_tensor", [("vh4", g), ("wc4", g)], [("vst4", g)], out=vst4[:], in0=vh4[:],
                  in1=wc4[:].rearrange("p (j o) -> p j o", o=1).to_broadcast([128, 4, 64]), op=ALU.mult)
                def stage_b():
                    for j in range(4):
                        h = 4 * g + j
                        E("pe", "matmul", [("PT4", g), ("vh4", g)], [("ps", 1 if h >= 4 else 0)], out=o_ap(256 + 64 * h, 64), lhsT=PT4[:, j, :], rhs=vh4[:, j, :], start=True, stop=False)
                        E("pe", "matmul", [("Ct4", g), ("Sssdb", h)], [("ps", 1 if h >= 4 else 0)], out=o_ap(256 + 64 * h, 64), lhsT=Ct4[gs, j, :], rhs=Sssdb[gs, j, :], start=False, stop=True)
                        qu = self.nextq(); psu = self.psq(qu)
                        E("pe", "matmul", [("B_tok", 0), ("B_tok", 1), ("vst4", g)], [("ps", qu)], out=psu[:, 0:64], lhsT=B_tok[:].rearrange("p g k -> p (g k)"), rhs=vst4[:, j, :], start=True, stop=True)
                        E("dve", "scalar_tensor_tensor", [("Sssd", h), ("ps", qu), ("EB4", g)], [("Sssd", h)], out=Sssd[gs, j, :], in0=Sssd[gs, j, :],
                          scalar=EB4[gs, j, 127:128], in1=psu[gs, 0:64], op0=ALU.mult, op1=ALU.add)
                        E("act", "activation", [("Sssd", h)], [("Sssdb", h)], out=Sssdb[gs, j, :], in_=Sssd[gs, j, :], func=AF.Copy)
                return stage_b

            pend = [ssd_stage(0), ssd_stage(1)]
            while pend:
                pend.pop(0)()

            qg, pg = proj_fm(n, 2824, 16)
            E("act", "activation", [("ps", qg)], ["gkl"], out=gkl[:], in_=pg, func=AF.Copy)
            def gla_stage(p2):
                la, bb, eb, enb, ek, gq, gk_, gkf, gkh, gkst = la_s[p2], bb_s[p2], eb_s[p2], enb_s[p2], ek_s[p2], gq_s[p2], gk__s[p2], gkf_s[p2], gkh_s[p2], gkst_s[p2]
                qk, pk = proj_fm(n, 2440 + 64 * p2, 64)
                E("act", "activation", [("ps", qk)], [("gkf", p2)], out=gkf[:], in_=pk, func=AF.Copy)
                qq, pq = proj_fm(n, 2312 + 64 * p2, 64)
                ql = self.nextq(); pl = self.psq(ql)
                E("pe", "matmul", ["wgk", "gkl"], [("ps", ql)], out=pl[0:64, :], lhsT=wgk[:, 64 * p2:64 * p2 + 64], rhs=gkl[:], start=True, stop=True)
                E("act", "activation", [("ps", ql), "nbgk"], [("la", p2)], out=la[:], in_=pl[0:64, :], func=AF.Exp, bias=nbgk[:, p2:p2 + 1], scale=-1.0)
                E("act", "activation", [("la", p2)], [("la", p2)], out=la[:], in_=la[:], func=AF.Ln, bias=1.0, scale=1.0)
                E("dve", "tensor_scalar", [("la", p2)], [("la", p2)], out=la[:], in0=la[:], scalar1=-1.0 / 16.0, scalar2=None, op0=ALU.mult)
                E("dve", "tensor_tensor_scan", [("la", p2), "cst"], [("bb", p2)], out=bb[:], data0=ones_row[0:64, 0:128], data1=la[:], initial=0.0, op0=ALU.mult, op1=ALU.add)
                E("act", "activation", [("bb", p2)], [("eb", p2)], out=eb[:], in_=bb[:], func=AF.Exp)
                E("act", "activation", [("bb", p2)], [("enb", p2)], out=enb[:], in_=bb[:], func=AF.Exp, scale=-1.0)
                E("act", "activation", [("bb", p2)], [("ek", p2)], out=ek[:], in_=bb[:], func=AF.Exp, scale=-1.0, bias=bb[:, 127:128])
                E("dve", "scalar_tensor_tensor", [("ps", qq), ("eb", p2)], [("gq", p2)], out=gq[:], in0=pq, scalar=float(32 ** -0.5), in1=eb[:], op0=ALU.mult, op1=ALU.mult)
                E("dve", "tensor_tensor", [("gkf", p2), ("enb", p2)], [("gk_", p2)], out=gk_[:], in0=gkf[:], in1=enb[:], op=ALU.mult)
                E("dve", "tensor_tensor", [("gkf", p2), ("ek", p2)], [("gkh", p2)], out=gkh[:], in0=gkf[:], in1=ek[:], op=ALU.mult)
                def stage_b():
                    for j in range(2):
                        h = 2 * p2 + j
                        hs = slice(32 * j, 32 * j + 32)
                        PT = PTg_s[j]
                        qs = self.nextq(); pss = self.psq(qs)
                        E("pe", "matmul", [("gk_", p2), ("gq", p2)], [("ps", qs)], out=pss, lhsT=gk_[hs, :], rhs=gq[hs, :], start=True, stop=True)
                        E("dve", "tensor_tensor", [("ps", qs), "cst"], [("PTg", j)], out=PT[:], in0=pss, in1=C("causal"), op=ALU.mult)
                        E("pe", "matmul", [("PTg", j), "tgv"], [("ps", 1)], out=o_ap(768 + 64 * h, 64), lhsT=PT[:], rhs=tgv[:, 64 * h:64 * h + 64], start=True, stop=False)
                        E("pe", "matmul", [("gq", p2), ("Sglab", h)], [("ps", 1)], out=o_ap(768 + 64 * h, 64), lhsT=gq[hs, :], rhs=Sglab[hs, p2, :], start=False, stop=True)
                    qt = self.nextq(); pst = self.psq(qt)
                    E("pe", "transpose", [("gkh", p2), "cst"], [("ps", qt)], out=pst[:, 0:64], in_=gkh[:], identity=ident[0:64, 0:64])
                    E("act", "activation", [("ps", qt)], [("gkst", p2)], out=gkst[:], in_=pst[:, 0:64], func=AF.Copy)
                    qu = self.nextq(); psu = self.psq(qu)
                    E("pe", "matmul", [("gkst", p2), "tgv"], [("ps", qu)], out=psu[0:64, 0:128], lhsT=gkst[:], rhs=tgv[:, 128 * p2:128 * p2 + 128], start=True, stop=True)
                    for j in range(2):
                        h = 2 * p2 + j
                        hs = slice(32 * j, 32 * j + 32)
                        E("dve", "scalar_tensor_tensor", [("Sgla", h), ("ps", qu), ("eb", p2)], [("Sgla", h)], out=Sgla[hs, p2, :], in0=Sgla[hs, p2, :],
                          scalar=eb[hs, 127:128], in1=psu[hs, 64 * j:64 * j + 64], op0=ALU.mult, op1=ALU.add)
                    E("act", "activation", [("Sgla", 2 * p2), ("Sgla", 2 * p2 + 1)], [("Sglab", 2 * p2), ("Sglab", 2 * p2 + 1)], out=Sglab[:, p2, :], in_=Sgla[:, p2, :], func=AF.Copy)
                return stage_b

            pend = [gla_stage(0), gla_stage(1)]
            while pend:
                pend.pop(0)()

            oret = po[0][:, 0:256]
            E("dve", "tensor_reduce", [("ps", 0)], ["st1"], out=st1[:, 0:4], in_=oret.rearrange("p (h d) -> p h d", h=4), axis=AX.X, op=ALU.add)
            E("act", "activation", [("ps", 0)], ["sq"], out=sq[:, 0:256], in_=oret, func=AF.Square)
            E("dve", "tensor_reduce", ["sq"], ["st2"], out=st2[:, 0:4], in_=sq[:, 0:256].rearrange("p (h d) -> p h d", h=4), axis=AX.X, op=ALU.add)
            E("dve", "tensor_scalar", ["st1"], ["st1"], out=st1[:, 0:4], in0=st1[:, 0:4], scalar1=1.0 / 64, scalar2=None, op0=ALU.mult)
            E("dve", "tensor_tensor", ["st1"], ["st3"], out=st3[:, 0:4], in0=st1[:, 0:4], in1=st1[:, 0:4], op=ALU.mult)
            E("dve", "scalar_tensor_tensor", ["st2", "st3"], ["st2"], out=st2[:, 0:4], in0=st2[:, 0:4], scalar=1.0 / 64, in1=st3[:, 0:4], op0=ALU.mult, op1=ALU.subtract)
            E("dve", "tensor_scalar", ["st2"], ["st2"], out=st2[:, 0:4], in0=st2[:, 0:4], scalar1=LN_EPS, scalar2=None, op0=ALU.add)
            E("act", "activation", ["st2"], ["st2"], out=st2[:, 0:4], in_=st2[:, 0:4], func=AF.Sqrt)
            E("dve", "reciprocal", ["st2"], ["st2"], out=st2[:, 0:4], in_=st2[:, 0:4])
            hr = h_tok[:, 0:256].rearrange("p (h d) -> p h d", h=4)
            E("dve", "tensor_tensor", [("ps", 0), "st1"], ["h_ret"], out=hr, in0=oret.rearrange("p (h d) -> p h d", h=4),
              in1=st1[:, 0:4].to_broadcast([128, 4, 64]) if False else st1[:, 0:4].rearrange("p (h o) -> p h o", o=1).to_broadcast([128, 4, 64]), op=ALU.subtract)
            E("dve", "tensor_tensor", ["h_ret", "st2"], ["h_ret"], out=hr, in0=hr, in1=st2[:, 0:4].rearrange("p (h o) -> p h o", o=1).to_broadcast([128, 4, 64]), op=ALU.mult)
            E("dve", "tensor_tensor", ["h_ret", "mp"], ["h_ret"], out=h_tok[:, 0:256], in0=h_tok[:, 0:256], in1=P("ret_nw"), op=ALU.mult)
            E("dve", "tensor_tensor", ["h_ret", "tmg"], ["h_ret"], out=h_tok[:, 0:256], in0=h_tok[:, 0:256], in1=tmg[:], op=ALU.mult)
            xs3 = xs_tok[:]
            E("dve", "tensor_tensor", [("xs_tok", r) for r in range(8)] + ["mp"], ["yz"], out=yz[:].rearrange("p (h d) -> p h d", h=8), in0=xs3,
              in1=P("ssd_d").rearrange("p (h o) -> p h o", o=1).to_broadcast([128, 8, 64]), op=ALU.mult)
            E("dve", "tensor_tensor", ["yz", ("ps", 0)], ["yz"], out=yz[:, 0:256], in0=yz[:, 0:256], in1=po[0][:, 256:512], op=ALU.add)
            E("dve", "tensor_tensor", ["yz", ("ps", 1)], ["yz"], out=yz[:, 256:512], in0=yz[:, 256:512], in1=po[1][:, 0:256], op=ALU.add)
            E("dve", "tensor_tensor", ["yz", "tmz"], ["yz"], out=yz[:], in0=yz[:], in1=tmz[:], op=ALU.mult)
            E("act", "activation", ["yz"], ["sq"], out=sq[:], in_=yz[:], func=AF.Square)
            E("dve", "tensor_reduce", ["sq"], ["st2"], out=st2[:, 0:2], in_=sq[:].rearrange("p (g d) -> p g d", g=2), axis=AX.X, op=ALU.add)
            E("dve", "tensor_scalar", ["st2"], ["st2"], out=st2[:, 0:2], in0=st2[:, 0:2], scalar1=1.0 / 256, scalar2=NORM_EPS, op0=ALU.mult, op1=ALU.add)
            E("act", "activation", ["st2"], ["st2"], out=st2[:, 0:2], in_=st2[:, 0:2], func=AF.Sqrt)
            E("dve", "reciprocal", ["st2"], ["st2"], out=st2[:, 0:2], in_=st2[:, 0:2])
            hs = h_tok[:, 256:768].rearrange("p (g d) -> p g d", g=2)
            E("dve", "tensor_tensor", ["yz", "st2"], ["h_ssd"], out=hs, in0=yz[:].rearrange("p (g d) -> p g d", g=2),
              in1=st2[:, 0:2].rearrange("p (g o) -> p g o", o=1).to_broadcast([128, 2, 256]), op=ALU.mult)
            E("dve", "tensor_tensor", ["h_ssd", "mp"], ["h_ssd"], out=h_tok[:, 256:768], in0=h_tok[:, 256:768], in1=P("ssd_nw"), op=ALU.mult)
            ogl = po[1][:, 256:512]
            E("act", "activation", [("ps", 1)], ["sq"], out=sq[:, 0:256], in_=ogl, func=AF.Square)
            E("dve", "tensor_reduce", ["sq"], ["st2"], out=st2[:, 0:4], in_=sq[:, 0:256].rearrange("p (h d) -> p h d", h=4), axis=AX.X, op=ALU.add)
            E("dve", "tensor_scalar", ["st2"], ["st2"], out=st2[:, 0:4], in0=st2[:, 0:4], scalar1=1.0 / 64, scalar2=NORM_EPS, op0=ALU.mult, op1=ALU.add)
            E("act", "activation", ["st2"], ["st2"], out=st2[:, 0:4], in_=st2[:, 0:4], func=AF.Sqrt)
            E("dve", "reciprocal", ["st2"], ["st2"], out=st2[:, 0:4], in_=st2[:, 0:4])
            hg = h_tok[:, 768:1024].rearrange("p (h d) -> p h d", h=4)
            E("dve", "tensor_tensor", [("ps", 1), "st2"], ["h_gla"], out=hg, in0=ogl.rearrange("p (h d) -> p h d", h=4),
              in1=st2[:, 0:4].rearrange("p (h o) -> p h o", o=1).to_broadcast([128, 4, 64]), op=ALU.mult)
            E("dve", "tensor_tensor", ["h_gla", "mp"], ["h_gla"], out=h_tok[:, 768:1024], in0=h_tok[:, 768:1024], in1=P("gla_nw"), op=ALU.mult)
            E("dve", "tensor_tensor", ["h_gla", "tgg"], ["h_gla"], out=h_tok[:, 768:1024], in0=h_tok[:, 768:1024], in1=tgg[:], op=ALU.mult)

            for ec in range(NCH):
                hres = "h_ret" if ec < 2 else ("h_ssd" if ec < 6 else "h_gla")
                qt = self.nextq(); pst = self.psq(qt)
                E("pe", "transpose", [hres, "cst"], [("ps", qt)], out=pst, in_=h_tok[:, ec * 128:(ec + 1) * 128], identity=ident)
                E("act", "activation", [("ps", qt)], [("hT", ec)], out=hT[:, ec, :], in_=pst, func=AF.Copy)
            S.add("sp", lambda e, tsl=tsl: e.dma_start(out=zc[:], in_=xsrc[:, tsl].rearrange("(c p) t -> p c t", p=128)),
                  reads=[("dram", srcres, c) for c in range(NCH)], writes=[("zc", c) for c in range(NCH)], dma=True)
            for half, (mt, mres) in enumerate(((sq, "sq"), (yz, "yz"))):
                bk = 2 + half
                pmt = self.psum[bk]
                for ec in range(NCH):
                    E("pe", "matmul", ["wout", ("hT", ec)], [("ps", bk)], out=pmt[:], lhsT=hT[:, ec, :], rhs=wout[:, ec, half * 512:(half + 1) * 512],
                      start=(ec == 0), stop=(ec == NCH - 1))
                E("act", "activation", [("ps", bk)], [mres], out=mt[:], in_=pmt[:], func=AF.Copy)
            for dc in range(NCH):
                mt, mres = (sq, "sq") if dc < 4 else (yz, "yz")
                qm = self.nextq(); pm = self.psq(qm)
                E("pe", "transpose", [mres, "cst"], [("ps", qm)], out=pm, in_=mt[:, (dc % 4) * 128:(dc % 4 + 1) * 128], identity=ident)
                E("dve", "scalar_tensor_tensor", [("zc", dc), ("ps", qm)], [("zc", dc)], out=zc[:, dc, :], in0=zc[:, dc, :], scalar=ALPHA, in1=pm, op0=ALU.mult, op1=ALU.add)
            onesm = C("onesm")
            qm_ = self.nextq(); pmn = self.psq(qm_)
            qq_ = self.nextq(); pqq = self.psq(qq_)
            for c in range(NCH):
                E("act", "activation", [("zc", c)], [("zsq", c)], out=zsq[:, c, :], in_=zc[:, c, :], func=AF.Square)
            for c in range(NCH):
                E("pe", "matmul", [("zc", c), "cst"], [("ps", qm_)], out=pmn, lhsT=onesm, rhs=zc[:, c, :], start=(c == 0), stop=(c == NCH - 1))
            for c in range(NCH):
                E("pe", "matmul", [("zsq", c), "cst"], [("ps", qq_)], out=pqq, lhsT=onesm, rhs=zsq[:, c, :], start=(c == 0), stop=(c == NCH - 1))
            E("act", "activation", [("ps", qm_)], ["lm2"], out=lm2[:], in_=pmn, func=AF.Square)
            E("dve", "tensor_tensor", [("ps", qq_), "lm2"], ["lrs"], out=lrs[:], in0=pqq, in1=lm2[:], op=ALU.subtract)
            E("dve", "tensor_scalar", ["lrs"], ["lrs"], out=lrs[:], in0=lrs[:], scalar1=LN_EPS, scalar2=None, op0=ALU.add)
            E("act", "activation", ["lrs"], ["lrs"], out=lrs[:], in_=lrs[:], func=AF.Sqrt)
            E("dve", "reciprocal", ["lrs"], ["lrs"], out=lrs[:], in_=lrs[:])
            for c in range(NCH):
                E("dve", "tensor_tensor", [("zc", c), ("ps", qm_)], ["lta"], out=lta[:], in0=zc[:, c, :], in1=pmn, op=ALU.subtract)
                E("dve", "tensor_tensor", ["lta", "lrs"], ["lta"], out=lta[:], in0=lta[:], in1=lrs[:], op=ALU.mult)
                E("act", "activation", ["lta", "mp"], [("zc", c)], out=zc[:, c, :], in_=lta[:], func=AF.Identity,
                  scale=P("ln1_g")[:, c:c + 1], bias=P("ln1_b")[:, c:c + 1])
            S.add("sp", lambda e, tsl=tsl: e.dma_start(out=xdst[:, tsl].rearrange("(c p) t -> p c t", p=128), in_=zc[:]),
                  reads=[("zc", c) for c in range(NCH)], writes=[("dramw", dstres, n)], dma=True)

    def layer_norm_fm(self, gcol, bcol, ptile, pcols, tmp_sq, tmp_a, tmp_b, stat_m2, stat_rs, tag):
        S = self.S
        onesm = self.col(self.cst, self.const_cols, "onesm")
        for g in range(NTG):
            sl = slice(g * TG, (g + 1) * TG)
            ps_m, ps_q = self.psum[6], self.psum[7]
            for c in range(NCH):
                S.add("act", lambda e, c=c, sl=sl: e.activation(out=tmp_sq[:, c, :], in_=self.xT[:, c, sl], func=AF.Square),
                      reads=[("xT", c, g)], writes=[(tag + "sq", c)])
            for c in range(NCH):
                S.add("pe", lambda e, c=c, sl=sl: e.matmul(ps_m[:], lhsT=onesm, rhs=self.xT[:, c, sl],
                                                            start=(c == 0), stop=(c == NCH - 1)),
                      reads=[("xT", c, g), "cst"], writes=[("ps", 6)])
            for c in range(NCH):
                S.add("pe", lambda e, c=c: e.matmul(ps_q[:], lhsT=onesm, rhs=tmp_sq[:, c, :],
                                                    start=(c == 0), stop=(c == NCH - 1)),
                      reads=[(tag + "sq", c), "cst"], writes=[("ps", 7)])
            S.add("act", lambda e: e.activation(out=stat_m2[:], in_=ps_m[:], func=AF.Square),
                  reads=[("ps", 6)], writes=[tag + "m2"])
            S.add("dve", lambda e: e.tensor_tensor(out=stat_rs[:], in0=ps_q[:], in1=stat_m2[:], op=ALU.subtract),
                  reads=[("ps", 7), tag + "m2"], writes=[tag + "rs"])
            S.add("dve", lambda e: e.tensor_scalar(out=stat_rs[:], in0=stat_rs[:], scalar1=LN_EPS, scalar2=None, op0=ALU.add),
                  reads=[tag + "rs"], writes=[tag + "rs"])
            S.add("act", lambda e: e.activation(out=stat_rs[:], in_=stat_rs[:], func=AF.Sqrt),
                  reads=[tag + "rs"], writes=[tag + "rs"])
            S.add("dve", lambda e: e.reciprocal(out=stat_rs[:], in_=stat_rs[:]),
                  reads=[tag + "rs"], writes=[tag + "rs"])
            for c in range(NCH):
                ta = tmp_a[c % 2]
                S.add("dve", lambda e, c=c, sl=sl, ta=ta: e.tensor_tensor(out=ta[:], in0=self.xT[:, c, sl], in1=ps_m[:], op=ALU.subtract),
                      reads=[("xT", c, g), ("ps", 6)], writes=[(tag + "ta", c % 2)])
                S.add("dve", lambda e, ta=ta: e.tensor_tensor(out=ta[:], in0=ta[:], in1=stat_rs[:], op=ALU.mult),
                      reads=[(tag + "ta", c % 2), tag + "rs"], writes=[(tag + "ta", c % 2)])
                gs = self.col(ptile, pcols, gcol, c, 1)
                bs = self.col(ptile, pcols, bcol, c, 1)
                S.add("act", lambda e, c=c, sl=sl, ta=ta, gs=gs, bs=bs: e.activation(
                    out=self.xT[:, c, sl], in_=ta[:], func=AF.Identity, scale=gs, bias=bs),
                    reads=[(tag + "ta", c % 2), tag + "p"], writes=[("xT", c, g)])

    def moe_layer(self, l, xsrc, srcres, xdst, dstres, last):
        nc, S, E = self.nc, self.S, self.E
        mc = self.moe_cols
        nmc = max(v[0] + v[1] for v in mc.values())
        tag = f"m{l}"
        Xd, Yd = self.Xd[l], self.Yd[l]
        self.xT = self.sb_cached("xT", [128, NCH, T], F32)
        for c in range(NCH):
            S.add("sp", lambda e, c=c: e.dma_start(out=self.xT[:, c, :], in_=xsrc[c * 128:(c + 1) * 128, :]),
                  reads=[("dramw", srcres, n) for n in range(T // 128)], writes=[("xT", c, g) for g in range(NTG)], dma=True)
        mp = self.sb([128, nmc], F32, "moep")
        S.add("sp", lambda e: e.dma_start(out=mp[:], in_=self.d_moep[l]), writes=[tag + "p"], dma=True)
        m_after_mp = self.mark()
        C = lambda name, lo=0, n=None: self.col(self.cst, self.const_cols, name, lo, n)
        ident, ones = C("ident"), C("ones")
        NT = T // 128
        GT = self.sb([32, T], F32, "GT")
        idx4 = self.sb([128, NT, 4], I32, "idx4")
        g4 = self.sb([128, NT, 4], F32, "g4")
        bar = self.sb([1, 8], F32, "bar")
        m_keep = self.mark()
        tot = self.sb([128, NE], F32, "tot")
        lg = self.sb([128, NE], F32, "lg"); m8 = self.sb([128, 8], F32, "m8"); nmx = self.sb([128, 1], F32, "nmx")
        msk = self.sb([128, NE], F32, "msk"); ex = self.sb([128, NE], F32, "ex"); ssum = self.sb([128, 1], F32, "ssum")
        gts = self.sb([128, NE], F32, "gts"); ptt = self.sb([128, NE], F32, "ptt"); okk = self.sb([128, NE], F32, "okk")
        s1 = self.sb([128, NE], F32, "s1"); m8b = self.sb([128, 8], F32, "m8b"); eq4 = self.sb([128, 4, NE], F32, "eq4")
        x1f = [self.sb([128, D], BF16, "x1f") for _ in range(2)]
        wr = self.col(mp, mc, "w_router"); br = self.col(mp, mc, "b_router")
        E("dve", "memset", [], [tag + "tot"], ap=tot[:], constant=0.0)
        for i in range(NT):
            ts_ = slice(i * 128, (i + 1) * 128)
            g = i // 4
            ps = self.psum[i % 2]
            for c in range(NCH):
                E("pe", "matmul", [("xT", c, g), tag + "p"], [("ps", i % 2)], out=ps[:, 0:NE], lhsT=self.xT[:, c, ts_],
                  rhs=wr[:, c * NE:(c + 1) * NE], start=(c == 0), stop=(c == NCH - 1))
            E("dve", "tensor_tensor", [("ps", i % 2), tag + "p"], [tag + "lg"], out=lg[:], in0=ps[:, 0:NE], in1=br, op=ALU.add)
            E("dve", "max", [tag + "lg"], [tag + "m8"], out=m8[:], in_=lg[:])
            E("dve", "tensor_scalar", [tag + "lg", tag + "m8"], [tag + "msk"], out=msk[:], in0=lg[:], scalar1=m8[:, 3:4], scalar2=None, op0=ALU.is_ge)
            E("dve", "tensor_scalar", [tag + "m8"], [tag + "nmx"], out=nmx[:], in0=m8[:, 0:1], scalar1=-1.0, scalar2=None, op0=ALU.mult)
            E("act", "activation", [tag + "lg", tag + "nmx"], [tag + "ex"], out=ex[:], in_=lg[:], func=AF.Exp, bias=nmx[:, 0:1], scale=1.0)
            E("dve", "tensor_tensor", [tag + "ex", tag + "msk"], [tag + "ex"], out=ex[:], in0=ex[:], in1=msk[:], op=ALU.mult)
            E("dve", "reduce_sum", [tag + "ex"], [tag + "ssum"], out=ssum[:], in_=ex[:], axis=AX.X)
            E("dve", "reciprocal", [tag + "ssum"], [tag + "ssum"], out=ssum[:], in_=ssum[:])
            E("dve", "tensor_scalar", [tag + "ex", tag + "ssum"], [tag + "gts"], out=gts[:], in0=ex[:], scalar1=ssum[:, 0:1], scalar2=None, op0=ALU.mult)
            pt = self.psum[2 + i % 2]
            E("pe", "transpose", [tag + "gts", "cst"], [("ps", 2 + i % 2)], out=pt[0:NE, 0:128], in_=gts[:], identity=ident)
            E("act", "activation", [("ps", 2 + i % 2)], [(tag + "GT", g)], out=GT[:, ts_], in_=pt[0:NE, 0:128], func=AF.Copy)
            pp = self.psum[4]
            E("pe", "matmul", [tag + "msk", "cst"], [("ps", 4)], out=pp[:, 0:NE], lhsT=C("triu"), rhs=msk[:], start=True, stop=True)
            E("dve", "tensor_tensor", [("ps", 4), tag + "tot"], [tag + "ptt"], out=ptt[:], in0=pp[:, 0:NE], in1=tot[:], op=ALU.add)
            pq = self.psum[5]
            E("pe", "matmul", [tag + "msk", "cst"], [("ps", 5)], out=pq[:, 0:NE], lhsT=ones, rhs=msk[:], start=True, stop=True)
            E("dve", "tensor_tensor", [("ps", 5), tag + "tot"], [tag + "tot"], out=tot[:], in0=pq[:, 0:NE], in1=tot[:], op=ALU.add)
            E("dve", "tensor_scalar", [tag + "ptt"], [tag + "okk"], out=okk[:], in0=ptt[:], scalar1=float(CAP), scalar2=None, op0=ALU.is_lt)
            E("dve", "tensor_tensor", [tag + "okk", tag + "msk"], [tag + "okk"], out=okk[:], in0=okk[:], in1=msk[:], op=ALU.mult)
            E("dve", "tensor_tensor", [tag + "ptt", "cst"], [tag + "s1"], out=s1[:], in0=ptt[:], in1=C("ec1"), op=ALU.add)
            E("dve", "tensor_tensor", [tag + "s1", tag + "okk"], [tag + "s1"], out=s1[:], in0=s1[:], in1=okk[:], op=ALU.mult)
            E("dve", "max", [tag + "s1"], [tag + "m8b"], out=m8b[:], in_=s1[:])
            E("dve", "tensor_copy", [tag + "m8b"], [(tag + "idx", i)], out=idx4[:, i, :], in_=m8b[:, 0:4])
            E("dve", "tensor_tensor", [tag + "s1", tag + "m8b"], [tag + "eq4"], out=eq4[:],
              in0=s1[:].rearrange("p (o e) -> p o e", o=1).to_broadcast([128, 4, NE]),
              in1=m8b[:, 0:4].rearrange("p (k o) -> p k o", o=1).to_broadcast([128, 4, NE]), op=ALU.is_equal)
            E("dve", "tensor_tensor", [tag + "eq4", tag + "gts"], [tag + "eq4"], out=eq4[:], in0=eq4[:],
              in1=gts[:].rearrange("p (o e) -> p o e", o=1).to_broadcast([128, 4, NE]), op=ALU.mult)
            E("dve", "tensor_reduce", [tag + "eq4"], [(tag + "g4", i)], out=g4[:, i, :], in_=eq4[:], axis=AX.X, op=ALU.add)
            xf = x1f[i % 2]
            for c in range(NCH):
                bk = 6 + c // 4
                E("pe", "transpose", [("xT", c, g), "cst"], [("ps", bk)], out=self.psum[bk][:, (c % 4) * 128:(c % 4 + 1) * 128],
                  in_=self.xT[:, c, ts_], identity=ident)
            for hb in range(2):
                E("act", "activation", [("ps", 6 + hb)], [(tag + "x1f", i % 2)], out=xf[:, hb * 512:(hb + 1) * 512], in_=self.psum[6 + hb][:], func=AF.Copy)
            for k in range(4):
                S.add("pool", lambda e, xf=xf, i=i, k=k: e.indirect_dma_start(
                    out=Xd, out_offset=bass.IndirectOffsetOnAxis(ap=idx4[:, i, k:k + 1], axis=0), in_=xf[:], in_offset=None),
                    reads=[(tag + "x1f", i % 2), (tag + "idx", i), ("Xdz", l)], writes=[(tag + "Xd", i, k)], dma=True)
        bd = self.col(mp, mc, "b_down")
        for c in range(NCH):
            for g in range(NTG):
                sl = slice(g * TG, (g + 1) * TG)
                bk = (c * NTG + g) % 2
                ps = self.psum[bk]
                E("pe", "matmul", [tag + "p", (tag + "GT", g)], [("ps", bk)], out=ps[:], lhsT=bd[0:NE, c * 128:(c + 1) * 128], rhs=GT[:, sl], start=True, stop=True)
                E("dve", "scalar_tensor_tensor", [("xT", c, g), ("ps", bk)], [("xT", c, g)], out=self.xT[:, c, sl], in0=self.xT[:, c, sl],
                  scalar=ALPHA, in1=ps[:], op0=ALU.mult, op1=ALU.add)
        E("dve", "memset", [(tag + "Xd", i, k) for i in range(NT) for k in range(4)], [tag + "Xd_ready", tag + "bar"], ap=bar[:], constant=0.0)
        NSLOT, QW = 6, 512
        ring = [self.sb([128, NCH, QW], BF16, "wr") for _ in range(NSLOT)]
        xe = self.sb([128, CAP // 128, D], BF16, "xe")
        xeT = self.sb([128, NCH, CAP], BF16, "xeT")
        hT = self.sb([128, NCH, CAP], BF16, "hT")
        ye = [self.sb([128, CAP // 128, D], BF16, "ye") for _ in range(2)]
        tmpg = [self.sb([128, CAP], F32, "tg") for _ in range(2)]
        tmps = [self.sb([128, CAP], BF16, "ts") for _ in range(2)]
        tmpu = [self.sb([128, CAP], BF16, "tu") for _ in range(2)]
        slot_ctr = [0]

        def load_q(dram_w, e_, q):
            s = slot_ctr[0] % NSLOT
            slot_ctr[0] += 1
            wsrc = dram_w[l, e_, :, q * QW:(q + 1) * QW].rearrange("(c p) f -> p c f", p=128)
            S.add("pool", lambda e, s=s, wsrc=wsrc: e.dma_start(out=ring[s][:], in_=wsrc), writes=[(tag + "ring", s)], dma=True)
            return s

        bgc = self.col(mp, mc, "b_gate"); buc = self.col(mp, mc, "b_up")
        import os
        nexp = int(os.environ.get("N_EXPERTS", NE))
        tcnt = 0
        for ex_ in range(nexp):
            r0 = 1 + ex_ * CAP
            S.add("sp", lambda e, r0=r0: e.dma_start(out=xe[:], in_=Xd[r0:r0 + CAP, :].rearrange("(s p) d -> p s d", p=128)),
                  reads=[tag + "Xd_ready"], writes=[tag + "xe"], dma=True)
            for c in range(NCH):
                bk = 6 + c % 2
                pbf = self.psum[bk][:].bitcast(BF16)
                for st in range(CAP // 128):
                    E("pe", "transpose", [tag + "xe", "identb"], [("ps", bk)], out=pbf[:, st * 128:(st + 1) * 128],
                      in_=xe[:, st, c * 128:(c + 1) * 128], identity=self.identb[:])
                E("act", "activation", [("ps", bk)], [(tag + "xeT", c)], out=xeT[:, c, :], in_=pbf[:, 0:CAP], func=AF.Copy)
            sg = su = None
            for f in range(NCH):
                if f % 4 == 0:
                    sg = load_q(self.d_wg, ex_, f // 4)
                    su = load_q(self.d_wu, ex_, f // 4)
                fi = f % 4
                k = tcnt % 2
                tcnt += 1
                pg, pu = self.psum[k], self.psum[2 + k]
                for c in range(NCH):
                    E("pe", "matmul", [(tag + "ring", sg), (tag + "xeT", c)], [("ps", k)], out=pg[:, 0:CAP], lhsT=ring[sg][:, c, fi * 128:(fi + 1) * 128],
                      rhs=xeT[:, c, :], start=(c == 0), stop=(c == NCH - 1))
                for c in range(NCH):
                    E("pe", "matmul", [(tag + "ring", su), (tag + "xeT", c)], [("ps", 2 + k)], out=pu[:, 0:CAP], lhsT=ring[su][:, c, fi * 128:(fi + 1) * 128],
                      rhs=xeT[:, c, :], start=(c == 0), stop=(c == NCH - 1))
                bg1 = bgc[:, ex_ * 8 + f:ex_ * 8 + f + 1]
                bu1 = buc[:, ex_ * 8 + f:ex_ * 8 + f + 1]
                tg_, ts2, tu_ = tmpg[k], tmps[k], tmpu[k]
                E("dve", "tensor_scalar", [("ps", k), tag + "p"], [(tag + "tg", k)], out=tg_[:], in0=pg[:, 0:CAP], scalar1=bg1, scalar2=7.0, op0=ALU.add, op1=ALU.min)
                E("act", "activation", [(tag + "tg", k)], [(tag + "ts", k)], out=ts2[:], in_=tg_[:], func=AF.Sigmoid, scale=1.702)
                E("dve", "tensor_scalar", [("ps", 2 + k), tag + "p"], [(tag + "tu", k)], out=tu_[:], in0=pu[:, 0:CAP], scalar1=bu1, scalar2=7.0, op0=ALU.add, op1=ALU.min)
                E("dve", "tensor_scalar", [(tag + "tu", k)], [(tag + "tu", k)], out=tu_[:], in0=tu_[:], scalar1=-7.0, scalar2=1.0, op0=ALU.max, op1=ALU.add)
                E("dve", "tensor_tensor", [(tag + "tg", k), (tag + "ts", k)], [(tag + "ts", k)], out=ts2[:], in0=tg_[:], in1=ts2[:], op=ALU.mult)
                E("dve", "tensor_tensor", [(tag + "tu", k), (tag + "ts", k)], [(tag + "hT", f)], out=hT[:, f, :], in0=tu_[:], in1=ts2[:], op=ALU.mult)
            yy = ye[ex_ % 2]
            ycnt = 0
            for half in range(2):
                sd = load_q(self.d_wd, ex_, half)
                for st in range(CAP // 128):
                    bk = 4 + ycnt % 2
                    ycnt += 1
                    py = self.psum[bk]
                    for f in range(NCH):
                        E("pe", "matmul", [(tag + "ring", sd), (tag + "hT", f)], [("ps", bk)], out=py[:], lhsT=hT[:, f, st * 128:(st + 1) * 128],
                          rhs=ring[sd][:, f, :], start=(f == 0), stop=(f == NCH - 1))
                    E("act", "activation", [("ps", bk)], [(tag + "ye", ex_ % 2)], out=yy[:, st, half * 512:(half + 1) * 512], in_=py[:], func=AF.Copy)
            S.add("sp", lambda e, r0=r0, yy=yy: e.dma_start(out=Yd[r0:r0 + CAP, :].rearrange("(s p) d -> p s d", p=128), in_=yy[:]),
                  reads=[(tag + "ye", ex_ % 2)], writes=[(tag + "Yd", ex_)], dma=True)
        E("dve", "memset", [(tag + "Yd", e_) for e_ in range(nexp)] + [("Ydz", l)], [tag + "Yd_ready", tag + "bar"], ap=bar[:], constant=0.0)
        S.fence()
        self.release(m_keep)
        yg = [self.sb([128, D], BF16, "yg") for _ in range(4)]
        acc = [self.sb([128, D], F32, "acc") for _ in range(2)]
        for i in range(NT):
            ts_ = slice(i * 128, (i + 1) * 128)
            g = i // 4
            for k in range(4):
                S.add("pool", lambda e, i=i, k=k: e.indirect_dma_start(
                    out=yg[k][:], out_offset=None, in_=Yd, in_offset=bass.IndirectOffsetOnAxis(ap=idx4[:, i, k:k + 1], axis=0)),
                    reads=[tag + "Yd_ready", (tag + "idx", i)], writes=[(tag + "yg", k)], dma=True)
            ac = acc[i % 2]
            E("dve", "tensor_scalar", [(tag + "yg", 0), (tag + "g4", i)], [(tag + "acc", i % 2)], out=ac[:], in0=yg[0][:], scalar1=g4[:, i, 0:1], scalar2=None, op0=ALU.mult)
            for k in range(1, 4):
                E("dve", "scalar_tensor_tensor", [(tag + "yg", k), (tag + "g4", i), (tag + "acc", i % 2)], [(tag + "acc", i % 2)], out=ac[:], in0=yg[k][:],
                  scalar=g4[:, i, k:k + 1], in1=ac[:], op0=ALU.mult, op1=ALU.add)
            for c in range(NCH):
                bk = 6 + c // 4
                E("pe", "transpose", [(tag + "acc", i % 2), "cst"], [("ps", bk)], out=self.psum[bk][:, (c % 4) * 128:(c % 4 + 1) * 128],
                  in_=ac[:, c * 128:(c + 1) * 128], identity=ident)
            for hb in range(2):
                E("dve", "tensor_tensor", [("ps", 6 + hb)] + [("xT", 4 * hb + cc_, g) for cc_ in range(4)], [("xT", 4 * hb + cc_, g) for cc_ in range(4)],
                  out=self.xT[:, 4 * hb:4 * hb + 4, ts_], in0=self.xT[:, 4 * hb:4 * hb + 4, ts_],
                  in1=self.psum[6 + hb][:].rearrange("p (c t) -> p c t", c=4), op=ALU.add)
        S.fence()
        m = self.mark()
        tmp_sq = self.sb([128, NCH, TG], F32, "lnsq")
        tmp_a = [self.sb([128, TG], F32, "lna") for _ in range(2)]
        st_m2 = self.sb([128, TG], F32, "lnm2")
        st_rs = self.sb([128, TG], F32, "lnrs")
        self.layer_norm_fm("ln2_g", "ln2_b", mp, mc, tmp_sq, tmp_a, None, st_m2, st_rs, tag)
        self.release(m)
        for c in range(NCH):
            op = S.add("sp", lambda e, c=c: e.dma_start(out=xdst[c * 128:(c + 1) * 128, :], in_=self.xT[:, c, :]),
                       reads=[("xT", c, g) for g in range(NTG)], writes=[("dram", dstres, c)], dma=True)
            if last:
                self.finals.append(op)


_CACHE = {}


def kernel(**inputs):
    x = np.asarray(inputs["x"], np.float32)
    pos = np.asarray(inputs["positions"], np.int32)
    B = x.shape[0]
    L = DEPTH
    cp = const_pack()
    xps = [mix_pack(l, inputs) for l in range(L)]
    mps = [moe_pack(l, inputs) for l in range(L)]
    if "full" not in _CACHE:
        _CACHE["full"] = Builder(L, moe_cols=mps[0].cols, const_cols=cp.cols, mode="full", mix_cols=xps[0].cols).build()
    nc = _CACHE["full"]
    shared = {
        "consts": cp.array(),
        "mixp": np.stack([p.array() for p in xps]),
        "moep": np.stack([p.array() for p in mps]),
        "w_in_ext": np.stack([w_in_ext(inputs["w_in"][l]) for l in range(L)]),
        "w_out": np.ascontiguousarray(inputs["w_out"], np.float32),
        "w_gate": np.ascontiguousarray(inputs["w_gate"], np.float32),
        "w_up": np.ascontiguousarray(inputs["w_up"], np.float32),
        "w_down": np.ascontiguousarray(inputs["w_down"], np.float32),
    }
    in_maps = []
    for b in range(B):
        m = dict(shared)
        m["xT"] = np.ascontiguousarray(x[b].T)
        m["posb"] = np.ascontiguousarray(np.broadcast_to(pos[b][None, :], (128, T))).astype(np.int32)
        in_maps.append(m)
    res = run_bass_kernel_spmd(nc, in_maps, core_ids=list(range(B)))
    return np.stack([r["outT"].T for r in res.results]).astype(np.float32)
```
